# Optimizing a Trainium2 kernel written in Bass

```python
import jax, jax.numpy as jnp
from jax import lax
import numpy as np


D_MODEL = 2048
BATCH = 2
SEQ = 8192
DEPTH = 4

GRID_W = 64
CTX_LEN = 256
HEAD_DIM = 128
EPS = 1e-6
NEG_INF = -1e30
NA_HEADS = D_MODEL // 256
NA_WIN_ROWS = 8
NA_WIN_COLS = 16
NA_COL_BLOCK = 16
NA_KEY_COLS = NA_COL_BLOCK + NA_WIN_COLS
CONV_DIM = D_MODEL // 2
CONV_WIDTH = 31
WA_HEADS = D_MODEL // 256
WA_KV_HEADS = WA_HEADS // 4
WA_WINDOW = 128
WA_BLOCK = 128
ROPE_BASE = 10000.0
ROPE_FREQS = HEAD_DIM // 4
N_BRANCH = 3
BRANCH_DIM = D_MODEL // 2
D_FF = 5632
N_EXPERTS = 8
TOP_K = 2
D_FF_EXPERT = 2 * D_MODEL
IN_SPLITS = (NA_HEADS * HEAD_DIM, NA_HEADS * HEAD_DIM, NA_HEADS * HEAD_DIM,
             WA_HEADS * HEAD_DIM, WA_KV_HEADS * HEAD_DIM, WA_KV_HEADS * HEAD_DIM,
             2 * CONV_DIM, N_BRANCH * D_MODEL)
IN_DIM = sum(IN_SPLITS)

kernel_name = 'hybrid_na_conformer_swa_moe_dit'


def rms_norm(x, g):
    xf = x.astype(jnp.float32)
    y = xf * lax.rsqrt(jnp.mean(xf * xf, axis=-1, keepdims=True) + EPS)
    return (y * g.astype(jnp.float32)).astype(x.dtype)


def modulate(h, shift, scale):
    return h * (1.0 + scale) + shift


def split_proj(u):
    idx = [int(i) for i in np.cumsum(IN_SPLITS)[:-1]]
    return jnp.split(u, idx, axis=-1)


def heads(t, n):
    return t.reshape(*t.shape[:-1], n, HEAD_DIM)


def axial_rope_tables(n_tokens):
    t = jnp.arange(n_tokens, dtype=jnp.int32)
    pos = jnp.stack([t // GRID_W, t % GRID_W], axis=-1).astype(jnp.float32)
    inv_freq = 1.0 / jnp.power(ROPE_BASE, jnp.arange(ROPE_FREQS, dtype=jnp.float32) / ROPE_FREQS)
    ang = pos[:, :, None] * inv_freq
    return jnp.cos(ang), jnp.sin(ang)


def apply_axial_rope(x, cos, sin):
    B, S, H, hd = x.shape
    xf = x.astype(jnp.float32).reshape(B, S, H, 2, 2, ROPE_FREQS)
    x1, x2 = xf[..., 0, :], xf[..., 1, :]
    cs, sn = cos[None, :, None], sin[None, :, None]
    out = jnp.stack([x1 * cs - x2 * sn, x2 * cs + x1 * sn], axis=-2)
    return out.reshape(B, S, H, hd).astype(x.dtype)


def neighbourhood_attention(q, k, v, kc, vc, rpb):
    B, S, H, hd = q.shape
    rows = S // GRID_W
    kh = min(NA_WIN_ROWS, rows)
    n_cb = GRID_W // NA_COL_BLOCK
    q5 = (q * hd ** -0.5).reshape(B, rows, GRID_W, H, hd)
    k5 = k.reshape(B, rows, GRID_W, H, hd)
    v5 = v.reshape(B, rows, GRID_W, H, hd)
    cb = np.arange(n_cb)[:, None]
    q_col = cb * NA_COL_BLOCK + np.arange(NA_COL_BLOCK)[None]
    k_start = np.clip(cb * NA_COL_BLOCK - NA_WIN_COLS // 2, 0, GRID_W - NA_KEY_COLS)
    k_col = k_start + np.arange(NA_KEY_COLS)[None]
    w_start = np.clip(q_col - NA_WIN_COLS // 2, 0, GRID_W - NA_WIN_COLS)
    kc3, ws3 = k_col[:, None, :], w_start[:, :, None]
    col_valid = (kc3 >= ws3) & (kc3 < ws3 + NA_WIN_COLS)
    col_bias_idx = np.clip(kc3 - q_col[:, :, None] + NA_WIN_COLS - 1, 0, 2 * NA_WIN_COLS - 2)
    rpb_f = rpb.astype(jnp.float32)
    n_lat = kh * NA_KEY_COLS

    def row_block(r):
        r0 = jnp.clip(r - kh // 2, 0, rows - kh)
        qb = lax.dynamic_index_in_dim(q5, r, axis=1, keepdims=False).reshape(B, n_cb, NA_COL_BLOCK, H, hd)
        kb = lax.dynamic_slice_in_dim(k5, r0, kh, axis=1)[:, :, k_col]
        vb = lax.dynamic_slice_in_dim(v5, r0, kh, axis=1)[:, :, k_col]
        row_idx = r0 + jnp.arange(kh) - r + NA_WIN_ROWS - 1
        bias = rpb_f[:, row_idx[:, None, None, None], col_bias_idx[None]]
        bias = jnp.transpose(bias, (0, 2, 3, 1, 4))
        s_lat = jnp.einsum('bjqhd,brjkhd->bhjqrk', qb, kb).astype(jnp.float32) + bias
        s_lat = jnp.where(col_valid[:, :, None, :], s_lat, NEG_INF)
        s_lat = s_lat.reshape(B, H, n_cb, NA_COL_BLOCK, n_lat)
        s_ctx = jnp.einsum('bjqhd,bmhd->bhjqm', qb, kc).astype(jnp.float32)
        p = jax.nn.softmax(jnp.concatenate([s_lat, s_ctx], axis=-1), axis=-1).astype(v.dtype)
        p_lat = p[..., :n_lat].reshape(B, H, n_cb, NA_COL_BLOCK, kh, NA_KEY_COLS)
        o = (jnp.einsum('bhjqrk,brjkhd->bjqhd', p_lat, vb)
             + jnp.einsum('bhjqm,bmhd->bjqhd', p[..., n_lat:], vc))
        return o.reshape(B, GRID_W, H, hd)

    out = lax.map(row_block, jnp.arange(rows))
    return jnp.moveaxis(out, 0, 1).reshape(B, S, H, hd)


def window_attention(q, k, v, kc, vc, sink):
    B, S, H, hd = q.shape
    kvh = k.shape[2]
    g = H // kvh
    nb = S // WA_BLOCK
    qb = (q * hd ** -0.5).reshape(B, nb, WA_BLOCK, kvh, g, hd)

    def banded(t):
        tp = jnp.pad(t, ((0, 0), (WA_BLOCK, WA_BLOCK), (0, 0), (0, 0))).reshape(B, nb + 2, WA_BLOCK, kvh, hd)
        return jnp.concatenate([tp[:, :-2], tp[:, 1:-1], tp[:, 2:]], axis=2)

    kw, vw = banded(k), banded(v)
    blk = np.arange(nb)[:, None, None] * WA_BLOCK
    qpos = blk + np.arange(WA_BLOCK)[None, :, None]
    kpos = blk - WA_BLOCK + np.arange(3 * WA_BLOCK)[None, None, :]
    valid = (np.abs(qpos - kpos) <= WA_WINDOW) & (kpos >= 0) & (kpos < S)
    s_lat = jnp.einsum('bnqkgd,bnmkd->bkgnqm', qb, kw).astype(jnp.float32)
    s_lat = jnp.where(valid, s_lat, NEG_INF)
    s_ctx = jnp.einsum('bnqkgd,bmkd->bkgnqm', qb, kc).astype(jnp.float32)
    s_sink = jnp.broadcast_to(sink.astype(jnp.float32).reshape(kvh, g)[None, :, :, None, None, None],
                              s_ctx.shape[:-1] + (1,))
    p = jax.nn.softmax(jnp.concatenate([s_lat, s_ctx, s_sink], axis=-1), axis=-1).astype(v.dtype)
    n_lat = 3 * WA_BLOCK
    n_ctx = kc.shape[1]
    o = (jnp.einsum('bkgnqm,bnmkd->bnqkgd', p[..., :n_lat], vw)
         + jnp.einsum('bkgnqm,bmkd->bnqkgd', p[..., n_lat:n_lat + n_ctx], vc))
    return o.reshape(B, S, H, hd)


def context_attention(q, k, v, sink):
    B, L, H, hd = q.shape
    kvh = k.shape[2]
    g = H // kvh
    qg = (q * hd ** -0.5).reshape(B, L, kvh, g, hd)
    s = jnp.einsum('blkgd,bmkd->bkglm', qg, k).astype(jnp.float32)
    if sink is not None:
        s_sink = jnp.broadcast_to(sink.astype(jnp.float32).reshape(kvh, g)[None, :, :, None, None],
                                  s.shape[:-1] + (1,))
        s = jnp.concatenate([s, s_sink], axis=-1)
    p = jax.nn.softmax(s, axis=-1)[..., :L].astype(v.dtype)
    o = jnp.einsum('bkglm,bmkd->blkgd', p, v)
    return o.reshape(B, L, H, hd)


def conformer_conv(u, w, b, ln_g, ln_b):
    a, gt = jnp.split(u, 2, axis=-1)
    z = a * jax.nn.sigmoid(gt)
    z = lax.conv_general_dilated(z, w[:, None, :], window_strides=(1,),
                                 padding=[(CONV_WIDTH // 2, CONV_WIDTH // 2)],
                                 dimension_numbers=('NWC', 'WIO', 'NWC'),
                                 feature_group_count=CONV_DIM) + b
    zf = z.astype(jnp.float32)
    mu = jnp.mean(zf, axis=-1, keepdims=True)
    var = jnp.mean(jnp.square(zf - mu), axis=-1, keepdims=True)
    zn = (zf - mu) * lax.rsqrt(var + EPS) * ln_g.astype(jnp.float32) + ln_b.astype(jnp.float32)
    return jax.nn.silu(zn.astype(u.dtype))


def merge_branches(outs, gates, w_branch, w_out):
    gs = jax.nn.sigmoid(gates.reshape(*gates.shape[:-1], N_BRANCH, D_MODEL))
    y = gs[..., 0, :] * (outs[0] @ w_branch[0])
    for i in range(1, N_BRANCH):
        y = y + gs[..., i, :] * (outs[i] @ w_branch[i])
    return y @ w_out


def token_mixer(h, hc, w_in, rpb, conv_w, conv_b, ln_g, ln_b, sink, w_branch, w_out, cos, sin, with_ctx_out):
    B, S, _ = h.shape
    Lc = hc.shape[1]
    na_q, na_k, na_v, wa_q, wa_k, wa_v, glu, gates = split_proj(h @ w_in)
    cna_q, cna_k, cna_v, cwa_q, cwa_k, cwa_v, cglu, cgates = split_proj(hc @ w_in)
    kc_na, vc_na = heads(cna_k, NA_HEADS), heads(cna_v, NA_HEADS)
    kc_wa, vc_wa = heads(cwa_k, WA_KV_HEADS), heads(cwa_v, WA_KV_HEADS)
    o_na = neighbourhood_attention(heads(na_q, NA_HEADS), heads(na_k, NA_HEADS), heads(na_v, NA_HEADS),
                                   kc_na, vc_na, rpb).reshape(B, S, -1)
    o_cv = conformer_conv(glu, conv_w, conv_b, ln_g, ln_b)
    o_wa = window_attention(apply_axial_rope(heads(wa_q, WA_HEADS), cos, sin),
                            apply_axial_rope(heads(wa_k, WA_KV_HEADS), cos, sin),
                            heads(wa_v, WA_KV_HEADS), kc_wa, vc_wa, sink).reshape(B, S, -1)
    y = merge_branches((o_na, o_cv, o_wa), gates, w_branch, w_out)
    if not with_ctx_out:
        return y, None
    co_na = context_attention(heads(cna_q, NA_HEADS), kc_na, vc_na, None).reshape(B, Lc, -1)
    co_cv = conformer_conv(cglu, conv_w, conv_b, ln_g, ln_b)
    co_wa = context_attention(heads(cwa_q, WA_HEADS), kc_wa, vc_wa, sink).reshape(B, Lc, -1)
    yc = merge_branches((co_na, co_cv, co_wa), cgates, w_branch, w_out)
    return y, yc


def swiglu(h, wi, wo):
    a, b = jnp.split(h @ wi, 2, axis=-1)
    return (jax.nn.silu(a) * b) @ wo


def moe_swiglu(h, w_router, wi, wo):
    logits = (h @ w_router).astype(jnp.float32)
    top_v, top_i = lax.top_k(logits, TOP_K)
    wts = jax.nn.softmax(top_v, axis=-1)
    gate = jnp.sum(jax.nn.one_hot(top_i, N_EXPERTS, dtype=jnp.float32) * wts[..., None], axis=-2)
    gate = gate.astype(h.dtype)
    out = gate[..., 0:1] * swiglu(h, wi[0], wo[0])
    for e in range(1, N_EXPERTS):
        out = out + gate[..., e:e + 1] * swiglu(h, wi[e], wo[e])
    return out


def setup_inputs(seed: int = 0) -> dict:
    key = jax.random.key(seed)
    ks = jax.random.split(key, 24)
    L, D = DEPTH, D_MODEL
    n_dense = (DEPTH + 1) // 2
    n_moe = DEPTH // 2

    def nrm(k, shape, s):
        return jax.random.normal(k, shape, jnp.float32) * s

    return {
        'x': nrm(ks[0], (BATCH, SEQ, D), 1.0),
        'c': nrm(ks[1], (BATCH, D), 1.0),
        'ctx': nrm(ks[2], (BATCH, CTX_LEN, D), 1.0),
        'c_ctx': nrm(ks[3], (D,), 1.0),
        'w_mod': nrm(ks[4], (L, D, 6 * D), 0.5 * D ** -0.5),
        'b_mod': nrm(ks[5], (L, 6 * D), 0.01),
        'g_norm1': 1.0 + nrm(ks[6], (L, D), 0.01),
        'g_norm2': 1.0 + nrm(ks[7], (L, D), 0.01),
        'w_in': nrm(ks[8], (L, D, IN_DIM), D ** -0.5),
        'rpb': nrm(ks[9], (L, NA_HEADS, 2 * NA_WIN_ROWS - 1, 2 * NA_WIN_COLS - 1), 0.1),
        'conv_w': nrm(ks[10], (L, CONV_WIDTH, CONV_DIM), CONV_WIDTH ** -0.5),
        'conv_b': nrm(ks[11], (L, CONV_DIM), 0.01),
        'ln_g': 1.0 + nrm(ks[12], (L, CONV_DIM), 0.01),
        'ln_b': nrm(ks[13], (L, CONV_DIM), 0.01),
        'sink': nrm(ks[14], (L, WA_HEADS), 0.5),
        'w_branch': nrm(ks[15], (L, N_BRANCH, BRANCH_DIM, D), BRANCH_DIM ** -0.5),
        'w_out': nrm(ks[16], (L, D, D), D ** -0.5),
        'ffn_wi': nrm(ks[17], (n_dense, D, 2 * D_FF), D ** -0.5),
        'ffn_wo': nrm(ks[18], (n_dense, D_FF, D), D_FF ** -0.5),
        'moe_router': nrm(ks[19], (n_moe, D, N_EXPERTS), D ** -0.5),
        'moe_wi': nrm(ks[20], (n_moe, N_EXPERTS, D, 2 * D_FF_EXPERT), D ** -0.5),
        'moe_wo': nrm(ks[21], (n_moe, N_EXPERTS, D_FF_EXPERT, D), D_FF_EXPERT ** -0.5),
        'g_final': 1.0 + nrm(ks[22], (D,), 0.01),
    }


def reference(x, c, ctx, c_ctx, w_mod, b_mod, g_norm1, g_norm2, w_in, rpb, conv_w, conv_b, ln_g, ln_b,
              sink, w_branch, w_out, ffn_wi, ffn_wo, moe_router, moe_wi, moe_wo, g_final):
    cos, sin = axial_rope_tables(x.shape[1])
    sc = jax.nn.silu(c)
    scc = jax.nn.silu(c_ctx)
    for l in range(DEPTH):
        keep_ctx = l < DEPTH - 1
        m = jnp.split(sc @ w_mod[l] + b_mod[l], 6, axis=-1)
        mc = jnp.split(scc @ w_mod[l] + b_mod[l], 6, axis=-1)
        h = modulate(rms_norm(x, g_norm1[l]), m[0][:, None], m[1][:, None])
        hc = modulate(rms_norm(ctx, g_norm1[l]), mc[0], mc[1])
        y, yc = token_mixer(h, hc, w_in[l], rpb[l], conv_w[l], conv_b[l], ln_g[l], ln_b[l], sink[l],
                            w_branch[l], w_out[l], cos, sin, keep_ctx)
        x = x + m[2][:, None] * y
        if keep_ctx:
            ctx = ctx + mc[2] * yc
        h = modulate(rms_norm(x, g_norm2[l]), m[3][:, None], m[4][:, None])
        if l % 2 == 0:
            x = x + m[5][:, None] * swiglu(h, ffn_wi[l // 2], ffn_wo[l // 2])
        else:
            x = x + m[5][:, None] * moe_swiglu(h, moe_router[l // 2], moe_wi[l // 2], moe_wo[l // 2])
        if keep_ctx:
            hc = modulate(rms_norm(ctx, g_norm2[l]), mc[3], mc[4])
            if l % 2 == 0:
                ctx = ctx + mc[5] * swiglu(hc, ffn_wi[l // 2], ffn_wo[l // 2])
            else:
                ctx = ctx + mc[5] * moe_swiglu(hc, moe_router[l // 2], moe_wi[l // 2], moe_wo[l // 2])
    return rms_norm(x, g_final)
```

```python
import numpy as np
from contextlib import ExitStack
import ml_dtypes
import concourse.bass as bass
import concourse.mybir as mybir
from concourse.bass_utils import run_bass_kernel_spmd

F32 = mybir.dt.float32
BF16 = mybir.dt.bfloat16
AF = mybir.ActivationFunctionType
ALU = mybir.AluOpType
AX = mybir.AxisListType

D = 2048
TL = 2048
CL = 256
TT = TL + CL
GW = 64
HD = 128
IN_DIM = 12800
D_FF = 5632
NE = 8
D_FFE = 4096
EPS = 1e-6
NEG = -1e30
SCALE = HD ** -0.5
TILES = [(0, 512), (512, 512), (1024, 512), (1536, 512), (2048, 256)]
NA_HALO = 7 * GW
WA_HALO = 128
CV_HALO = 15

SEM_ROT = 30000
N_DMA_SEMS = 32


class Ctx:
    def __init__(self, nc, es):
        self.nc = nc
        self.es = es
        self.eng = {'pe': nc.tensor, 'act': nc.scalar, 'dve': nc.vector, 'pool': nc.gpsimd, 'sp': nc.sync}
        self.sems = {}
        self.cur = {}
        self.gen = {e: 0 for e in self.eng}
        for e in self.eng:
            self._new_sem(e)
        self.dma_sems = []
        for j in range(N_DMA_SEMS):
            k = ('dma', j)
            self.sems[k] = es.enter_context(nc.semaphore(f"dma{j}"))
            self.dma_sems.append([k, 0])
        self.dma_rr = 0
        self.waited = {e: {} for e in self.eng}
        self.lastw = {}
        self.readers = {}
        self.n_inst = 0
        self.n_wait = 0
        self.bank_rr = 0
        self.uid = 0

    def _new_sem(self, e):
        k = (e, self.gen[e])
        self.gen[e] += 1
        self.sems[k] = self.es.enter_context(self.nc.semaphore(f"s_{e}_{k[1]}"))
        self.cur[e] = [k, 0]

    def _wait(self, e, tok):
        if tok is None:
            return
        k, v = tok
        w = self.waited[e]
        if w.get(k, 0) >= v:
            return
        self.eng[e].wait_ge(self.sems[k], v)
        w[k] = v
        self.n_wait += 1

    def _deps(self, e, reads, writes, acc=False):
        for r in reads:
            self._wait(e, self.lastw.get(r))
        for wk in writes:
            lw = self.lastw.get(wk)
            if not (acc and lw is not None and lw[0][0] == 'pe'):
                self._wait(e, lw)
            for t in self.readers.get(wk, ()):
                self._wait(e, t)

    def _commit(self, tok, reads, writes):
        for r in reads:
            lst = self.readers.setdefault(r, [])
            lst.append(tok)
            if len(lst) > 12:
                d = {}
                for k, v in lst:
                    d[k] = max(d.get(k, 0), v)
                self.readers[r] = list(d.items())
        for wk in writes:
            self.lastw[wk] = tok
            self.readers[wk] = []

    def _bump(self, e, ins):
        c = self.cur[e]
        c[1] += 1
        ins.then_inc(self.sems[c[0]], 1)
        tok = (c[0], c[1])
        if c[1] >= SEM_ROT:
            self._new_sem(e)
        return tok

    def op(self, e, fn, reads=(), writes=()):
        self._deps(e, reads, writes)
        ins = fn()
        tok = self._bump(e, ins)
        self._commit(tok, reads, writes)
        self.n_inst += 1
        return tok

    def mm(self, fns, reads=(), writes=(), acc=False):
        self._deps('pe', reads, writes, acc)
        ins = None
        for fn in fns:
            ins = fn()
            self.n_inst += 1
        tok = self._bump('pe', ins)
        self._commit(tok, reads, writes)
        return tok

    def dma(self, q, out, in_, reads=(), writes=(), **kw):
        self._deps(q, reads, writes)
        s = self.dma_sems[self.dma_rr]
        self.dma_rr = (self.dma_rr + 1) % len(self.dma_sems)
        k = s[0]
        if s[1] > 0:
            self._wait(q, (k, 16 * s[1]))
        ins = self.eng[q].dma_start(out=out, in_=in_, **kw)
        s[1] += 1
        ins.then_inc(self.sems[k], 16)
        tok = (k, 16 * s[1])
        self._commit(tok, reads, writes)
        self.n_inst += 1
        return tok

    def finish(self, e='sp'):
        for s in self.dma_sems:
            if s[1] > 0:
                self._wait(e, (s[0], 16 * s[1]))
        for en in self.eng:
            for g in range(self.gen[en]):
                k = (en, g)
                v = self.cur[en][1] if self.cur[en][0] == k else SEM_ROT
                if v > 0:
                    self._wait(e, (k, v))

    def bank(self, lo=0, hi=8):
        b = lo + (self.bank_rr % (hi - lo))
        self.bank_rr += 1
        return b

    def key(self, base):
        self.uid += 1
        return f"{base}_{self.uid}"


class Pool:
    def __init__(self, nc, es, name, shape, dtype, n):
        self.bufs = [es.enter_context(nc.sbuf_tensor(f"{name}{i}", shape, dtype)) for i in range(n)]
        self.keys = [f"{name}{i}" for i in range(n)]
        self.i = 0

    def get(self):
        j = self.i % len(self.bufs)
        self.i += 1
        return self.bufs[j], self.keys[j]


class Prog:
    def __init__(self, es):
        self.nc = bass.Bass("TRN2", target_bir_lowering=False)
        self.es = es
        self.cx = Ctx(self.nc, es)
        nc = self.nc
        self.ps = es.enter_context(nc.psum_tensor("ps", [128, 8, 512], F32))
        self.wpool = Pool(nc, es, "wb", [128, 8192], BF16, 3)
        self.ones_f = es.enter_context(nc.sbuf_tensor("ones_f", [128, 128], F32))
        self.ones_b = es.enter_context(nc.sbuf_tensor("ones_b", [128, 128], BF16))
        self.cx.op('dve', lambda: nc.vector.memset(self.ones_f[:], 1.0), writes=['ones_f'])
        self.cx.op('dve', lambda: nc.vector.memset(self.ones_b[:], 1.0), writes=['ones_b'])
        self.wq = 0

    def dram(self, name, shape, dt, kind):
        return self.nc.dram_tensor(name, list(shape), dt, kind=kind).ap()

    def load_w(self, W, r0, nk, c0, ncols, extra_reads=()):
        assert nk * ncols <= 8192
        buf, key = self.wpool.get()
        view = buf[:, 0:nk * ncols].rearrange("p (k c) -> p k c", k=nk)
        src = W[r0:r0 + nk * 128, c0:c0 + ncols].rearrange("(k p) c -> p k c", p=128)
        self.cx.dma('pool', view, src, reads=list(extra_reads), writes=[key])
        return view, key


def linear_fm(P, act, act_key, nk, W, r0, c0, ncols, tiles, evac, group=512):
    cx, nc = P.cx, P.nc
    g = min(group, 8192 // nk // 128 * 128)
    for gc in range(0, ncols, g):
        gn = min(g, ncols - gc)
        wt, wkey = P.load_w(W, r0, nk, c0 + gc, gn)
        for m in range(gn // 128):
            for (t0, tn) in tiles:
                b = cx.bank()
                pk = f"ps{b}"
                fns = [(lambda k=k: nc.tensor.matmul(P.ps[:, b, :tn], wt[:, k, m * 128:(m + 1) * 128],
                                                     act[:, k, t0:t0 + tn], start=(k == 0), stop=(k == nk - 1)))
                       for k in range(nk)]
                cx.mm(fns, reads=[act_key, wkey], writes=[pk])
                evac((gc // 128) + m, (t0, tn), P.ps[:, b, :tn], pk)


def linear_tm(P, act, act_key, nk, W, r0, c0, ncols, ntok, evac):
    cx, nc = P.cx, P.nc
    assert ncols <= 512
    wt, wkey = P.load_w(W, r0, nk, c0, ncols)
    for tt in range(ntok // 128):
        b = cx.bank()
        pk = f"ps{b}"
        fns = [(lambda k=k: nc.tensor.matmul(P.ps[:, b, :ncols], act[:, k, tt * 128:(tt + 1) * 128],
                                             wt[:, k, :], start=(k == 0), stop=(k == nk - 1)))
               for k in range(nk)]
        cx.mm(fns, reads=[act_key, wkey], writes=[pk])
        evac(tt, P.ps[:, b, :ncols], pk)


def alt_copy(P, i, out, in_, reads, writes, scale=None):
    cx, nc = P.cx, P.nc
    if i % 2 == 0:
        cx.op('act', lambda: nc.scalar.activation(out, in_, AF.Copy), reads=reads, writes=writes)
    else:
        cx.op('dve', lambda: nc.vector.tensor_copy(out, in_), reads=reads, writes=writes)


DEPTH = 4
NQR = [56, 48, 40, 32]
NKVR = [64, 56, 48, 40]
WROWS = 64
XW = WROWS * GW
XC = XW + CL
NAM = 3 * GW
NKT = 11
NIDX = 28


def barrier(cx):
    for e in cx.eng:
        cx.finish(e)


def kv_tiles(l):
    nq = NQR[l] * GW
    t = [(0, 256, 'm')] + [(256 + 512 * i, 512, 'o') for i in range(nq // 512)] + [(256 + nq, 256, 'm')]
    return t


def o_tiles(l):
    nq = NQR[l] * GW
    q0 = 256 * (l + 1)
    return [(q0 + 512 * i, 512 * i, 512, False) for i in range(nq // 512)] + [(XW, nq, CL, True)]


def rms_norm_tile(P, pools, xsrc, xcol, n, c, A, Sh, dst_fn, hf_cb=None):
    cx, nc = P.cx, P.nc
    xp, sqp, rp = pools
    b = cx.bank()
    pk = f"ps{b}"
    for k in range(16):
        xt, xk = xp.get()
        cx.dma('sp', xt[:, :n], xsrc[k * 128:(k + 1) * 128, xcol:xcol + n], writes=[xk])
        sq, sk = sqp.get()
        cx.op('act', lambda: nc.scalar.activation(sq[:, :n], xt[:, :n], AF.Square), reads=[xk], writes=[sk])
        cx.mm([lambda: nc.tensor.matmul(P.ps[:, b, :n], P.ones_f[:], sq[:, :n], start=(k == 0), stop=(k == 15))],
              reads=[sk, 'ones_f'], writes=[pk], acc=(k > 0))
    rt, rk = rp.get()
    cx.op('dve', lambda: nc.vector.tensor_scalar(rt[:, :n], P.ps[:, b, :n], 1.0 / D, EPS, ALU.mult, ALU.add), reads=[pk], writes=[rk])
    cx.op('act', lambda: nc.scalar.activation(rt[:, :n], rt[:, :n], AF.Sqrt), reads=[rk], writes=[rk])
    cx.op('dve', lambda: nc.vector.reciprocal(rt[:, :n], rt[:, :n]), reads=[rk], writes=[rk])
    for k in range(16):
        xt, xk = xp.get()
        cx.dma('sp', xt[:, :n], xsrc[k * 128:(k + 1) * 128, xcol:xcol + n], writes=[xk])
        sq, sk = sqp.get()
        cx.op('dve', lambda: nc.vector.scalar_tensor_tensor(sq[:, :n], xt[:, :n], A[:, k, c:c + 1], rt[:, :n], ALU.mult, ALU.mult),
              reads=[xk, rk, 'modA'], writes=[sk])
        dst, dkey = dst_fn(k)
        if hf_cb is None:
            cx.op('act', lambda: nc.scalar.activation(dst, sq[:, :n], AF.Identity, bias=Sh[:, k, c:c + 1]),
                  reads=[sk, 'modA'], writes=[dkey])
        else:
            cx.op('act', lambda: nc.scalar.activation(sq[:, :n], sq[:, :n], AF.Identity, bias=Sh[:, k, c:c + 1]),
                  reads=[sk, 'modA'], writes=[sk])
            cx.op('dve', lambda: nc.vector.tensor_copy(dst, sq[:, :n]), reads=[sk], writes=[dkey])
            hf_cb(k, n, sq, sk)


def norm_pools(P, pes):
    nc, cx = P.nc, P.cx
    return (Pool(nc, pes, cx.key("rx"), [128, 512], F32, 4), Pool(nc, pes, cx.key("rsq"), [128, 512], F32, 4),
            Pool(nc, pes, cx.key("rr"), [128, 512], F32, 2))


def phase_modvec(P, cT_d, wmod_d, bmodT_d, mod, pes):
    cx, nc = P.cx, P.nc
    cf = pes.enter_context(nc.sbuf_tensor(cx.key("cf"), [128, 16, 2], F32))
    cb = pes.enter_context(nc.sbuf_tensor(cx.key("cb"), [128, 16, 2], BF16))
    bm = pes.enter_context(nc.sbuf_tensor(cx.key("bm"), [128, 96], F32))
    cx.dma('sp', cf[:], cT_d, writes=['cf'])
    cx.dma('sp', bm[:], bmodT_d, writes=['bm'])
    cx.op('act', lambda: nc.scalar.activation(cb[:], cf[:], AF.Silu), reads=['cf'], writes=['cb'])
    b = cx.bank()
    pk = f"ps{b}"
    for g in range(24):
        wt, wkey = P.load_w(wmod_d, 0, 16, g * 512, 512)
        for m in range(4):
            f = g * 4 + m
            fns = [(lambda k=k: nc.tensor.matmul(P.ps[:, b, 2 * f:2 * f + 2], wt[:, k, m * 128:(m + 1) * 128], cb[:, k, :],
                                                 start=(k == 0), stop=(k == 15))) for k in range(16)]
            cx.mm(fns, reads=['cb', wkey], writes=[pk], acc=(f > 0))
    pv = P.ps[:, b, 0:192].rearrange("p (f c) -> p f c", c=2)
    for c in range(2):
        cx.op('dve', lambda: nc.vector.tensor_tensor(mod[:, :, c], pv[:, :, c], bm[:, :], ALU.add), reads=[pk, 'bm'], writes=['mod'])


def make_A(P, mod, gT, A, sc0, gcol):
    cx, nc = P.cx, P.nc
    for c in range(2):
        cx.op('dve', lambda: nc.vector.scalar_tensor_tensor(A[:, :, c], mod[:, sc0:sc0 + 16, c], 1.0, gT[:, gcol:gcol + 16],
                                                            ALU.add, ALU.mult), reads=['mod', 'gT'], writes=['modA'])


def phase_norm1(P, pes, l, XB, HT, A, Sh):
    cx, nc = P.cx, P.nc
    pools = norm_pools(P, pes)
    stg = Pool(nc, pes, cx.key("n1s"), [128, 16, 512], BF16, 2)
    nkv = NKVR[l] * GW
    tl = [(256 * l + c0, c0, n, 0) for (c0, n, _) in kv_tiles(l)] + [(XW, nkv, CL, 1)]
    for (xcol, hcol, n, c) in tl:
        st, sk = stg.get()
        rms_norm_tile(P, pools, XB, xcol, n, c, A, Sh, lambda k: (st[:, k, :n], sk))
        cx.dma('sp', HT[:, hcol:hcol + n].rearrange("(k p) t -> p k t", p=128), st[:, :, :n], reads=[sk])


def phase_inproj(P, pes, l, HT, win, lw, cos_d, sin_d, tval_d, B):
    cx, nc = P.cx, P.nc
    nq, nkv = NQR[l] * GW, NKVR[l] * GW
    nak_ctx = nkv + 2 * NAM
    hT = pes.enter_context(nc.sbuf_tensor(cx.key("hT"), [128, 16, 2048], BF16))
    sbp = Pool(nc, pes, cx.key("stb"), [128, 512], BF16, 4)
    sfp = Pool(nc, pes, cx.key("stf"), [128, 512], F32, 3)
    tmpf = Pool(nc, pes, cx.key("tmf"), [128, 512], F32, 4)
    tabp = Pool(nc, pes, cx.key("tab"), [128, 512], F32, 4)
    wperm = pes.enter_context(nc.sbuf_tensor(cx.key("wperm"), [128, 8192], BF16))
    tiles = [(c0, n, kind) for (c0, n, kind) in kv_tiles(l)] + [(nkv, CL, 'c')]
    chunks, cur, tot = [], [], 0
    for t in tiles:
        if tot + t[1] > 2048:
            chunks.append(cur)
            cur, tot = [], 0
        cur.append((tot,) + t)
        tot += t[1]
    chunks.append(cur)
    cnt = [0]
    for ch in chunks:
        ctot = sum(t[2] for t in ch)
        h0 = ch[0][1]
        cx.dma('sp', hT[:, :, :ctot], HT[:, h0:h0 + ctot].rearrange("(k p) t -> p k t", p=128), writes=['hT'])
        all_t = ch
        oc_t = [t for t in ch if t[3] in ('o', 'c')]

        def mm16(b, wt, m, hc, n):
            fns = [(lambda k=k: nc.tensor.matmul(P.ps[:, b, :n], wt[:, k, m * 128:(m + 1) * 128], hT[:, k, hc:hc + n],
                                                 start=(k == 0), stop=(k == 15))) for k in range(16)]
            return fns

        def plain(dst, c0, ncols, tl, colfn, sig=False, f32=False):
            if not tl:
                return
            for gc in range(0, ncols, 512):
                gn = min(512, ncols - gc)
                wt, wkey = P.load_w(win, lw, 16, c0 + gc, gn)
                for m in range(gn // 128):
                    mi = gc // 128 + m
                    for (hc, kvc, n, kind) in tl:
                        b = cx.bank()
                        cx.mm(mm16(b, wt, m, hc, n), reads=['hT', wkey], writes=[f"ps{b}"])
                        buf, key = (sfp if f32 else sbp).get()
                        cnt[0] += 1
                        if sig:
                            cx.op('act', lambda: nc.scalar.activation(buf[:, :n], P.ps[:, b, :n], AF.Sigmoid), reads=[f"ps{b}"], writes=[key])
                        else:
                            alt_copy(P, cnt[0], buf[:, :n], P.ps[:, b, :n], [f"ps{b}"], [key])
                        dc = colfn(kvc, kind)
                        cx.dma('sp', dst[mi * 128:(mi + 1) * 128, dc:dc + n], buf[:, :n], reads=[key])

        def vsec(dst, c0, ncols, rowfn):
            for gc in range(0, ncols, 512):
                gn = min(512, ncols - gc)
                wt, wkey = P.load_w(win, lw, 16, c0 + gc, gn)
                for (hc, kvc, n, kind) in all_t:
                    r0 = rowfn(kvc, kind)
                    for tt in range(n // 128):
                        b = cx.bank()
                        fns = [(lambda k=k: nc.tensor.matmul(P.ps[:, b, :gn], hT[:, k, hc + tt * 128:hc + (tt + 1) * 128], wt[:, k, :],
                                                             start=(k == 0), stop=(k == 15))) for k in range(16)]
                        cx.mm(fns, reads=['hT', wkey], writes=[f"ps{b}"])
                        buf, key = sbp.get()
                        cnt[0] += 1
                        alt_copy(P, cnt[0], buf[:, :gn], P.ps[:, b, :gn], [f"ps{b}"], [key])
                        cx.dma('sp', dst[r0 + tt * 128:r0 + (tt + 1) * 128, gc:gc + gn], buf[:, :gn], reads=[key])

        def rope(dst, c0, ncols, tl, colfn):
            if not tl:
                return
            for gc in range(0, ncols, 512):
                gn = min(512, ncols - gc)
                wt, wkey = P.load_w(win, lw, 16, c0 + gc, gn)
                wv = wt.rearrange("p k (h j f) -> p k h j f", j=2, f=32)
                pv = wperm[:, 0:16 * gn].rearrange("p (k c) -> p k c", k=16)
                pvv = pv.rearrange("p k (h j f) -> p k h j f", j=2, f=32)
                for j in range(2):
                    cx.op('pool', lambda: nc.gpsimd.tensor_copy(pvv[:, :, :, j, :], wv[:, :, :, 1 - j, :]), reads=[wkey], writes=['wperm'])
                for (hc, kvc, n, kind) in tl:
                    if kind != 'c':
                        ct, ck = tabp.get()
                        st_, stk = tabp.get()
                        wc = 256 * l + kvc
                        cx.dma('sp', ct[:, :n], cos_d[:, wc:wc + n], writes=[ck])
                        cx.dma('sp', st_[:, :n], sin_d[:, wc:wc + n], writes=[stk])
                    for m in range(gn // 128):
                        mi = gc // 128 + m
                        buf, key = sbp.get()
                        b1 = cx.bank()
                        cx.mm(mm16(b1, wt, m, hc, n), reads=['hT', wkey], writes=[f"ps{b1}"])
                        if kind != 'c':
                            b2 = cx.bank()
                            cx.mm(mm16(b2, pv, m, hc, n), reads=['hT', 'wperm'], writes=[f"ps{b2}"])
                            t1, k1 = tmpf.get()
                            t2, k2 = tmpf.get()
                            cx.op('dve', lambda: nc.vector.tensor_tensor(t1[:, :n], P.ps[:, b1, :n], ct[:, :n], ALU.mult),
                                  reads=[f"ps{b1}", ck], writes=[k1])
                            cx.op('dve', lambda: nc.vector.tensor_tensor(t2[:, :n], P.ps[:, b2, :n], st_[:, :n], ALU.mult),
                                  reads=[f"ps{b2}", stk], writes=[k2])
                            cx.op('pool', lambda: nc.gpsimd.tensor_tensor(buf[:, :n], t1[:, :n], t2[:, :n], ALU.add),
                                  reads=[k1, k2], writes=[key])
                        else:
                            cx.op('act', lambda: nc.scalar.activation(buf[:, :n], P.ps[:, b1, :n], AF.Copy), reads=[f"ps{b1}"], writes=[key])
                        dc = colfn(kvc, kind)
                        cx.dma('sp', dst[mi * 128:(mi + 1) * 128, dc:dc + n], buf[:, :n], reads=[key])

        def glu():
            for gc in range(0, 1024, 512):
                wa, ka = P.load_w(win, lw, 16, 4608 + gc, 512)
                wg, kg = P.load_w(win, lw, 16, 5632 + gc, 512)
                for (hc, kvc, n, kind) in all_t:
                    if kind != 'c':
                        tv, tvk = tabp.get()
                        wc = 256 * l + kvc
                        cx.dma('sp', tv[:, :n], tval_d[:, wc:wc + n], writes=[tvk])
                    for m in range(4):
                        mi = gc // 128 + m
                        b1, b2 = cx.bank(), cx.bank()
                        cx.mm(mm16(b1, wa, m, hc, n), reads=['hT', ka], writes=[f"ps{b1}"])
                        cx.mm(mm16(b2, wg, m, hc, n), reads=['hT', kg], writes=[f"ps{b2}"])
                        t1, k1 = tmpf.get()
                        buf, key = sfp.get()
                        cx.op('act', lambda: nc.scalar.activation(t1[:, :n], P.ps[:, b2, :n], AF.Sigmoid), reads=[f"ps{b2}"], writes=[k1])
                        if kind != 'c':
                            cx.op('dve', lambda: nc.vector.tensor_tensor(t1[:, :n], P.ps[:, b1, :n], t1[:, :n], ALU.mult),
                                  reads=[f"ps{b1}", k1], writes=[k1])
                            cx.op('pool', lambda: nc.gpsimd.tensor_tensor(buf[:, :n], t1[:, :n], tv[:, :n], ALU.mult),
                                  reads=[k1, tvk], writes=[key])
                        else:
                            cx.op('dve', lambda: nc.vector.tensor_tensor(buf[:, :n], P.ps[:, b1, :n], t1[:, :n], ALU.mult),
                                  reads=[f"ps{b1}", k1], writes=[key])
                        dc = kvc
                        cx.dma('sp', B['ZT'][mi * 128:(mi + 1) * 128, dc:dc + n], buf[:, :n], reads=[key])

        qcol = lambda kvc, kind: (nq if kind == 'c' else kvc - 256)
        plain(B['NAQ'], 0, 1024, oc_t, qcol)
        plain(B['NAK'], 1024, 1024, all_t, lambda kvc, kind: (nak_ctx if kind == 'c' else NAM + kvc))
        vsec(B['NAV'], 2048, 1024, lambda kvc, kind: (nak_ctx if kind == 'c' else NAM + kvc))
        rope(B['WAQ'], 3072, 1024, oc_t, qcol)
        rope(B['WAK'], 4096, 256, all_t, lambda kvc, kind: kvc)
        vsec(B['WAV'], 4352, 256, lambda kvc, kind: kvc)
        glu()
        plain(B['GST'], 6656, 6144, oc_t, qcol, sig=True, f32=True)


def phase_na(P, pes, l, B, ttab_h, maskL_d, rsel_d):
    cx, nc = P.cx, P.nc
    nq, nkv = NQR[l] * GW, NKVR[l] * GW
    kw = nkv + 2 * NAM
    nvt = kw // 128
    nqb = NQR[l] // 8
    kp = Pool(nc, pes, cx.key("nak"), [128, kw + CL], BF16, 2)
    qp = Pool(nc, pes, cx.key("naq"), [128, nq + CL], BF16, 2)
    vp = Pool(nc, pes, cx.key("nav"), [128, nvt + 2, 128], BF16, 2)
    tp = Pool(nc, pes, cx.key("ntt"), [128, NIDX * 64], F32, 2)
    op_ = Pool(nc, pes, cx.key("nao"), [128, nq + CL], BF16, 2)
    sp = Pool(nc, pes, cx.key("nas"), [128, 512], F32, 3)
    pp = Pool(nc, pes, cx.key("nap"), [128, 512], BF16, 4)
    rp = Pool(nc, pes, cx.key("nar"), [128, 512], F32, 2)
    mL = pes.enter_context(nc.sbuf_tensor(cx.key("namL"), [8, nqb * NKT * 128], BF16))
    rs = pes.enter_context(nc.sbuf_tensor(cx.key("nars"), [8, 512], BF16))
    cx.dma('sp', mL[:], maskL_d, writes=['namL'])
    cx.dma('sp', rs[:], rsel_d, writes=['nars'])
    PO, PD = 6, 7

    def attend(q_ap, n, tiles, kT, kk, V, vk, Tt, tk, qkey, obuf, okey, ocol):
        first = True
        for ti, tl in enumerate(tiles):
            last = ti == len(tiles) - 1
            b = cx.bank(0, 6)
            pk = f"ps{b}"
            pT, pkey = pp.get()
            if tl[0] == 'ctx':
                i = tl[1]
                kcol = kw + i * 128
                vt = nvt + i
                cx.mm([lambda: nc.tensor.matmul(P.ps[:, b, :n], kT[:, kcol:kcol + 128], q_ap, start=True, stop=True)],
                      reads=[kk, qkey], writes=[pk])
                cx.op('act', lambda: nc.scalar.activation(pT[:, :n], P.ps[:, b, :n], AF.Exp, scale=SCALE), reads=[pk], writes=[pkey])
            else:
                t, qb = tl[1], tl[2]
                vt = 4 * qb + t
                kcol = vt * 128
                mcol = (qb * NKT + t) * 128
                cx.mm([lambda: nc.tensor.matmul(P.ps[:, b, :n], kT[:, kcol:kcol + 128], q_ap, start=True, stop=False),
                       lambda: nc.tensor.matmul(P.ps[:, b, :n], mL[:, mcol:mcol + 128], rs[:, :n], start=False, stop=True)],
                      reads=[kk, qkey, 'namL', 'nars'], writes=[pk])
                sT, sk = sp.get()
                i0 = (20 - 2 * t) * 64
                cx.op('dve', lambda: nc.vector.scalar_tensor_tensor(sT[:, :n], P.ps[:, b, :n], SCALE, Tt[:, i0:i0 + 512],
                                                                    ALU.mult, ALU.add), reads=[pk, tk], writes=[sk])
                cx.op('act', lambda: nc.scalar.activation(pT[:, :n], sT[:, :n], AF.Exp), reads=[sk], writes=[pkey])
            cx.mm([lambda: nc.tensor.matmul(P.ps[:, PO, :n], V[:, vt, :], pT[:, :n], start=first, stop=last)],
                  reads=[vk, pkey], writes=[f"ps{PO}"], acc=not first)
            cx.mm([lambda: nc.tensor.matmul(P.ps[:, PD, :n], P.ones_b[:], pT[:, :n], start=first, stop=last)],
                  reads=['ones_b', pkey], writes=[f"ps{PD}"], acc=not first)
            first = False
        rd, rk = rp.get()
        cx.op('dve', lambda: nc.vector.reciprocal(rd[:, :n], P.ps[:, PD, :n]), reads=[f"ps{PD}"], writes=[rk])
        cx.op('dve', lambda: nc.vector.tensor_tensor(obuf[:, ocol:ocol + n], P.ps[:, PO, :n], rd[:, :n], ALU.mult),
              reads=[f"ps{PO}", rk], writes=[okey])

    for h in range(8):
        kT, kk = kp.get()
        qT, qk = qp.get()
        V, vk = vp.get()
        Tt, tk = tp.get()
        ob, ok = op_.get()
        cx.dma('sp', kT[:], B['NAK'][h * 128:(h + 1) * 128, 0:kw + CL], writes=[kk])
        cx.dma('sp', qT[:], B['NAQ'][h * 128:(h + 1) * 128, 0:nq + CL], writes=[qk])
        cx.dma('sp', V[:], B['NAV'][0:kw + CL, h * 128:(h + 1) * 128].rearrange("(t p) d -> p t d", p=128), writes=[vk])
        cx.dma('sp', Tt[:], ttab_h(h), writes=[tk])
        for qb in range(nqb):
            tiles = [('ctx', 0), ('ctx', 1)] + [('lat', t, qb) for t in range(NKT)]
            attend(qT[:, qb * 512:(qb + 1) * 512], 512, tiles, kT, kk, V, vk, Tt, tk, qk, ob, ok, qb * 512)
        attend(qT[:, nq:nq + CL], CL, [('ctx', 0), ('ctx', 1)], kT, kk, V, vk, Tt, tk, qk, ob, ok, nq)
        cx.dma('sp', B['ONA'][h * 128:(h + 1) * 128, 0:nq + CL], ob[:], reads=[ok])


def phase_wa(P, pes, l, B, m3_d, kvb_d, sinkb_d):
    cx, nc = P.cx, P.nc
    nq, nkv = NQR[l] * GW, NKVR[l] * GW
    nkt = nkv // 128
    kp = Pool(nc, pes, cx.key("wak"), [128, nkv + CL], BF16, 2)
    qp = Pool(nc, pes, cx.key("waq"), [128, nq + CL], BF16, 2)
    vp = Pool(nc, pes, cx.key("wav"), [128, nkt + 2, 128], BF16, 2)
    op_ = Pool(nc, pes, cx.key("wao"), [128, nq + CL], BF16, 2)
    sp = Pool(nc, pes, cx.key("was"), [128, 384], F32, 3)
    pp = Pool(nc, pes, cx.key("wap"), [128, 512], BF16, 4)
    rp = Pool(nc, pes, cx.key("war"), [128, 512], F32, 2)
    m3 = pes.enter_context(nc.sbuf_tensor(cx.key("wam3"), [128, 384], F32))
    kvb = pes.enter_context(nc.sbuf_tensor(cx.key("wakvb"), [128, nkt], F32))
    esink = pes.enter_context(nc.sbuf_tensor(cx.key("waes"), [128, 8], F32))
    cx.dma('sp', m3[:], m3_d, writes=['wam3'])
    cx.dma('sp', kvb[:], kvb_d, writes=['waed'])
    cx.dma('sp', esink[:], sinkb_d, writes=['waes'])
    cx.op('act', lambda: nc.scalar.activation(esink[:], esink[:], AF.Exp), reads=['waes'], writes=['waes'])
    PO, PD = 6, 7

    def finalize(n, h, obuf, okey, ocol):
        rd, rk = rp.get()
        cx.op('dve', lambda: nc.vector.tensor_scalar(rd[:, :n], P.ps[:, PD, :n], esink[:, h:h + 1], None, ALU.add),
              reads=[f"ps{PD}", 'waes'], writes=[rk])
        cx.op('dve', lambda: nc.vector.reciprocal(rd[:, :n], rd[:, :n]), reads=[rk], writes=[rk])
        cx.op('dve', lambda: nc.vector.tensor_tensor(obuf[:, ocol:ocol + n], P.ps[:, PO, :n], rd[:, :n], ALU.mult),
              reads=[f"ps{PO}", rk], writes=[okey])

    def ctx_tiles(q_ap, n, kT, kk, V, vk, qk, last_i):
        for i in range(2):
            b = cx.bank(0, 6)
            pk = f"ps{b}"
            pT, pkey = pp.get()
            kcol = nkv + i * 128
            cx.mm([lambda: nc.tensor.matmul(P.ps[:, b, :n], kT[:, kcol:kcol + 128], q_ap, start=True, stop=True)],
                  reads=[kk, qk], writes=[pk])
            cx.op('act', lambda: nc.scalar.activation(pT[:, :n], P.ps[:, b, :n], AF.Exp, scale=SCALE), reads=[pk], writes=[pkey])
            lst = (i == 1) and last_i
            cx.mm([lambda: nc.tensor.matmul(P.ps[:, PO, :n], V[:, nkt + i, :], pT[:, :n], start=(i == 0), stop=lst)],
                  reads=[vk, pkey], writes=[f"ps{PO}"], acc=(i > 0))
            cx.mm([lambda: nc.tensor.matmul(P.ps[:, PD, :n], P.ones_b[:], pT[:, :n], start=(i == 0), stop=lst)],
                  reads=['ones_b', pkey], writes=[f"ps{PD}"], acc=(i > 0))

    for g in range(2):
        kT, kk = kp.get()
        V, vk = vp.get()
        cx.dma('sp', kT[:], B['WAK'][g * 128:(g + 1) * 128, 0:nkv + CL], writes=[kk])
        cx.dma('sp', V[:], B['WAV'][0:nkv + CL, g * 128:(g + 1) * 128].rearrange("(t p) d -> p t d", p=128), writes=[vk])
        for hh in range(4):
            h = 4 * g + hh
            qT, qk = qp.get()
            ob, ok = op_.get()
            cx.dma('sp', qT[:], B['WAQ'][h * 128:(h + 1) * 128, 0:nq + CL], writes=[qk])
            for Q in range(nq // 512):
                ctx_tiles(qT[:, Q * 512:(Q + 1) * 512], 512, kT, kk, V, vk, qk, False)
                for jj in range(4 * Q + 1, 4 * Q + 7):
                    j = jj - 2
                    qlo, qhi = max(j - 1, 4 * Q), min(j + 1, 4 * Q + 3)
                    n = (qhi - qlo + 1) * 128
                    c0 = (qlo - 4 * Q) * 128
                    mc0 = (qlo - j + 1) * 128
                    b = cx.bank(0, 6)
                    pk = f"ps{b}"
                    q_ap = qT[:, Q * 512 + c0:Q * 512 + c0 + n]
                    cx.mm([lambda: nc.tensor.matmul(P.ps[:, b, :n], kT[:, jj * 128:(jj + 1) * 128], q_ap, start=True, stop=True)],
                          reads=[kk, qk], writes=[pk])
                    sT, sk = sp.get()
                    cx.op('dve', lambda: nc.vector.tensor_tensor(sT[:, :n], P.ps[:, b, :n], m3[:, mc0:mc0 + n], ALU.add),
                          reads=[pk, 'wam3'], writes=[sk])
                    pT, pkey = pp.get()
                    cx.op('act', lambda: nc.scalar.activation(pT[:, :n], sT[:, :n], AF.Exp, bias=kvb[:, jj:jj + 1], scale=SCALE),
                          reads=[sk, 'waed'], writes=[pkey])
                    lst = jj == 4 * Q + 6
                    cx.mm([lambda: nc.tensor.matmul(P.ps[:, PO, c0:c0 + n], V[:, jj, :], pT[:, :n], start=False, stop=lst)],
                          reads=[vk, pkey], writes=[f"ps{PO}"], acc=True)
                    cx.mm([lambda: nc.tensor.matmul(P.ps[:, PD, c0:c0 + n], P.ones_b[:], pT[:, :n], start=False, stop=lst)],
                          reads=['ones_b', pkey], writes=[f"ps{PD}"], acc=True)
                finalize(512, h, ob, ok, Q * 512)
            ctx_tiles(qT[:, nq:nq + CL], CL, kT, kk, V, vk, qk, True)
            finalize(CL, h, ob, ok, nq)
            cx.dma('sp', B['OWA'][h * 128:(h + 1) * 128, 0:nq + CL], ob[:], reads=[ok])


def phase_conv(P, pes, l, B, cwT_d, cvb_d, lng_d, lnb_d):
    cx, nc = P.cx, P.nc
    nq, nkv = NQR[l] * GW, NKVR[l] * GW
    SEG = 2048
    co = pes.enter_context(nc.sbuf_tensor(cx.key("cvo"), [128, 8, SEG], F32))
    zp = Pool(nc, pes, cx.key("cvz"), [128, SEG + 2 * CV_HALO], F32, 2)
    cw = pes.enter_context(nc.sbuf_tensor(cx.key("cvw"), [128, 8, 31], F32))
    cb = pes.enter_context(nc.sbuf_tensor(cx.key("cvb"), [128, 8], F32))
    lg = pes.enter_context(nc.sbuf_tensor(cx.key("cvg"), [128, 8], F32))
    lb = pes.enter_context(nc.sbuf_tensor(cx.key("cvlb"), [128, 8], F32))
    sqp = Pool(nc, pes, cx.key("cvs"), [128, 512], F32, 3)
    stp = Pool(nc, pes, cx.key("cvt"), [128, 512], F32, 6)
    obp = Pool(nc, pes, cx.key("cvob"), [128, 512], BF16, 4)
    cx.dma('sp', cw[:], cwT_d, writes=['cvw'])
    cx.dma('sp', cb[:], cvb_d, writes=['cvw'])
    cx.dma('sp', lg[:], lng_d, writes=['cvw'])
    cx.dma('sp', lb[:], lnb_d, writes=['cvw'])
    segs = [(s0, min(SEG, nq - s0), False) for s0 in range(0, nq, SEG)] + [(nq, CL, True)]
    for si, (s0, sn, isc) in enumerate(segs):
        for j in range(8):
            e = 'dve'
            E = nc.vector
            z, zk = zp.get()
            if isc:
                cx.op(e, lambda: E.memset(z[:, 0:CV_HALO], 0.0), writes=[zk])
                cx.op(e, lambda: E.memset(z[:, CV_HALO + CL:2 * CV_HALO + CL], 0.0), writes=[zk])
                cx.dma('sp', z[:, CV_HALO:CV_HALO + CL], B['ZT'][j * 128:(j + 1) * 128, nkv:nkv + CL], writes=[zk])
            else:
                zc = 256 + s0 - CV_HALO
                cx.dma('sp', z[:, 0:sn + 2 * CV_HALO], B['ZT'][j * 128:(j + 1) * 128, zc:zc + sn + 2 * CV_HALO], writes=[zk])
            acc = co[:, j, 0:sn]
            ck = f"cvo{j}"
            cx.op(e, lambda: E.tensor_scalar(acc, z[:, 0:sn], cw[:, j, 0:1], cb[:, j:j + 1], ALU.mult, ALU.add),
                  reads=[zk, 'cvw'], writes=[ck])
            for tap in range(1, 31):
                cx.op(e, lambda: E.scalar_tensor_tensor(acc, z[:, tap:tap + sn], cw[:, j, tap:tap + 1], acc,
                                                        ALU.mult, ALU.add), reads=[zk, 'cvw', ck], writes=[ck])
        for t0 in range(0, sn, 512):
            tn = min(512, sn - t0)
            bmu, bsq = cx.bank(), cx.bank()
            for j in range(8):
                ck = f"cvo{j}"
                cx.mm([lambda: nc.tensor.matmul(P.ps[:, bmu, :tn], P.ones_f[:], co[:, j, t0:t0 + tn], start=(j == 0), stop=(j == 7))],
                      reads=[ck, 'ones_f'], writes=[f"ps{bmu}"], acc=(j > 0))
                sq, sk = sqp.get()
                cx.op('act', lambda: nc.scalar.activation(sq[:, :tn], co[:, j, t0:t0 + tn], AF.Square), reads=[ck], writes=[sk])
                cx.mm([lambda: nc.tensor.matmul(P.ps[:, bsq, :tn], P.ones_f[:], sq[:, :tn], start=(j == 0), stop=(j == 7))],
                      reads=[sk, 'ones_f'], writes=[f"ps{bsq}"], acc=(j > 0))
            mu, mk = stp.get()
            ms, msk = stp.get()
            rs, rk = stp.get()
            cx.op('dve', lambda: nc.vector.tensor_scalar(mu[:, :tn], P.ps[:, bmu, :tn], 1.0 / 1024, None, ALU.mult), reads=[f"ps{bmu}"], writes=[mk])
            cx.op('dve', lambda: nc.vector.tensor_tensor(ms[:, :tn], mu[:, :tn], mu[:, :tn], ALU.mult), reads=[mk], writes=[msk])
            cx.op('dve', lambda: nc.vector.scalar_tensor_tensor(rs[:, :tn], P.ps[:, bsq, :tn], 1.0 / 1024, ms[:, :tn], ALU.mult, ALU.subtract),
                  reads=[f"ps{bsq}", msk], writes=[rk])
            cx.op('dve', lambda: nc.vector.tensor_scalar(rs[:, :tn], rs[:, :tn], EPS, None, ALU.add), reads=[rk], writes=[rk])
            cx.op('act', lambda: nc.scalar.activation(rs[:, :tn], rs[:, :tn], AF.Sqrt), reads=[rk], writes=[rk])
            cx.op('dve', lambda: nc.vector.reciprocal(rs[:, :tn], rs[:, :tn]), reads=[rk], writes=[rk])
            for j in range(8):
                ck = f"cvo{j}"
                t1, k1 = sqp.get()
                cx.op('dve', lambda: nc.vector.tensor_tensor(t1[:, :tn], co[:, j, t0:t0 + tn], mu[:, :tn], ALU.subtract), reads=[ck, mk], writes=[k1])
                cx.op('pool', lambda: nc.gpsimd.tensor_tensor(t1[:, :tn], t1[:, :tn], rs[:, :tn], ALU.mult), reads=[k1, rk], writes=[k1])
                ob, obk = obp.get()
                cx.op('act', lambda: nc.scalar.activation(ob[:, :tn], t1[:, :tn], AF.Silu, bias=lb[:, j:j + 1], scale=lg[:, j:j + 1]),
                      reads=[k1, 'cvw'], writes=[obk])
                cx.dma('sp', B['OCV'][j * 128:(j + 1) * 128, s0 + t0:s0 + t0 + tn], ob[:, :tn], reads=[obk])


def phase_merge(P, pes, l, B, XB, wbr, wbr_r0, wout, wout_r0, mod):
    cx, nc = P.cx, P.nc
    otp = Pool(nc, pes, cx.key("mo"), [128, 24, 512], BF16, 2)
    ytp = Pool(nc, pes, cx.key("my"), [128, 16, 512], BF16, 2)
    gtp = Pool(nc, pes, cx.key("mg"), [128, 3, 512], F32, 3)
    tmp = Pool(nc, pes, cx.key("mt"), [128, 512], F32, 6)
    xp = Pool(nc, pes, cx.key("mx"), [128, 512], F32, 4)
    gs_v = B['GST'].rearrange("(i m p) t -> p i m t", i=3, p=128)
    srcs = (B['ONA'], B['OCV'], B['OWA'])
    for (xcol, t0, tn, isc) in o_tiles(l):
        c = 1 if isc else 0
        ot, otk = otp.get()
        for i in range(3):
            cx.dma('sp', ot[:, i * 8:(i + 1) * 8, :tn], srcs[i][:, t0:t0 + tn].rearrange("(k p) t -> p k t", p=128), writes=[otk])
        yT, yk = ytp.get()
        for mg in range(4):
            wts = [P.load_w(wbr, wbr_r0 + i * 1024, 8, mg * 512, 512) for i in range(3)]
            for m in range(4):
                mi = mg * 4 + m
                gt, gk = gtp.get()
                cx.dma('sp', gt[:, :, :tn], gs_v[:, :, mi, t0:t0 + tn], writes=[gk])
                bs = []
                for i in range(3):
                    b = cx.bank()
                    wt, wk = wts[i]
                    fns = [(lambda k=k: nc.tensor.matmul(P.ps[:, b, :tn], wt[:, k, m * 128:(m + 1) * 128], ot[:, i * 8 + k, :tn],
                                                         start=(k == 0), stop=(k == 7))) for k in range(8)]
                    cx.mm(fns, reads=[otk, wk], writes=[f"ps{b}"])
                    bs.append(b)
                ys = []
                for i in range(3):
                    t1, k1 = tmp.get()
                    cx.op('dve', lambda: nc.vector.tensor_tensor(t1[:, :tn], P.ps[:, bs[i], :tn], gt[:, i, :tn], ALU.mult),
                          reads=[f"ps{bs[i]}", gk], writes=[k1])
                    ys.append((t1, k1))
                cx.op('pool', lambda: nc.gpsimd.tensor_tensor(ys[0][0][:, :tn], ys[0][0][:, :tn], ys[1][0][:, :tn], ALU.add),
                      reads=[ys[0][1], ys[1][1]], writes=[ys[0][1]])
                cx.op('pool', lambda: nc.gpsimd.tensor_tensor(yT[:, mi, :tn], ys[0][0][:, :tn], ys[2][0][:, :tn], ALU.add),
                      reads=[ys[0][1], ys[2][1]], writes=[yk])
        for mg in range(4):
            wt, wk = P.load_w(wout, wout_r0, 16, mg * 512, 512)
            for m in range(4):
                mi = mg * 4 + m
                b = cx.bank()
                fns = [(lambda k=k: nc.tensor.matmul(P.ps[:, b, :tn], wt[:, k, m * 128:(m + 1) * 128], yT[:, k, :tn],
                                                     start=(k == 0), stop=(k == 15))) for k in range(16)]
                cx.mm(fns, reads=[yk, wk], writes=[f"ps{b}"])
                xt, xk = xp.get()
                cx.dma('sp', xt[:, :tn], XB[mi * 128:(mi + 1) * 128, xcol:xcol + tn], writes=[xk])
                cx.op('dve', lambda: nc.vector.scalar_tensor_tensor(xt[:, :tn], P.ps[:, b, :tn], mod[:, 32 + mi, c:c + 1], xt[:, :tn],
                                                                    ALU.mult, ALU.add), reads=[f"ps{b}", xk, 'mod'], writes=[xk])
                cx.dma('sp', XB[mi * 128:(mi + 1) * 128, xcol:xcol + tn], xt[:, :tn], reads=[xk])


def ffn_tile(P, res, h2, tn, wi, wi_r0, wo, wo_r0, dff, gate_ap, gate_key, xt, xk, modg_col):
    cx, nc = P.cx, P.nc
    nj = dff // 128
    g, gk = res['g'], 'ffg'
    tmp = res['tmp']
    for jg in range(0, nj, 4):
        wa, ka = P.load_w(wi, wi_r0, 16, jg * 128, 512)
        wb, kb = P.load_w(wi, wi_r0, 16, dff + jg * 128, 512)
        for m in range(4):
            j = jg + m
            b1, b2 = cx.bank(), cx.bank()
            for (bb, ww, kk) in ((b1, wa, ka), (b2, wb, kb)):
                fns = [(lambda k=k: nc.tensor.matmul(P.ps[:, bb, :tn], ww[:, k, m * 128:(m + 1) * 128], h2[:, k, :tn],
                                                     start=(k == 0), stop=(k == 15))) for k in range(16)]
                cx.mm(fns, reads=['h2', kk], writes=[f"ps{bb}"])
            t1, k1 = tmp.get()
            cx.op('act', lambda: nc.scalar.activation(t1[:, :tn], P.ps[:, b1, :tn], AF.Silu), reads=[f"ps{b1}"], writes=[k1])
            if gate_ap is None:
                cx.op('dve', lambda: nc.vector.tensor_tensor(g[:, j, :tn], t1[:, :tn], P.ps[:, b2, :tn], ALU.mult),
                      reads=[k1, f"ps{b2}"], writes=[gk])
            else:
                cx.op('dve', lambda: nc.vector.tensor_tensor(t1[:, :tn], t1[:, :tn], P.ps[:, b2, :tn], ALU.mult),
                      reads=[k1, f"ps{b2}"], writes=[k1])
                cx.op('pool', lambda: nc.gpsimd.tensor_tensor(g[:, j, :tn], t1[:, :tn], gate_ap, ALU.mult),
                      reads=[k1, gate_key], writes=[gk])
    for m in range(16):
        wt, wk = P.load_w(wo, wo_r0, nj, m * 128, 128)
        b = cx.bank()
        fns = [(lambda k=k: nc.tensor.matmul(P.ps[:, b, :tn], wt[:, k, :], g[:, k, :tn], start=(k == 0), stop=(k == nj - 1)))
               for k in range(nj)]
        cx.mm(fns, reads=[gk, wk], writes=[f"ps{b}"])
        cx.op('dve', lambda: nc.vector.scalar_tensor_tensor(xt[:, m, :tn], P.ps[:, b, :tn], modg_col(m), xt[:, m, :tn],
                                                            ALU.mult, ALU.add), reads=[f"ps{b}", xk, 'mod'], writes=[xk])


def phase_ffn(P, pes, l, XB, mod, A2, moe, wi, wi_r0, wo, wo_r0, wr_d=None, ident_d=None, selm_d=None, skip_ctx=False):
    cx, nc = P.cx, P.nc
    h2 = pes.enter_context(nc.sbuf_tensor(cx.key("h2"), [128, 16, 512], BF16))
    res = {'g': pes.enter_context(nc.sbuf_tensor(cx.key("ffg"), [128, 44, 512], BF16)),
           'tmp': Pool(nc, pes, cx.key("fft"), [128, 512], F32, 4)}
    xp = Pool(nc, pes, cx.key("ffx"), [128, 16, 512], F32, 1)
    pools = norm_pools(P, pes)
    if moe:
        wr = pes.enter_context(nc.sbuf_tensor(cx.key("wr"), [128, 16, 8], F32))
        ident = pes.enter_context(nc.sbuf_tensor(cx.key("ident"), [128, 128], F32))
        selm = pes.enter_context(nc.sbuf_tensor(cx.key("selm"), [8, 8 * 128], F32))
        lgT = pes.enter_context(nc.sbuf_tensor(cx.key("lgT"), [8, 512], F32))
        L = pes.enter_context(nc.sbuf_tensor(cx.key("rL"), [128, 4, 8], F32))
        W1 = pes.enter_context(nc.sbuf_tensor(cx.key("rW1"), [128, 4, 8], F32))
        W2 = pes.enter_context(nc.sbuf_tensor(cx.key("rW2"), [128, 4, 8], F32))
        m1 = pes.enter_context(nc.sbuf_tensor(cx.key("rm1"), [128, 4], F32))
        m2 = pes.enter_context(nc.sbuf_tensor(cx.key("rm2"), [128, 4], F32))
        gT = pes.enter_context(nc.sbuf_tensor(cx.key("rgT"), [8, 512], F32))
        G = pes.enter_context(nc.sbuf_tensor(cx.key("rG"), [128, 8, 512], F32))
        cx.dma('sp', wr[:], wr_d.rearrange("(k p) e -> p k e", p=128), writes=['wr'])
        cx.dma('sp', ident[:], ident_d, writes=['ident'])
        cx.dma('sp', selm[:], selm_d, writes=['selm'])
    for (xcol, _, tn, isc) in o_tiles(l):
        if isc and skip_ctx:
            continue
        c = 1 if isc else 0
        nb = tn // 128
        hf_cb = None
        if moe:
            br = cx.bank()

            def hf_cb(k, n, hf, hk):
                cx.mm([lambda: nc.tensor.matmul(P.ps[0:8, br, :n], wr[:, k, :], hf[:, :n], start=(k == 0), stop=(k == 15))],
                      reads=[hk, 'wr'], writes=[f"ps{br}"], acc=(k > 0))
        rms_norm_tile(P, pools, XB, xcol, tn, c, A2, mod[:, 48:64, :], lambda k: (h2[:, k, :tn], 'h2'), hf_cb=hf_cb)
        xt, xk = xp.get()
        cx.dma('sp', xt[:, :, :tn], XB[:, xcol:xcol + tn].rearrange("(k p) t -> p k t", p=128), writes=[xk])
        mg = lambda m: mod[:, 80 + m, c:c + 1]
        if not moe:
            ffn_tile(P, res, h2, tn, wi, wi_r0, wo, wo_r0, D_FF, None, None, xt, xk, mg)
        else:
            cx.op('dve', lambda: nc.vector.tensor_copy(lgT[:, :tn], P.ps[0:8, br, :tn]), reads=[f"ps{br}"], writes=['lgT'])
            bt = cx.bank()
            for blk in range(nb):
                cx.mm([lambda: nc.tensor.matmul(P.ps[:, bt, blk * 8:(blk + 1) * 8], lgT[:, blk * 128:(blk + 1) * 128], ident[0:8, 0:8],
                                                start=True, stop=True)], reads=['lgT', 'ident'], writes=[f"ps{bt}"], acc=(blk > 0))
            Lv, W1v, W2v = L[:, :nb, :], W1[:, :nb, :], W2[:, :nb, :]
            cx.op('dve', lambda: nc.vector.tensor_copy(Lv, P.ps[:, bt, 0:nb * 8].rearrange("p (b e) -> p b e", e=8)),
                  reads=[f"ps{bt}"], writes=['rL'])
            cx.op('dve', lambda: nc.vector.tensor_reduce(m1[:, :nb], Lv, AX.X, ALU.max), reads=['rL'], writes=['rm1'])
            for blk in range(nb):
                cx.op('dve', lambda: nc.vector.tensor_scalar(W1[:, blk, :], L[:, blk, :], m1[:, blk:blk + 1], None, ALU.is_equal),
                      reads=['rL', 'rm1'], writes=['rW1'])
            cx.op('dve', lambda: nc.vector.scalar_tensor_tensor(W2v, W1v, NEG, Lv, ALU.mult, ALU.add), reads=['rW1', 'rL'], writes=['rW2'])
            cx.op('dve', lambda: nc.vector.tensor_reduce(m2[:, :nb], W2v, AX.X, ALU.max), reads=['rW2'], writes=['rm2'])
            for blk in range(nb):
                cx.op('dve', lambda: nc.vector.tensor_scalar(W1[:, blk, :], L[:, blk, :], m2[:, blk:blk + 1], None, ALU.is_ge),
                      reads=['rL', 'rm2', 'rW1'], writes=['rW1'])
                cx.op('dve', lambda: nc.vector.tensor_scalar(W2[:, blk, :], L[:, blk, :], m1[:, blk:blk + 1], None, ALU.subtract),
                      reads=['rL', 'rm1', 'rW2'], writes=['rW2'])
            cx.op('act', lambda: nc.scalar.activation(W2v, W2v, AF.Exp), reads=['rW2'], writes=['rW2'])
            cx.op('dve', lambda: nc.vector.tensor_tensor(W2v, W2v, W1v, ALU.mult), reads=['rW2', 'rW1'], writes=['rW2'])
            cx.op('dve', lambda: nc.vector.tensor_reduce(m1[:, :nb], W2v, AX.X, ALU.add), reads=['rW2', 'rm1'], writes=['rm1'])
            cx.op('dve', lambda: nc.vector.reciprocal(m1[:, :nb], m1[:, :nb]), reads=['rm1'], writes=['rm1'])
            for blk in range(nb):
                cx.op('dve', lambda: nc.vector.tensor_scalar(W2[:, blk, :], W2[:, blk, :], m1[:, blk:blk + 1], None, ALU.mult),
                      reads=['rW2', 'rm1'], writes=['rW2'])
            bg = cx.bank()
            for blk in range(nb):
                cx.mm([lambda: nc.tensor.matmul(P.ps[0:8, bg, blk * 128:(blk + 1) * 128], W2[:, blk, :], ident[:, :], start=True, stop=True)],
                      reads=['rW2', 'ident'], writes=[f"ps{bg}"], acc=(blk > 0))
            cx.op('dve', lambda: nc.vector.tensor_copy(gT[:, :tn], P.ps[0:8, bg, :tn]), reads=[f"ps{bg}"], writes=['rgT'])
            for e in range(NE):
                be = cx.bank()
                cx.mm([lambda: nc.tensor.matmul(P.ps[:, be, :tn], selm[:, e * 128:(e + 1) * 128], gT[:, :tn], start=True, stop=True)],
                      reads=['selm', 'rgT'], writes=[f"ps{be}"])
                cx.op('act', lambda: nc.scalar.activation(G[:, e, :tn], P.ps[:, be, :tn], AF.Copy), reads=[f"ps{be}"], writes=['rG'])
            for e in range(NE):
                ffn_tile(P, res, h2, tn, wi, wi_r0 + e * D, wo, wo_r0 + e * D_FFE, D_FFE, G[:, e, :tn], 'rG', xt, xk, mg)
        cx.dma('sp', XB[:, xcol:xcol + tn].rearrange("(k p) t -> p k t", p=128), xt[:, :, :tn], reads=[xk])


def phase_final(P, pes, XB, gfT_d, yo):
    cx, nc = P.cx, P.nc
    gf = pes.enter_context(nc.sbuf_tensor(cx.key("gf"), [128, 16], F32))
    cx.dma('sp', gf[:], gfT_d, writes=['gf'])
    xp = Pool(nc, pes, cx.key("fx"), [128, 16, 512], F32, 2)
    sqp = Pool(nc, pes, cx.key("fsq"), [128, 512], F32, 3)
    rp = Pool(nc, pes, cx.key("fr"), [128, 512], F32, 2)
    for i in range(4):
        xcol = 1024 + 512 * i
        xt, xk = xp.get()
        cx.dma('sp', xt[:], XB[:, xcol:xcol + 512].rearrange("(k p) t -> p k t", p=128), writes=[xk])
        b = cx.bank()
        for k in range(16):
            sq, sk = sqp.get()
            cx.op('act', lambda: nc.scalar.activation(sq[:], xt[:, k, :], AF.Square), reads=[xk], writes=[sk])
            cx.mm([lambda: nc.tensor.matmul(P.ps[:, b, :], P.ones_f[:], sq[:], start=(k == 0), stop=(k == 15))],
                  reads=[sk, 'ones_f'], writes=[f"ps{b}"], acc=(k > 0))
        rt, rk = rp.get()
        cx.op('dve', lambda: nc.vector.tensor_scalar(rt[:], P.ps[:, b, :], 1.0 / D, EPS, ALU.mult, ALU.add), reads=[f"ps{b}"], writes=[rk])
        cx.op('act', lambda: nc.scalar.activation(rt[:], rt[:], AF.Sqrt), reads=[rk], writes=[rk])
        cx.op('dve', lambda: nc.vector.reciprocal(rt[:], rt[:]), reads=[rk], writes=[rk])
        for k in range(16):
            e = 'dve'
            E = nc.vector
            cx.op(e, lambda: E.scalar_tensor_tensor(xt[:, k, :], xt[:, k, :], gf[:, k:k + 1], rt[:], ALU.mult, ALU.mult),
                  reads=[xk, rk, 'gf'], writes=[xk])
        cx.dma('sp', yo[:, 512 * i:512 * (i + 1)].rearrange("(k p) t -> p k t", p=128), xt[:], reads=[xk])


def build_all(depth=DEPTH, dbg=None):
    es = ExitStack()
    P = Prog(es)
    nc, cx = P.nc, P.cx
    I = lambda n, s, d: P.dram(n, s, d, "ExternalInput")
    n_dense, n_moe = (depth + 1) // 2, depth // 2
    L0 = DEPTH - depth
    xin = I("xT", [D, XC], F32)
    cT = I("cT", [128, 16, 2], F32)
    wmod = I("wmod", [depth * D, 6 * D], F32)
    bmodT = I("bmodT", [depth, 128, 96], F32)
    gT_d = I("gT", [128, depth * 32 + 16], F32)
    win = I("win", [depth * D, IN_DIM], F32)
    cosd, sind, tval = I("cosT", [128, XW], F32), I("sinT", [128, XW], F32), I("tval", [128, XW], F32)
    ttab = I("ttab", [depth * 8, 128, NIDX * 64], F32)
    maskL = [I(f"maskL{l}", [8, (NQR[L0 + l] // 8) * NKT * 128], BF16) for l in range(depth)]
    rsel = I("rsel", [8, 512], BF16)
    m3 = I("m3", [128, 384], F32)
    kvb = [I(f"kvb{l}", [128, NKVR[L0 + l] * GW // 128], F32) for l in range(depth)]
    sinkb = I("sinkb", [depth, 128, 8], F32)
    cwT = I("cwT", [depth, 128, 8, 31], F32)
    cvp = I("cvp", [depth, 3, 128, 8], F32)
    wbr = I("wbr", [depth * 3072, D], F32)
    wout = I("wout", [depth * D, D], F32)
    fwi = I("fwi", [n_dense * D, 2 * D_FF], F32)
    fwo = I("fwo", [n_dense * D_FF, D], F32)
    if n_moe:
        mwi = I("mwi", [n_moe * NE * D, 2 * D_FFE], F32)
        mwo = I("mwo", [n_moe * NE * D_FFE, D], F32)
        mwr = I("mwr", [n_moe * D, NE], F32)
        ident = I("ident", [128, 128], F32)
        selm = I("selm", [8, 8 * 128], F32)
    yo = P.dram("yo", [D, TL], F32, "ExternalOutput")
    T = lambda n, s, d: P.dram(n, s, d, "Internal")
    XB = T("XB", [D, XC], F32)
    HT = T("HT", [D, XC], BF16)
    NQM = NQR[0] * GW + CL
    KWM = NKVR[0] * GW + 2 * NAM + CL
    B = {'NAQ': T("NAQ", [1024, NQM], BF16), 'NAK': T("NAK", [1024, KWM], BF16), 'NAV': T("NAV", [KWM, 1024], BF16),
         'WAQ': T("WAQ", [1024, NQM], BF16), 'WAK': T("WAK", [256, XC], BF16), 'WAV': T("WAV", [XC, 256], BF16),
         'ZT': T("ZT", [1024, XC], F32), 'GST': T("GST", [6144, NQM], F32),
         'ONA': T("ONA", [1024, NQM], BF16), 'OCV': T("OCV", [1024, NQM], BF16), 'OWA': T("OWA", [1024, NQM], BF16)}
    dbg_out = {}
    if dbg:
        for k in dbg:
            a = B[k] if k in B else {'XB': XB, 'HT': HT}[k]
            dbg_out[k] = P.dram("dbg_" + k, list(a.shape), a.dtype, "ExternalOutput")
    mod = es.enter_context(nc.sbuf_tensor("mod", [128, 96, 2], F32))
    A = es.enter_context(nc.sbuf_tensor("modA", [128, 16, 2], F32))
    gT = es.enter_context(nc.sbuf_tensor("gT_sb", [128, depth * 32 + 16], F32))
    cx.dma('sp', gT[:], gT_d, writes=['gT'])
    for i in range(4):
        cx.dma('sp' if i % 2 == 0 else 'act', XB[i * 512:(i + 1) * 512, :], xin[i * 512:(i + 1) * 512, :], writes=['XB'])
    with ExitStack() as pes:
        zt = pes.enter_context(nc.sbuf_tensor("zt", [128, 8192], BF16))
        cx.op('pool', lambda: nc.gpsimd.memset(zt[:], 0.0), writes=['zt'])
        for r in range(8):
            cx.dma('sp', B['NAK'][r * 128:(r + 1) * 128, :], zt[:, 0:KWM], reads=['zt'], writes=['NAK'])
        nav_v = B['NAV'].rearrange("(t p) d -> p t d", p=128)
        ntile = KWM // 128
        for t0 in range(0, ntile, 8):
            tn = min(8, ntile - t0)
            cx.dma('sp', nav_v[:, t0:t0 + tn, :], zt[:, 0:tn * 1024].rearrange("p (t d) -> p t d", d=1024), reads=['zt'], writes=['NAV'])
        barrier(cx)
    for l in range(depth):
        g = L0 + l
        moe = (l % 2 == 1)
        li = l // 2
        last = (l == depth - 1)
        with ExitStack() as pes:
            phase_modvec(P, cT, wmod[l * D:(l + 1) * D, :], bmodT[l], mod, pes)
            make_A(P, mod, gT, A, 16, l * 32)
            barrier(cx)
        with ExitStack() as pes:
            phase_norm1(P, pes, g, XB, HT, A, mod[:, 0:16, :])
            barrier(cx)
        with ExitStack() as pes:
            phase_inproj(P, pes, g, HT, win, l * D, cosd, sind, tval, B)
            barrier(cx)
        with ExitStack() as pes:
            phase_conv(P, pes, g, B, cwT[l], cvp[l, 0], cvp[l, 1], cvp[l, 2])
            barrier(cx)
        with ExitStack() as pes:
            phase_na(P, pes, g, B, lambda h: ttab[l * 8 + h], maskL[l], rsel)
            barrier(cx)
        with ExitStack() as pes:
            phase_wa(P, pes, g, B, m3, kvb[l], sinkb[l])
            barrier(cx)
        with ExitStack() as pes:
            phase_merge(P, pes, g, B, XB, wbr, l * 3072, wout, l * D, mod)
            barrier(cx)
        make_A(P, mod, gT, A, 64, l * 32 + 16)
        with ExitStack() as pes:
            if moe:
                phase_ffn(P, pes, g, XB, mod, A, True, mwi, li * NE * D, mwo, li * NE * D_FFE, mwr[li * D:(li + 1) * D, :], ident, selm,
                          skip_ctx=last)
            else:
                phase_ffn(P, pes, g, XB, mod, A, False, fwi, li * D, fwo, li * D_FF, skip_ctx=last)
            barrier(cx)
    for k, o in dbg_out.items():
        a = B[k] if k in B else {'XB': XB, 'HT': HT}[k]
        cx.dma('sp', o, a)
    with ExitStack() as pes:
        phase_final(P, pes, XB, gT_d[:, depth * 32:depth * 32 + 16], yo)
        barrier(cx)
    cx.finish('sp')
    print("fused program: depth", depth, "inst", cx.n_inst, "waits", cx.n_wait)
    return P


def fm(v, n):
    return np.ascontiguousarray(np.asarray(v, dtype=np.float32).reshape(n, 128).T)


def rope_tables(tok0, ntok):
    t = np.arange(tok0, tok0 + ntok, dtype=np.int64)
    pos = np.stack([t // GW, t % GW], axis=-1).astype(np.float32)
    inv_freq = (1.0 / np.power(np.float32(10000.0), np.arange(32, dtype=np.float32) / np.float32(32))).astype(np.float32)
    ang = (pos[:, :, None] * inv_freq).astype(np.float32)
    cos, sin = np.cos(ang).astype(np.float32), np.sin(ang).astype(np.float32)
    cosT = np.zeros((128, ntok), np.float32)
    sinT = np.zeros((128, ntok), np.float32)
    for a in range(2):
        for j in range(2):
            sl = slice(a * 64 + j * 32, a * 64 + j * 32 + 32)
            cosT[sl] = cos[:, a, :].T
            sinT[sl] = (-1.0 if j == 0 else 1.0) * sin[:, a, :].T
    return cosT, sinT


def na_bias_table(rpb_l):
    p = np.arange(128)
    a, kc = p // 64, p % 64
    idx = np.arange(NIDX)
    c = np.arange(64)
    dr = a[:, None] + 13 - idx[None, :]
    drv = (np.abs(dr) <= 7)
    ws = np.clip(c - 8, 0, 48)
    colv = (kc[:, None] >= ws[None, :]) & (kc[:, None] < ws[None, :] + 16)
    cidx = np.clip(kc[:, None] - c[None, :] + 15, 0, 30)
    g = rpb_l[:, np.clip(dr + 7, 0, 14)[:, :, None], cidx[:, None, :]]
    g = np.where(drv[None, :, :, None], g, np.float32(0.0))
    g = np.where(colv[None, :, None, :], g, np.float32(NEG))
    return np.ascontiguousarray(g.reshape(8, 128, NIDX * 64).astype(np.float32))


def na_mask_lhs(s_row, nqb, rows):
    m = np.zeros((8, nqb, NKT, 2, 64), np.float32)
    for qb in range(nqb):
        for t in range(NKT):
            for a in range(2):
                kr = s_row + 8 * qb - 7 + 2 * t + a
                for rp in range(8):
                    r = s_row + 8 * qb + rp
                    r0 = min(max(r - 4, 0), rows - 8)
                    if not ((0 <= kr < rows) and (r0 <= kr < r0 + 8)):
                        m[rp, qb, t, a, :] = NEG
    return np.ascontiguousarray(m.reshape(8, nqb * NKT * 128)).astype(ml_dtypes.bfloat16)


def wa_band_mask():
    k = np.arange(128)[:, None]
    rel = np.arange(384)[None, :] // 128
    q = np.arange(384)[None, :] % 128
    ok = np.abs((rel - 1) * 128 + q - k) <= 128
    return np.where(ok, np.float32(0), np.float32(NEG)).astype(np.float32)


RSEL = np.zeros((8, 512), np.float32)
for _r in range(8):
    RSEL[_r, _r * 64:(_r + 1) * 64] = 1.0
RSEL = RSEL.astype(ml_dtypes.bfloat16)
IDENT = np.eye(128, dtype=np.float32)
SELM = np.zeros((8, 8 * 128), np.float32)
for _e in range(8):
    SELM[_e, _e * 128:(_e + 1) * 128] = 1.0

_PROGS = {}


def run_model(inp, depth, dbg=None, trace=False):
    x = np.asarray(inp['x'], np.float32)
    Bn, S, _ = x.shape
    rows = S // GW
    cpb = rows // 32
    ncore = Bn * cpb
    L0 = DEPTH - depth
    n_dense, n_moe = (depth + 1) // 2, depth // 2
    key = (depth, tuple(dbg) if dbg else None)
    if key not in _PROGS:
        _PROGS[key] = build_all(depth, dbg)
    P = _PROGS[key]
    f32 = lambda a: np.ascontiguousarray(np.asarray(a, np.float32))
    shared = dict(
        wmod=f32(inp['w_mod'][:depth]).reshape(depth * D, 6 * D),
        bmodT=np.stack([fm(inp['b_mod'][l], 96) for l in range(depth)]),
        gT=np.concatenate([np.concatenate([fm(inp['g_norm1'][l], 16), fm(inp['g_norm2'][l], 16)], axis=1) for l in range(depth)]
                          + [fm(inp['g_final'], 16)], axis=1),
        win=f32(inp['w_in'][:depth]).reshape(depth * D, IN_DIM),
        ttab=np.concatenate([na_bias_table(f32(inp['rpb'][l])) for l in range(depth)], axis=0),
        rsel=RSEL, m3=wa_band_mask(),
        sinkb=np.stack([np.ascontiguousarray(np.broadcast_to(f32(inp['sink'][l])[None, :], (128, 8))) for l in range(depth)]),
        cwT=np.stack([np.ascontiguousarray(f32(inp['conv_w'][l]).T.reshape(8, 128, 31).transpose(1, 0, 2)) for l in range(depth)]),
        cvp=np.stack([np.stack([fm(inp['conv_b'][l], 8), fm(inp['ln_g'][l], 8), fm(inp['ln_b'][l], 8)]) for l in range(depth)]),
        wbr=f32(inp['w_branch'][:depth]).reshape(depth * 3072, D),
        wout=f32(inp['w_out'][:depth]).reshape(depth * D, D),
        fwi=f32(inp['ffn_wi'][:n_dense]).reshape(n_dense * D, 2 * D_FF),
        fwo=f32(inp['ffn_wo'][:n_dense]).reshape(n_dense * D_FF, D),
    )
    if n_moe:
        shared.update(mwi=f32(inp['moe_wi'][:n_moe]).reshape(n_moe * NE * D, 2 * D_FFE),
                      mwo=f32(inp['moe_wo'][:n_moe]).reshape(n_moe * NE * D_FFE, D),
                      mwr=f32(inp['moe_router'][:n_moe]).reshape(n_moe * D, NE), ident=IDENT, selm=SELM)
    maps = []
    for core in range(ncore):
        b, ci = core // cpb, core % cpb
        R0 = 32 * ci
        w0 = R0 - 16
        xw = np.zeros((XW, D), np.float32)
        lo, hi = max(w0, 0), min(w0 + WROWS, rows)
        xw[(lo - w0) * GW:(hi - w0) * GW] = x[b, lo * GW:hi * GW]
        xT = np.ascontiguousarray(np.concatenate([xw.T, f32(inp['ctx'][b]).T], axis=1))
        cT = np.ascontiguousarray(np.stack([fm(inp['c'][b], 16), fm(inp['c_ctx'], 16)], axis=-1))
        cosT, sinT = rope_tables(w0 * GW, XW)
        tok = np.arange(XW) + w0 * GW
        valid = (tok >= 0) & (tok < S)
        tval = np.ascontiguousarray(np.broadcast_to(valid.astype(np.float32)[None, :], (128, XW)))
        d = dict(shared, xT=xT, cT=cT, cosT=cosT, sinT=sinT, tval=tval)
        for l in range(depth):
            g = L0 + l
            d[f"maskL{l}"] = na_mask_lhs(w0 + 4 * (g + 1), NQR[g] // 8, rows)
            nkt = NKVR[g] * GW // 128
            kv = valid[256 * g:256 * g + nkt * 128].reshape(nkt, 128).T
            d[f"kvb{l}"] = np.ascontiguousarray(np.where(kv, np.float32(0), np.float32(NEG)).astype(np.float32))
        maps.append(d)
    res = run_bass_kernel_spmd(P.nc, maps, core_ids=list(range(ncore)), **({'trace': True} if trace else {}))
    out = np.zeros((Bn, S, D), np.float32)
    for core in range(ncore):
        b, ci = core // cpb, core % cpb
        out[b, ci * TL:(ci + 1) * TL, :] = np.asarray(res.results[core]['yo']).T
    return out, res


def kernel(**inputs):
    out, _ = run_model(inputs, DEPTH)
    return out
```

```python
import numpy as np
from contextlib import ExitStack
import ml_dtypes
import concourse.bass as bass
import concourse.mybir as mybir
from concourse.bass_utils import run_bass_kernel_spmd

F32 = mybir.dt.float32
BF16 = mybir.dt.bfloat16
AF = mybir.ActivationFunctionType
ALU = mybir.AluOpType
AX = mybir.AxisListType

D = 2048
TL = 2048
CL = 256
TT = TL + CL
GW = 64
HD = 128
IN_DIM = 12800
D_FF = 5632
NE = 8
D_FFE = 4096
EPS = 1e-6
NEG = -1e30
SCALE = HD ** -0.5
TILES = [(0, 512), (512, 512), (1024, 512), (1536, 512), (2048, 256)]
NA_HALO = 7 * GW
WA_HALO = 128
CV_HALO = 15

SEM_ROT = 30000
N_DMA_SEMS = 32


class Ctx:
    def __init__(self, nc, es):
        self.nc = nc
        self.es = es
        self.eng = {'pe': nc.tensor, 'act': nc.scalar, 'dve': nc.vector, 'pool': nc.gpsimd, 'sp': nc.sync}
        self.sems = {}
        self.cur = {}
        self.gen = {e: 0 for e in self.eng}
        for e in self.eng:
            self._new_sem(e)
        self.dma_sems = []
        for j in range(N_DMA_SEMS):
            k = ('dma', j)
            self.sems[k] = es.enter_context(nc.semaphore(f"dma{j}"))
            self.dma_sems.append([k, 0])
        self.dma_rr = 0
        self.waited = {e: {} for e in self.eng}
        self.lastw = {}
        self.readers = {}
        self.n_inst = 0
        self.n_wait = 0
        self.bank_rr = 0
        self.uid = 0

    def _new_sem(self, e):
        k = (e, self.gen[e])
        self.gen[e] += 1
        self.sems[k] = self.es.enter_context(self.nc.semaphore(f"s_{e}_{k[1]}"))
        self.cur[e] = [k, 0]

    def _wait(self, e, tok):
        if tok is None:
            return
        k, v = tok
        w = self.waited[e]
        if w.get(k, 0) >= v:
            return
        self.eng[e].wait_ge(self.sems[k], v)
        w[k] = v
        self.n_wait += 1

    def _deps(self, e, reads, writes, acc=False):
        for r in reads:
            self._wait(e, self.lastw.get(r))
        for wk in writes:
            lw = self.lastw.get(wk)
            if not (acc and lw is not None and lw[0][0] == 'pe'):
                self._wait(e, lw)
            for t in self.readers.get(wk, ()):
                self._wait(e, t)

    def _commit(self, tok, reads, writes):
        for r in reads:
            lst = self.readers.setdefault(r, [])
            lst.append(tok)
            if len(lst) > 12:
                d = {}
                for k, v in lst:
                    d[k] = max(d.get(k, 0), v)
                self.readers[r] = list(d.items())
        for wk in writes:
            self.lastw[wk] = tok
            self.readers[wk] = []

    def _bump(self, e, ins):
        c = self.cur[e]
        c[1] += 1
        ins.then_inc(self.sems[c[0]], 1)
        tok = (c[0], c[1])
        if c[1] >= SEM_ROT:
            self._new_sem(e)
        return tok

    def op(self, e, fn, reads=(), writes=()):
        self._deps(e, reads, writes)
        ins = fn()
        tok = self._bump(e, ins)
        self._commit(tok, reads, writes)
        self.n_inst += 1
        return tok

    def mm(self, fns, reads=(), writes=(), acc=False):
        self._deps('pe', reads, writes, acc)
        ins = None
        for fn in fns:
            ins = fn()
            self.n_inst += 1
        tok = self._bump('pe', ins)
        self._commit(tok, reads, writes)
        return tok

    def dma(self, q, out, in_, reads=(), writes=(), **kw):
        self._deps(q, reads, writes)
        s = self.dma_sems[self.dma_rr]
        self.dma_rr = (self.dma_rr + 1) % len(self.dma_sems)
        k = s[0]
        if s[1] > 0:
            self._wait(q, (k, 16 * s[1]))
        ins = self.eng[q].dma_start(out=out, in_=in_, **kw)
        s[1] += 1
        ins.then_inc(self.sems[k], 16)
        tok = (k, 16 * s[1])
        self._commit(tok, reads, writes)
        self.n_inst += 1
        return tok

    def finish(self, e='sp'):
        for s in self.dma_sems:
            if s[1] > 0:
                self._wait(e, (s[0], 16 * s[1]))
        for en in self.eng:
            for g in range(self.gen[en]):
                k = (en, g)
                v = self.cur[en][1] if self.cur[en][0] == k else SEM_ROT
                if v > 0:
                    self._wait(e, (k, v))

    def bank(self, lo=0, hi=8):
        b = lo + (self.bank_rr % (hi - lo))
        self.bank_rr += 1
        return b

    def key(self, base):
        self.uid += 1
        return f"{base}_{self.uid}"


class Pool:
    def __init__(self, nc, es, name, shape, dtype, n):
        self.bufs = [es.enter_context(nc.sbuf_tensor(f"{name}{i}", shape, dtype)) for i in range(n)]
        self.keys = [f"{name}{i}" for i in range(n)]
        self.i = 0

    def get(self):
        j = self.i % len(self.bufs)
        self.i += 1
        return self.bufs[j], self.keys[j]


class Prog:
    def __init__(self, es):
        self.nc = bass.Bass("TRN2", target_bir_lowering=False)
        self.es = es
        self.cx = Ctx(self.nc, es)
        nc = self.nc
        self.ps = es.enter_context(nc.psum_tensor("ps", [128, 8, 512], F32))
        self.wpool = Pool(nc, es, "wb", [128, 8192], BF16, 3)
        self.ones_f = es.enter_context(nc.sbuf_tensor("ones_f", [128, 128], F32))
        self.ones_b = es.enter_context(nc.sbuf_tensor("ones_b", [128, 128], BF16))
        self.cx.op('dve', lambda: nc.vector.memset(self.ones_f[:], 1.0), writes=['ones_f'])
        self.cx.op('dve', lambda: nc.vector.memset(self.ones_b[:], 1.0), writes=['ones_b'])
        self.wq = 0

    def dram(self, name, shape, dt, kind):
        return self.nc.dram_tensor(name, list(shape), dt, kind=kind).ap()

    def load_w(self, W, r0, nk, c0, ncols, extra_reads=()):
        assert nk * ncols <= 8192
        buf, key = self.wpool.get()
        view = buf[:, 0:nk * ncols].rearrange("p (k c) -> p k c", k=nk)
        src = W[r0:r0 + nk * 128, c0:c0 + ncols].rearrange("(k p) c -> p k c", p=128)
        self.cx.dma('pool', view, src, reads=list(extra_reads), writes=[key])
        return view, key


def linear_fm(P, act, act_key, nk, W, r0, c0, ncols, tiles, evac, group=512):
    cx, nc = P.cx, P.nc
    g = min(group, 8192 // nk // 128 * 128)
    for gc in range(0, ncols, g):
        gn = min(g, ncols - gc)
        wt, wkey = P.load_w(W, r0, nk, c0 + gc, gn)
        for m in range(gn // 128):
            for (t0, tn) in tiles:
                b = cx.bank()
                pk = f"ps{b}"
                fns = [(lambda k=k: nc.tensor.matmul(P.ps[:, b, :tn], wt[:, k, m * 128:(m + 1) * 128],
                                                     act[:, k, t0:t0 + tn], start=(k == 0), stop=(k == nk - 1)))
                       for k in range(nk)]
                cx.mm(fns, reads=[act_key, wkey], writes=[pk])
                evac((gc // 128) + m, (t0, tn), P.ps[:, b, :tn], pk)


def linear_tm(P, act, act_key, nk, W, r0, c0, ncols, ntok, evac):
    cx, nc = P.cx, P.nc
    assert ncols <= 512
    wt, wkey = P.load_w(W, r0, nk, c0, ncols)
    for tt in range(ntok // 128):
        b = cx.bank()
        pk = f"ps{b}"
        fns = [(lambda k=k: nc.tensor.matmul(P.ps[:, b, :ncols], act[:, k, tt * 128:(tt + 1) * 128],
                                             wt[:, k, :], start=(k == 0), stop=(k == nk - 1)))
               for k in range(nk)]
        cx.mm(fns, reads=[act_key, wkey], writes=[pk])
        evac(tt, P.ps[:, b, :ncols], pk)


def alt_copy(P, i, out, in_, reads, writes, scale=None):
    cx, nc = P.cx, P.nc
    if i % 2 == 0:
        cx.op('act', lambda: nc.scalar.activation(out, in_, AF.Copy), reads=reads, writes=writes)
    else:
        cx.op('dve', lambda: nc.vector.tensor_copy(out, in_), reads=reads, writes=writes)


DEPTH = 4
NQR = [56, 48, 40, 32]
NKVR = [64, 56, 48, 40]
WROWS = 64
XW = WROWS * GW
XC = XW + CL
NAM = 3 * GW
NKT = 11
NIDX = 28


def barrier(cx):
    for e in cx.eng:
        cx.finish(e)


def kv_tiles(l):
    nq = NQR[l] * GW
    t = [(0, 256, 'm')] + [(256 + 512 * i, 512, 'o') for i in range(nq // 512)] + [(256 + nq, 256, 'm')]
    return t


def o_tiles(l):
    nq = NQR[l] * GW
    q0 = 256 * (l + 1)
    return [(q0 + 512 * i, 512 * i, 512, False) for i in range(nq // 512)] + [(XW, nq, CL, True)]


def rms_norm_tile(P, pools, xsrc, xcol, n, c, A, Sh, dst_fn, hf_cb=None):
    cx, nc = P.cx, P.nc
    xp, sqp, rp = pools
    b = cx.bank()
    pk = f"ps{b}"
    for k in range(16):
        xt, xk = xp.get()
        cx.dma('sp', xt[:, :n], xsrc[k * 128:(k + 1) * 128, xcol:xcol + n], writes=[xk])
        sq, sk = sqp.get()
        cx.op('act', lambda: nc.scalar.activation(sq[:, :n], xt[:, :n], AF.Square), reads=[xk], writes=[sk])
        cx.mm([lambda: nc.tensor.matmul(P.ps[:, b, :n], P.ones_f[:], sq[:, :n], start=(k == 0), stop=(k == 15))],
              reads=[sk, 'ones_f'], writes=[pk], acc=(k > 0))
    rt, rk = rp.get()
    cx.op('dve', lambda: nc.vector.tensor_scalar(rt[:, :n], P.ps[:, b, :n], 1.0 / D, EPS, ALU.mult, ALU.add), reads=[pk], writes=[rk])
    cx.op('act', lambda: nc.scalar.activation(rt[:, :n], rt[:, :n], AF.Sqrt), reads=[rk], writes=[rk])
    cx.op('dve', lambda: nc.vector.reciprocal(rt[:, :n], rt[:, :n]), reads=[rk], writes=[rk])
    for k in range(16):
        xt, xk = xp.get()
        cx.dma('sp', xt[:, :n], xsrc[k * 128:(k + 1) * 128, xcol:xcol + n], writes=[xk])
        sq, sk = sqp.get()
        cx.op('dve', lambda: nc.vector.scalar_tensor_tensor(sq[:, :n], xt[:, :n], A[:, k, c:c + 1], rt[:, :n], ALU.mult, ALU.mult),
              reads=[xk, rk, 'modA'], writes=[sk])
        dst, dkey = dst_fn(k)
        if hf_cb is None:
            cx.op('act', lambda: nc.scalar.activation(dst, sq[:, :n], AF.Identity, bias=Sh[:, k, c:c + 1]),
                  reads=[sk, 'modA'], writes=[dkey])
        else:
            cx.op('act', lambda: nc.scalar.activation(sq[:, :n], sq[:, :n], AF.Identity, bias=Sh[:, k, c:c + 1]),
                  reads=[sk, 'modA'], writes=[sk])
            cx.op('dve', lambda: nc.vector.tensor_copy(dst, sq[:, :n]), reads=[sk], writes=[dkey])
            hf_cb(k, n, sq, sk)


def norm_pools(P, pes):
    nc, cx = P.nc, P.cx
    return (Pool(nc, pes, cx.key("rx"), [128, 512], F32, 4), Pool(nc, pes, cx.key("rsq"), [128, 512], F32, 4),
            Pool(nc, pes, cx.key("rr"), [128, 512], F32, 2))


def phase_modvec(P, cT_d, wmod_d, bmodT_d, mod, pes):
    cx, nc = P.cx, P.nc
    cf = pes.enter_context(nc.sbuf_tensor(cx.key("cf"), [128, 16, 2], F32))
    cb = pes.enter_context(nc.sbuf_tensor(cx.key("cb"), [128, 16, 2], BF16))
    bm = pes.enter_context(nc.sbuf_tensor(cx.key("bm"), [128, 96], F32))
    cx.dma('sp', cf[:], cT_d, writes=['cf'])
    cx.dma('sp', bm[:], bmodT_d, writes=['bm'])
    cx.op('act', lambda: nc.scalar.activation(cb[:], cf[:], AF.Silu), reads=['cf'], writes=['cb'])
    b = cx.bank()
    pk = f"ps{b}"
    for g in range(24):
        wt, wkey = P.load_w(wmod_d, 0, 16, g * 512, 512)
        for m in range(4):
            f = g * 4 + m
            fns = [(lambda k=k: nc.tensor.matmul(P.ps[:, b, 2 * f:2 * f + 2], wt[:, k, m * 128:(m + 1) * 128], cb[:, k, :],
                                                 start=(k == 0), stop=(k == 15))) for k in range(16)]
            cx.mm(fns, reads=['cb', wkey], writes=[pk], acc=(f > 0))
    pv = P.ps[:, b, 0:192].rearrange("p (f c) -> p f c", c=2)
    for c in range(2):
        cx.op('dve', lambda: nc.vector.tensor_tensor(mod[:, :, c], pv[:, :, c], bm[:, :], ALU.add), reads=[pk, 'bm'], writes=['mod'])


def make_A(P, mod, gT, A, sc0, gcol):
    cx, nc = P.cx, P.nc
    for c in range(2):
        cx.op('dve', lambda: nc.vector.scalar_tensor_tensor(A[:, :, c], mod[:, sc0:sc0 + 16, c], 1.0, gT[:, gcol:gcol + 16],
                                                            ALU.add, ALU.mult), reads=['mod', 'gT'], writes=['modA'])


def phase_norm1(P, pes, l, XB, HT, A, Sh):
    cx, nc = P.cx, P.nc
    pools = norm_pools(P, pes)
    stg = Pool(nc, pes, cx.key("n1s"), [128, 16, 512], BF16, 2)
    nkv = NKVR[l] * GW
    tl = [(256 * l + c0, c0, n, 0) for (c0, n, _) in kv_tiles(l)] + [(XW, nkv, CL, 1)]
    for (xcol, hcol, n, c) in tl:
        st, sk = stg.get()
        rms_norm_tile(P, pools, XB, xcol, n, c, A, Sh, lambda k: (st[:, k, :n], sk))
        cx.dma('sp', HT[:, hcol:hcol + n].rearrange("(k p) t -> p k t", p=128), st[:, :, :n], reads=[sk])


def phase_inproj(P, pes, l, HT, win, lw, cos_d, sin_d, tval_d, B):
    cx, nc = P.cx, P.nc
    nq, nkv = NQR[l] * GW, NKVR[l] * GW
    nak_ctx = nkv + 2 * NAM
    hT = pes.enter_context(nc.sbuf_tensor(cx.key("hT"), [128, 16, 2048], BF16))
    sbp = Pool(nc, pes, cx.key("stb"), [128, 512], BF16, 4)
    sfp = Pool(nc, pes, cx.key("stf"), [128, 512], F32, 3)
    tmpf = Pool(nc, pes, cx.key("tmf"), [128, 512], F32, 4)
    tabp = Pool(nc, pes, cx.key("tab"), [128, 512], F32, 4)
    wperm = pes.enter_context(nc.sbuf_tensor(cx.key("wperm"), [128, 8192], BF16))
    tiles = [(c0, n, kind) for (c0, n, kind) in kv_tiles(l)] + [(nkv, CL, 'c')]
    chunks, cur, tot = [], [], 0
    for t in tiles:
        if tot + t[1] > 2048:
            chunks.append(cur)
            cur, tot = [], 0
        cur.append((tot,) + t)
        tot += t[1]
    chunks.append(cur)
    cnt = [0]
    for ch in chunks:
        ctot = sum(t[2] for t in ch)
        h0 = ch[0][1]
        cx.dma('sp', hT[:, :, :ctot], HT[:, h0:h0 + ctot].rearrange("(k p) t -> p k t", p=128), writes=['hT'])
        all_t = ch
        oc_t = [t for t in ch if t[3] in ('o', 'c')]

        def mm16(b, wt, m, hc, n):
            fns = [(lambda k=k: nc.tensor.matmul(P.ps[:, b, :n], wt[:, k, m * 128:(m + 1) * 128], hT[:, k, hc:hc + n],
                                                 start=(k == 0), stop=(k == 15))) for k in range(16)]
            return fns

        def plain(dst, c0, ncols, tl, colfn, sig=False, f32=False):
            if not tl:
                return
            for gc in range(0, ncols, 512):
                gn = min(512, ncols - gc)
                wt, wkey = P.load_w(win, lw, 16, c0 + gc, gn)
                for m in range(gn // 128):
                    mi = gc // 128 + m
                    for (hc, kvc, n, kind) in tl:
                        b = cx.bank()
                        cx.mm(mm16(b, wt, m, hc, n), reads=['hT', wkey], writes=[f"ps{b}"])
                        buf, key = (sfp if f32 else sbp).get()
                        cnt[0] += 1
                        if sig:
                            cx.op('act', lambda: nc.scalar.activation(buf[:, :n], P.ps[:, b, :n], AF.Sigmoid), reads=[f"ps{b}"], writes=[key])
                        else:
                            alt_copy(P, cnt[0], buf[:, :n], P.ps[:, b, :n], [f"ps{b}"], [key])
                        dc = colfn(kvc, kind)
                        cx.dma('sp', dst[mi * 128:(mi + 1) * 128, dc:dc + n], buf[:, :n], reads=[key])

        def vsec(dst, c0, ncols, rowfn):
            for gc in range(0, ncols, 512):
                gn = min(512, ncols - gc)
                wt, wkey = P.load_w(win, lw, 16, c0 + gc, gn)
                for (hc, kvc, n, kind) in all_t:
                    r0 = rowfn(kvc, kind)
                    for tt in range(n // 128):
                        b = cx.bank()
                        fns = [(lambda k=k: nc.tensor.matmul(P.ps[:, b, :gn], hT[:, k, hc + tt * 128:hc + (tt + 1) * 128], wt[:, k, :],
                                                             start=(k == 0), stop=(k == 15))) for k in range(16)]
                        cx.mm(fns, reads=['hT', wkey], writes=[f"ps{b}"])
                        buf, key = sbp.get()
                        cnt[0] += 1
                        alt_copy(P, cnt[0], buf[:, :gn], P.ps[:, b, :gn], [f"ps{b}"], [key])
                        cx.dma('sp', dst[r0 + tt * 128:r0 + (tt + 1) * 128, gc:gc + gn], buf[:, :gn], reads=[key])

        def rope(dst, c0, ncols, tl, colfn):
            if not tl:
                return
            for gc in range(0, ncols, 512):
                gn = min(512, ncols - gc)
                wt, wkey = P.load_w(win, lw, 16, c0 + gc, gn)
                wv = wt.rearrange("p k (h j f) -> p k h j f", j=2, f=32)
                pv = wperm[:, 0:16 * gn].rearrange("p (k c) -> p k c", k=16)
                pvv = pv.rearrange("p k (h j f) -> p k h j f", j=2, f=32)
                for j in range(2):
                    cx.op('pool', lambda: nc.gpsimd.tensor_copy(pvv[:, :, :, j, :], wv[:, :, :, 1 - j, :]), reads=[wkey], writes=['wperm'])
                for (hc, kvc, n, kind) in tl:
                    if kind != 'c':
                        ct, ck = tabp.get()
                        st_, stk = tabp.get()
                        wc = 256 * l + kvc
                        cx.dma('sp', ct[:, :n], cos_d[:, wc:wc + n], writes=[ck])
                        cx.dma('sp', st_[:, :n], sin_d[:, wc:wc + n], writes=[stk])
                    for m in range(gn // 128):
                        mi = gc // 128 + m
                        buf, key = sbp.get()
                        b1 = cx.bank()
                        cx.mm(mm16(b1, wt, m, hc, n), reads=['hT', wkey], writes=[f"ps{b1}"])
                        if kind != 'c':
                            b2 = cx.bank()
                            cx.mm(mm16(b2, pv, m, hc, n), reads=['hT', 'wperm'], writes=[f"ps{b2}"])
                            t1, k1 = tmpf.get()
                            t2, k2 = tmpf.get()
                            cx.op('dve', lambda: nc.vector.tensor_tensor(t1[:, :n], P.ps[:, b1, :n], ct[:, :n], ALU.mult),
                                  reads=[f"ps{b1}", ck], writes=[k1])
                            cx.op('dve', lambda: nc.vector.tensor_tensor(t2[:, :n], P.ps[:, b2, :n], st_[:, :n], ALU.mult),
                                  reads=[f"ps{b2}", stk], writes=[k2])
                            cx.op('pool', lambda: nc.gpsimd.tensor_tensor(buf[:, :n], t1[:, :n], t2[:, :n], ALU.add),
                                  reads=[k1, k2], writes=[key])
                        else:
                            cx.op('act', lambda: nc.scalar.activation(buf[:, :n], P.ps[:, b1, :n], AF.Copy), reads=[f"ps{b1}"], writes=[key])
                        dc = colfn(kvc, kind)
                        cx.dma('sp', dst[mi * 128:(mi + 1) * 128, dc:dc + n], buf[:, :n], reads=[key])

        def glu():
            for gc in range(0, 1024, 512):
                wa, ka = P.load_w(win, lw, 16, 4608 + gc, 512)
                wg, kg = P.load_w(win, lw, 16, 5632 + gc, 512)
                for (hc, kvc, n, kind) in all_t:
                    if kind != 'c':
                        tv, tvk = tabp.get()
                        wc = 256 * l + kvc
                        cx.dma('sp', tv[:, :n], tval_d[:, wc:wc + n], writes=[tvk])
                    for m in range(4):
                        mi = gc // 128 + m
                        b1, b2 = cx.bank(), cx.bank()
                        cx.mm(mm16(b1, wa, m, hc, n), reads=['hT', ka], writes=[f"ps{b1}"])
                        cx.mm(mm16(b2, wg, m, hc, n), reads=['hT', kg], writes=[f"ps{b2}"])
                        t1, k1 = tmpf.get()
                        buf, key = sfp.get()
                        cx.op('act', lambda: nc.scalar.activation(t1[:, :n], P.ps[:, b2, :n], AF.Sigmoid), reads=[f"ps{b2}"], writes=[k1])
                        if kind != 'c':
                            cx.op('dve', lambda: nc.vector.tensor_tensor(t1[:, :n], P.ps[:, b1, :n], t1[:, :n], ALU.mult),
                                  reads=[f"ps{b1}", k1], writes=[k1])
                            cx.op('pool', lambda: nc.gpsimd.tensor_tensor(buf[:, :n], t1[:, :n], tv[:, :n], ALU.mult),
                                  reads=[k1, tvk], writes=[key])
                        else:
                            cx.op('dve', lambda: nc.vector.tensor_tensor(buf[:, :n], P.ps[:, b1, :n], t1[:, :n], ALU.mult),
                                  reads=[f"ps{b1}", k1], writes=[key])
                        dc = kvc
                        cx.dma('sp', B['ZT'][mi * 128:(mi + 1) * 128, dc:dc + n], buf[:, :n], reads=[key])

        qcol = lambda kvc, kind: (nq if kind == 'c' else kvc - 256)
        plain(B['NAQ'], 0, 1024, oc_t, qcol)
        plain(B['NAK'], 1024, 1024, all_t, lambda kvc, kind: (nak_ctx if kind == 'c' else NAM + kvc))
        vsec(B['NAV'], 2048, 1024, lambda kvc, kind: (nak_ctx if kind == 'c' else NAM + kvc))
        rope(B['WAQ'], 3072, 1024, oc_t, qcol)
        rope(B['WAK'], 4096, 256, all_t, lambda kvc, kind: kvc)
        vsec(B['WAV'], 4352, 256, lambda kvc, kind: kvc)
        glu()
        plain(B['GST'], 6656, 6144, oc_t, qcol, sig=True, f32=True)


def phase_na(P, pes, l, B, ttab_h, maskL_d, rsel_d):
    cx, nc = P.cx, P.nc
    nq, nkv = NQR[l] * GW, NKVR[l] * GW
    kw = nkv + 2 * NAM
    nvt = kw // 128
    nqb = NQR[l] // 8
    kp = Pool(nc, pes, cx.key("nak"), [128, kw + CL], BF16, 2)
    qp = Pool(nc, pes, cx.key("naq"), [128, nq + CL], BF16, 2)
    vp = Pool(nc, pes, cx.key("nav"), [128, nvt + 2, 128], BF16, 2)
    tp = Pool(nc, pes, cx.key("ntt"), [128, NIDX * 64], F32, 2)
    op_ = Pool(nc, pes, cx.key("nao"), [128, nq + CL], BF16, 2)
    sp = Pool(nc, pes, cx.key("nas"), [128, 512], F32, 3)
    pp = Pool(nc, pes, cx.key("nap"), [128, 512], BF16, 4)
    rp = Pool(nc, pes, cx.key("nar"), [128, 512], F32, 2)
    mL = pes.enter_context(nc.sbuf_tensor(cx.key("namL"), [8, nqb * NKT * 128], BF16))
    rs = pes.enter_context(nc.sbuf_tensor(cx.key("nars"), [8, 512], BF16))
    cx.dma('sp', mL[:], maskL_d, writes=['namL'])
    cx.dma('sp', rs[:], rsel_d, writes=['nars'])
    PO, PD = 6, 7

    def attend(q_ap, n, tiles, kT, kk, V, vk, Tt, tk, qkey, obuf, okey, ocol):
        first = True
        for ti, tl in enumerate(tiles):
            last = ti == len(tiles) - 1
            b = cx.bank(0, 6)
            pk = f"ps{b}"
            pT, pkey = pp.get()
            if tl[0] == 'ctx':
                i = tl[1]
                kcol = kw + i * 128
                vt = nvt + i
                cx.mm([lambda: nc.tensor.matmul(P.ps[:, b, :n], kT[:, kcol:kcol + 128], q_ap, start=True, stop=True)],
                      reads=[kk, qkey], writes=[pk])
                cx.op('act', lambda: nc.scalar.activation(pT[:, :n], P.ps[:, b, :n], AF.Exp, scale=SCALE), reads=[pk], writes=[pkey])
            else:
                t, qb = tl[1], tl[2]
                vt = 4 * qb + t
                kcol = vt * 128
                mcol = (qb * NKT + t) * 128
                cx.mm([lambda: nc.tensor.matmul(P.ps[:, b, :n], kT[:, kcol:kcol + 128], q_ap, start=True, stop=False),
                       lambda: nc.tensor.matmul(P.ps[:, b, :n], mL[:, mcol:mcol + 128], rs[:, :n], start=False, stop=True)],
                      reads=[kk, qkey, 'namL', 'nars'], writes=[pk])
                sT, sk = sp.get()
                i0 = (20 - 2 * t) * 64
                cx.op('dve', lambda: nc.vector.scalar_tensor_tensor(sT[:, :n], P.ps[:, b, :n], SCALE, Tt[:, i0:i0 + 512],
                                                                    ALU.mult, ALU.add), reads=[pk, tk], writes=[sk])
                cx.op('act', lambda: nc.scalar.activation(pT[:, :n], sT[:, :n], AF.Exp), reads=[sk], writes=[pkey])
            cx.mm([lambda: nc.tensor.matmul(P.ps[:, PO, :n], V[:, vt, :], pT[:, :n], start=first, stop=last)],
                  reads=[vk, pkey], writes=[f"ps{PO}"], acc=not first)
            cx.mm([lambda: nc.tensor.matmul(P.ps[:, PD, :n], P.ones_b[:], pT[:, :n], start=first, stop=last)],
                  reads=['ones_b', pkey], writes=[f"ps{PD}"], acc=not first)
            first = False
        rd, rk = rp.get()
        cx.op('dve', lambda: nc.vector.reciprocal(rd[:, :n], P.ps[:, PD, :n]), reads=[f"ps{PD}"], writes=[rk])
        cx.op('dve', lambda: nc.vector.tensor_tensor(obuf[:, ocol:ocol + n], P.ps[:, PO, :n], rd[:, :n], ALU.mult),
              reads=[f"ps{PO}", rk], writes=[okey])

    for h in range(8):
        kT, kk = kp.get()
        qT, qk = qp.get()
        V, vk = vp.get()
        Tt, tk = tp.get()
        ob, ok = op_.get()
        cx.dma('sp', kT[:], B['NAK'][h * 128:(h + 1) * 128, 0:kw + CL], writes=[kk])
        cx.dma('sp', qT[:], B['NAQ'][h * 128:(h + 1) * 128, 0:nq + CL], writes=[qk])
        cx.dma('sp', V[:], B['NAV'][0:kw + CL, h * 128:(h + 1) * 128].rearrange("(t p) d -> p t d", p=128), writes=[vk])
        cx.dma('sp', Tt[:], ttab_h(h), writes=[tk])
        for qb in range(nqb):
            tiles = [('ctx', 0), ('ctx', 1)] + [('lat', t, qb) for t in range(NKT)]
            attend(qT[:, qb * 512:(qb + 1) * 512], 512, tiles, kT, kk, V, vk, Tt, tk, qk, ob, ok, qb * 512)
        attend(qT[:, nq:nq + CL], CL, [('ctx', 0), ('ctx', 1)], kT, kk, V, vk, Tt, tk, qk, ob, ok, nq)
        cx.dma('sp', B['ONA'][h * 128:(h + 1) * 128, 0:nq + CL], ob[:], reads=[ok])


def phase_wa(P, pes, l, B, m3_d, kvb_d, sinkb_d):
    cx, nc = P.cx, P.nc
    nq, nkv = NQR[l] * GW, NKVR[l] * GW
    nkt = nkv // 128
    kp = Pool(nc, pes, cx.key("wak"), [128, nkv + CL], BF16, 2)
    qp = Pool(nc, pes, cx.key("waq"), [128, nq + CL], BF16, 2)
    vp = Pool(nc, pes, cx.key("wav"), [128, nkt + 2, 128], BF16, 2)
    op_ = Pool(nc, pes, cx.key("wao"), [128, nq + CL], BF16, 2)
    sp = Pool(nc, pes, cx.key("was"), [128, 384], F32, 3)
    pp = Pool(nc, pes, cx.key("wap"), [128, 512], BF16, 4)
    rp = Pool(nc, pes, cx.key("war"), [128, 512], F32, 2)
    m3 = pes.enter_context(nc.sbuf_tensor(cx.key("wam3"), [128, 384], F32))
    kvb = pes.enter_context(nc.sbuf_tensor(cx.key("wakvb"), [128, nkt], F32))
    esink = pes.enter_context(nc.sbuf_tensor(cx.key("waes"), [128, 8], F32))
    cx.dma('sp', m3[:], m3_d, writes=['wam3'])
    cx.dma('sp', kvb[:], kvb_d, writes=['waed'])
    cx.dma('sp', esink[:], sinkb_d, writes=['waes'])
    cx.op('act', lambda: nc.scalar.activation(esink[:], esink[:], AF.Exp), reads=['waes'], writes=['waes'])
    PO, PD = 6, 7

    def finalize(n, h, obuf, okey, ocol):
        rd, rk = rp.get()
        cx.op('dve', lambda: nc.vector.tensor_scalar(rd[:, :n], P.ps[:, PD, :n], esink[:, h:h + 1], None, ALU.add),
              reads=[f"ps{PD}", 'waes'], writes=[rk])
        cx.op('dve', lambda: nc.vector.reciprocal(rd[:, :n], rd[:, :n]), reads=[rk], writes=[rk])
        cx.op('dve', lambda: nc.vector.tensor_tensor(obuf[:, ocol:ocol + n], P.ps[:, PO, :n], rd[:, :n], ALU.mult),
              reads=[f"ps{PO}", rk], writes=[okey])

    def ctx_tiles(q_ap, n, kT, kk, V, vk, qk, last_i):
        for i in range(2):
            b = cx.bank(0, 6)
            pk = f"ps{b}"
            pT, pkey = pp.get()
            kcol = nkv + i * 128
            cx.mm([lambda: nc.tensor.matmul(P.ps[:, b, :n], kT[:, kcol:kcol + 128], q_ap, start=True, stop=True)],
                  reads=[kk, qk], writes=[pk])
            cx.op('act', lambda: nc.scalar.activation(pT[:, :n], P.ps[:, b, :n], AF.Exp, scale=SCALE), reads=[pk], writes=[pkey])
            lst = (i == 1) and last_i
            cx.mm([lambda: nc.tensor.matmul(P.ps[:, PO, :n], V[:, nkt + i, :], pT[:, :n], start=(i == 0), stop=lst)],
                  reads=[vk, pkey], writes=[f"ps{PO}"], acc=(i > 0))
            cx.mm([lambda: nc.tensor.matmul(P.ps[:, PD, :n], P.ones_b[:], pT[:, :n], start=(i == 0), stop=lst)],
                  reads=['ones_b', pkey], writes=[f"ps{PD}"], acc=(i > 0))

    for g in range(2):
        kT, kk = kp.get()
        V, vk = vp.get()
        cx.dma('sp', kT[:], B['WAK'][g * 128:(g + 1) * 128, 0:nkv + CL], writes=[kk])
        cx.dma('sp', V[:], B['WAV'][0:nkv + CL, g * 128:(g + 1) * 128].rearrange("(t p) d -> p t d", p=128), writes=[vk])
        for hh in range(4):
            h = 4 * g + hh
            qT, qk = qp.get()
            ob, ok = op_.get()
            cx.dma('sp', qT[:], B['WAQ'][h * 128:(h + 1) * 128, 0:nq + CL], writes=[qk])
            for Q in range(nq // 512):
                ctx_tiles(qT[:, Q * 512:(Q + 1) * 512], 512, kT, kk, V, vk, qk, False)
                for jj in range(4 * Q + 1, 4 * Q + 7):
                    j = jj - 2
                    qlo, qhi = max(j - 1, 4 * Q), min(j + 1, 4 * Q + 3)
                    n = (qhi - qlo + 1) * 128
                    c0 = (qlo - 4 * Q) * 128
                    mc0 = (qlo - j + 1) * 128
                    b = cx.bank(0, 6)
                    pk = f"ps{b}"
                    q_ap = qT[:, Q * 512 + c0:Q * 512 + c0 + n]
                    cx.mm([lambda: nc.tensor.matmul(P.ps[:, b, :n], kT[:, jj * 128:(jj + 1) * 128], q_ap, start=True, stop=True)],
                          reads=[kk, qk], writes=[pk])
                    sT, sk = sp.get()
                    cx.op('dve', lambda: nc.vector.tensor_tensor(sT[:, :n], P.ps[:, b, :n], m3[:, mc0:mc0 + n], ALU.add),
                          reads=[pk, 'wam3'], writes=[sk])
                    pT, pkey = pp.get()
                    cx.op('act', lambda: nc.scalar.activation(pT[:, :n], sT[:, :n], AF.Exp, bias=kvb[:, jj:jj + 1], scale=SCALE),
                          reads=[sk, 'waed'], writes=[pkey])
                    lst = jj == 4 * Q + 6
                    cx.mm([lambda: nc.tensor.matmul(P.ps[:, PO, c0:c0 + n], V[:, jj, :], pT[:, :n], start=False, stop=lst)],
                          reads=[vk, pkey], writes=[f"ps{PO}"], acc=True)
                    cx.mm([lambda: nc.tensor.matmul(P.ps[:, PD, c0:c0 + n], P.ones_b[:], pT[:, :n], start=False, stop=lst)],
                          reads=['ones_b', pkey], writes=[f"ps{PD}"], acc=True)
                finalize(512, h, ob, ok, Q * 512)
            ctx_tiles(qT[:, nq:nq + CL], CL, kT, kk, V, vk, qk, True)
            finalize(CL, h, ob, ok, nq)
            cx.dma('sp', B['OWA'][h * 128:(h + 1) * 128, 0:nq + CL], ob[:], reads=[ok])


def phase_conv(P, pes, l, B, cwT_d, cvb_d, lng_d, lnb_d):
    cx, nc = P.cx, P.nc
    nq, nkv = NQR[l] * GW, NKVR[l] * GW
    SEG = 2048
    co = pes.enter_context(nc.sbuf_tensor(cx.key("cvo"), [128, 8, SEG], F32))
    zp = Pool(nc, pes, cx.key("cvz"), [128, SEG + 2 * CV_HALO], F32, 2)
    cw = pes.enter_context(nc.sbuf_tensor(cx.key("cvw"), [128, 8, 31], F32))
    cb = pes.enter_context(nc.sbuf_tensor(cx.key("cvb"), [128, 8], F32))
    lg = pes.enter_context(nc.sbuf_tensor(cx.key("cvg"), [128, 8], F32))
    lb = pes.enter_context(nc.sbuf_tensor(cx.key("cvlb"), [128, 8], F32))
    sqp = Pool(nc, pes, cx.key("cvs"), [128, 512], F32, 3)
    stp = Pool(nc, pes, cx.key("cvt"), [128, 512], F32, 6)
    obp = Pool(nc, pes, cx.key("cvob"), [128, 512], BF16, 4)
    cx.dma('sp', cw[:], cwT_d, writes=['cvw'])
    cx.dma('sp', cb[:], cvb_d, writes=['cvw'])
    cx.dma('sp', lg[:], lng_d, writes=['cvw'])
    cx.dma('sp', lb[:], lnb_d, writes=['cvw'])
    segs = [(s0, min(SEG, nq - s0), False) for s0 in range(0, nq, SEG)] + [(nq, CL, True)]
    for si, (s0, sn, isc) in enumerate(segs):
        for j in range(8):
            e = 'dve'
            E = nc.vector
            z, zk = zp.get()
            if isc:
                cx.op(e, lambda: E.memset(z[:, 0:CV_HALO], 0.0), writes=[zk])
                cx.op(e, lambda: E.memset(z[:, CV_HALO + CL:2 * CV_HALO + CL], 0.0), writes=[zk])
                cx.dma('sp', z[:, CV_HALO:CV_HALO + CL], B['ZT'][j * 128:(j + 1) * 128, nkv:nkv + CL], writes=[zk])
            else:
                zc = 256 + s0 - CV_HALO
                cx.dma('sp', z[:, 0:sn + 2 * CV_HALO], B['ZT'][j * 128:(j + 1) * 128, zc:zc + sn + 2 * CV_HALO], writes=[zk])
            acc = co[:, j, 0:sn]
            ck = f"cvo{j}"
            cx.op(e, lambda: E.tensor_scalar(acc, z[:, 0:sn], cw[:, j, 0:1], cb[:, j:j + 1], ALU.mult, ALU.add),
                  reads=[zk, 'cvw'], writes=[ck])
            for tap in range(1, 31):
                cx.op(e, lambda: E.scalar_tensor_tensor(acc, z[:, tap:tap + sn], cw[:, j, tap:tap + 1], acc,
                                                        ALU.mult, ALU.add), reads=[zk, 'cvw', ck], writes=[ck])
        for t0 in range(0, sn, 512):
            tn = min(512, sn - t0)
            bmu, bsq = cx.bank(), cx.bank()
            for j in range(8):
                ck = f"cvo{j}"
                cx.mm([lambda: nc.tensor.matmul(P.ps[:, bmu, :tn], P.ones_f[:], co[:, j, t0:t0 + tn], start=(j == 0), stop=(j == 7))],
                      reads=[ck, 'ones_f'], writes=[f"ps{bmu}"], acc=(j > 0))
                sq, sk = sqp.get()
                cx.op('act', lambda: nc.scalar.activation(sq[:, :tn], co[:, j, t0:t0 + tn], AF.Square), reads=[ck], writes=[sk])
                cx.mm([lambda: nc.tensor.matmul(P.ps[:, bsq, :tn], P.ones_f[:], sq[:, :tn], start=(j == 0), stop=(j == 7))],
                      reads=[sk, 'ones_f'], writes=[f"ps{bsq}"], acc=(j > 0))
            mu, mk = stp.get()
            ms, msk = stp.get()
            rs, rk = stp.get()
            cx.op('dve', lambda: nc.vector.tensor_scalar(mu[:, :tn], P.ps[:, bmu, :tn], 1.0 / 1024, None, ALU.mult), reads=[f"ps{bmu}"], writes=[mk])
            cx.op('dve', lambda: nc.vector.tensor_tensor(ms[:, :tn], mu[:, :tn], mu[:, :tn], ALU.mult), reads=[mk], writes=[msk])
            cx.op('dve', lambda: nc.vector.scalar_tensor_tensor(rs[:, :tn], P.ps[:, bsq, :tn], 1.0 / 1024, ms[:, :tn], ALU.mult, ALU.subtract),
                  reads=[f"ps{bsq}", msk], writes=[rk])
            cx.op('dve', lambda: nc.vector.tensor_scalar(rs[:, :tn], rs[:, :tn], EPS, None, ALU.add), reads=[rk], writes=[rk])
            cx.op('act', lambda: nc.scalar.activation(rs[:, :tn], rs[:, :tn], AF.Sqrt), reads=[rk], writes=[rk])
            cx.op('dve', lambda: nc.vector.reciprocal(rs[:, :tn], rs[:, :tn]), reads=[rk], writes=[rk])
            for j in range(8):
                ck = f"cvo{j}"
                t1, k1 = sqp.get()
                cx.op('dve', lambda: nc.vector.tensor_tensor(t1[:, :tn], co[:, j, t0:t0 + tn], mu[:, :tn], ALU.subtract), reads=[ck, mk], writes=[k1])
                cx.op('pool', lambda: nc.gpsimd.tensor_tensor(t1[:, :tn], t1[:, :tn], rs[:, :tn], ALU.mult), reads=[k1, rk], writes=[k1])
                ob, obk = obp.get()
                cx.op('act', lambda: nc.scalar.activation(ob[:, :tn], t1[:, :tn], AF.Silu, bias=lb[:, j:j + 1], scale=lg[:, j:j + 1]),
                      reads=[k1, 'cvw'], writes=[obk])
                cx.dma('sp', B['OCV'][j * 128:(j + 1) * 128, s0 + t0:s0 + t0 + tn], ob[:, :tn], reads=[obk])


def phase_merge(P, pes, l, B, XB, wbr, wbr_r0, wout, wout_r0, mod):
    cx, nc = P.cx, P.nc
    otp = Pool(nc, pes, cx.key("mo"), [128, 24, 512], BF16, 2)
    ytp = Pool(nc, pes, cx.key("my"), [128, 16, 512], BF16, 2)
    gtp = Pool(nc, pes, cx.key("mg"), [128, 3, 512], F32, 3)
    tmp = Pool(nc, pes, cx.key("mt"), [128, 512], F32, 6)
    xp = Pool(nc, pes, cx.key("mx"), [128, 512], F32, 4)
    gs_v = B['GST'].rearrange("(i m p) t -> p i m t", i=3, p=128)
    srcs = (B['ONA'], B['OCV'], B['OWA'])
    for (xcol, t0, tn, isc) in o_tiles(l):
        c = 1 if isc else 0
        ot, otk = otp.get()
        for i in range(3):
            cx.dma('sp', ot[:, i * 8:(i + 1) * 8, :tn], srcs[i][:, t0:t0 + tn].rearrange("(k p) t -> p k t", p=128), writes=[otk])
        yT, yk = ytp.get()
        for mg in range(4):
            wts = [P.load_w(wbr, wbr_r0 + i * 1024, 8, mg * 512, 512) for i in range(3)]
            for m in range(4):
                mi = mg * 4 + m
                gt, gk = gtp.get()
                cx.dma('sp', gt[:, :, :tn], gs_v[:, :, mi, t0:t0 + tn], writes=[gk])
                bs = []
                for i in range(3):
                    b = cx.bank()
                    wt, wk = wts[i]
                    fns = [(lambda k=k: nc.tensor.matmul(P.ps[:, b, :tn], wt[:, k, m * 128:(m + 1) * 128], ot[:, i * 8 + k, :tn],
                                                         start=(k == 0), stop=(k == 7))) for k in range(8)]
                    cx.mm(fns, reads=[otk, wk], writes=[f"ps{b}"])
                    bs.append(b)
                ys = []
                for i in range(3):
                    t1, k1 = tmp.get()
                    cx.op('dve', lambda: nc.vector.tensor_tensor(t1[:, :tn], P.ps[:, bs[i], :tn], gt[:, i, :tn], ALU.mult),
                          reads=[f"ps{bs[i]}", gk], writes=[k1])
                    ys.append((t1, k1))
                cx.op('pool', lambda: nc.gpsimd.tensor_tensor(ys[0][0][:, :tn], ys[0][0][:, :tn], ys[1][0][:, :tn], ALU.add),
                      reads=[ys[0][1], ys[1][1]], writes=[ys[0][1]])
                cx.op('pool', lambda: nc.gpsimd.tensor_tensor(yT[:, mi, :tn], ys[0][0][:, :tn], ys[2][0][:, :tn], ALU.add),
                      reads=[ys[0][1], ys[2][1]], writes=[yk])
        for mg in range(4):
            wt, wk = P.load_w(wout, wout_r0, 16, mg * 512, 512)
            for m in range(4):
                mi = mg * 4 + m
                b = cx.bank()
                fns = [(lambda k=k: nc.tensor.matmul(P.ps[:, b, :tn], wt[:, k, m * 128:(m + 1) * 128], yT[:, k, :tn],
                                                     start=(k == 0), stop=(k == 15))) for k in range(16)]
                cx.mm(fns, reads=[yk, wk], writes=[f"ps{b}"])
                xt, xk = xp.get()
                cx.dma('sp', xt[:, :tn], XB[mi * 128:(mi + 1) * 128, xcol:xcol + tn], writes=[xk])
                cx.op('dve', lambda: nc.vector.scalar_tensor_tensor(xt[:, :tn], P.ps[:, b, :tn], mod[:, 32 + mi, c:c + 1], xt[:, :tn],
                                                                    ALU.mult, ALU.add), reads=[f"ps{b}", xk, 'mod'], writes=[xk])
                cx.dma('sp', XB[mi * 128:(mi + 1) * 128, xcol:xcol + tn], xt[:, :tn], reads=[xk])


def ffn_group(P, res, h2, tl, wi, wi_r0, wo, wo_r0, dff, Ge, XB, modg):
    cx, nc = P.cx, P.nc
    nj = dff // 128
    g, gk = res['g'], 'ffg'
    tmp, xp = res['tmp'], res['xp']
    for jg in range(0, nj, 4):
        wa, ka = P.load_w(wi, wi_r0, 16, jg * 128, 512)
        wb, kb = P.load_w(wi, wi_r0, 16, dff + jg * 128, 512)
        for m in range(4):
            j = jg + m
            for (off, xcol, n, c) in tl:
                b1, b2 = cx.bank(), cx.bank()
                for (bb, ww, kk) in ((b1, wa, ka), (b2, wb, kb)):
                    fns = [(lambda k=k: nc.tensor.matmul(P.ps[:, bb, :n], ww[:, k, m * 128:(m + 1) * 128], h2[:, k, off:off + n],
                                                         start=(k == 0), stop=(k == 15))) for k in range(16)]
                    cx.mm(fns, reads=['h2', kk], writes=[f"ps{bb}"])
                t1, k1 = tmp.get()
                cx.op('act', lambda: nc.scalar.activation(t1[:, :n], P.ps[:, b1, :n], AF.Silu), reads=[f"ps{b1}"], writes=[k1])
                if Ge is None:
                    cx.op('dve', lambda: nc.vector.tensor_tensor(g[:, j, off:off + n], t1[:, :n], P.ps[:, b2, :n], ALU.mult),
                          reads=[k1, f"ps{b2}"], writes=[gk])
                else:
                    cx.op('dve', lambda: nc.vector.tensor_tensor(t1[:, :n], t1[:, :n], P.ps[:, b2, :n], ALU.mult),
                          reads=[k1, f"ps{b2}"], writes=[k1])
                    cx.op('pool', lambda: nc.gpsimd.tensor_tensor(g[:, j, off:off + n], t1[:, :n], Ge[0][:, off:off + n], ALU.mult),
                          reads=[k1, Ge[1]], writes=[gk])
    for m in range(16):
        wt, wk = P.load_w(wo, wo_r0, nj, m * 128, 128)
        for (off, xcol, n, c) in tl:
            b = cx.bank()
            fns = [(lambda k=k: nc.tensor.matmul(P.ps[:, b, :n], wt[:, k, :], g[:, k, off:off + n], start=(k == 0), stop=(k == nj - 1)))
                   for k in range(nj)]
            cx.mm(fns, reads=[gk, wk], writes=[f"ps{b}"])
            xt, xk = xp.get()
            dk = f"XB_{m}_{xcol}"
            cx.dma('sp', xt[:, :n], XB[m * 128:(m + 1) * 128, xcol:xcol + n], reads=[dk], writes=[xk])
            cx.op('dve', lambda: nc.vector.scalar_tensor_tensor(xt[:, :n], P.ps[:, b, :n], modg(m, c), xt[:, :n],
                                                                ALU.mult, ALU.add), reads=[f"ps{b}", xk, 'mod'], writes=[xk])
            cx.dma('sp', XB[m * 128:(m + 1) * 128, xcol:xcol + n], xt[:, :n], reads=[xk], writes=[dk])


def phase_ffn(P, pes, l, XB, mod, A2, moe, wi, wi_r0, wo, wo_r0, wr_d=None, ident_d=None, selm_d=None, skip_ctx=False):
    cx, nc = P.cx, P.nc
    TG = 1024
    h2 = pes.enter_context(nc.sbuf_tensor(cx.key("h2"), [128, 16, TG], BF16))
    res = {'g': pes.enter_context(nc.sbuf_tensor(cx.key("ffg"), [128, 32 if moe else 44, TG], BF16)),
           'tmp': Pool(nc, pes, cx.key("fft"), [128, 512], F32, 4),
           'xp': Pool(nc, pes, cx.key("ffx"), [128, 512], F32, 4)}
    pools = (Pool(nc, pes, cx.key("rx"), [128, 512], F32, 3), Pool(nc, pes, cx.key("rsq"), [128, 512], F32, 3),
             Pool(nc, pes, cx.key("rr"), [128, 512], F32, 2))
    if moe:
        wr = pes.enter_context(nc.sbuf_tensor(cx.key("wr"), [128, 16, 8], F32))
        ident = pes.enter_context(nc.sbuf_tensor(cx.key("ident"), [128, 128], F32))
        selm = pes.enter_context(nc.sbuf_tensor(cx.key("selm"), [8, 8 * 128], F32))
        lgT = pes.enter_context(nc.sbuf_tensor(cx.key("lgT"), [8, 512], F32))
        L = pes.enter_context(nc.sbuf_tensor(cx.key("rL"), [128, 4, 8], F32))
        W1 = pes.enter_context(nc.sbuf_tensor(cx.key("rW1"), [128, 4, 8], F32))
        W2 = pes.enter_context(nc.sbuf_tensor(cx.key("rW2"), [128, 4, 8], F32))
        m1 = pes.enter_context(nc.sbuf_tensor(cx.key("rm1"), [128, 4], F32))
        m2 = pes.enter_context(nc.sbuf_tensor(cx.key("rm2"), [128, 4], F32))
        gT = pes.enter_context(nc.sbuf_tensor(cx.key("rgT"), [8, TG], F32))
        Gp = Pool(nc, pes, cx.key("rG"), [128, TG], F32, 2)
        cx.dma('sp', wr[:], wr_d.rearrange("(k p) e -> p k e", p=128), writes=['wr'])
        cx.dma('sp', ident[:], ident_d, writes=['ident'])
        cx.dma('sp', selm[:], selm_d, writes=['selm'])
    tiles = [(xcol, n, 1 if isc else 0) for (xcol, _, n, isc) in o_tiles(l) if not (isc and skip_ctx)]
    groups, cur, tot = [], [], 0
    for (xcol, n, c) in tiles:
        if tot + n > TG:
            groups.append(cur)
            cur, tot = [], 0
        cur.append((tot, xcol, n, c))
        tot += n
    groups.append(cur)
    modg = lambda m, c: mod[:, 80 + m, c:c + 1]
    for tl in groups:
        for (off, xcol, tn, c) in tl:
            nb = tn // 128
            hf_cb = None
            if moe:
                br = cx.bank()

                def hf_cb(k, n, hf, hk):
                    cx.mm([lambda: nc.tensor.matmul(P.ps[0:8, br, :n], wr[:, k, :], hf[:, :n], start=(k == 0), stop=(k == 15))],
                          reads=[hk, 'wr'], writes=[f"ps{br}"], acc=(k > 0))
            rms_norm_tile(P, pools, XB, xcol, tn, c, A2, mod[:, 48:64, :], lambda k: (h2[:, k, off:off + tn], 'h2'), hf_cb=hf_cb)
            if moe:
                cx.op('dve', lambda: nc.vector.tensor_copy(lgT[:, :tn], P.ps[0:8, br, :tn]), reads=[f"ps{br}"], writes=['lgT'])
                bt = cx.bank()
                for blk in range(nb):
                    cx.mm([lambda: nc.tensor.matmul(P.ps[:, bt, blk * 8:(blk + 1) * 8], lgT[:, blk * 128:(blk + 1) * 128], ident[0:8, 0:8],
                                                    start=True, stop=True)], reads=['lgT', 'ident'], writes=[f"ps{bt}"], acc=(blk > 0))
                Lv, W1v, W2v = L[:, :nb, :], W1[:, :nb, :], W2[:, :nb, :]
                cx.op('dve', lambda: nc.vector.tensor_copy(Lv, P.ps[:, bt, 0:nb * 8].rearrange("p (b e) -> p b e", e=8)),
                      reads=[f"ps{bt}"], writes=['rL'])
                cx.op('dve', lambda: nc.vector.tensor_reduce(m1[:, :nb], Lv, AX.X, ALU.max), reads=['rL'], writes=['rm1'])
                for blk in range(nb):
                    cx.op('dve', lambda: nc.vector.tensor_scalar(W1[:, blk, :], L[:, blk, :], m1[:, blk:blk + 1], None, ALU.is_equal),
                          reads=['rL', 'rm1'], writes=['rW1'])
                cx.op('dve', lambda: nc.vector.scalar_tensor_tensor(W2v, W1v, NEG, Lv, ALU.mult, ALU.add), reads=['rW1', 'rL'], writes=['rW2'])
                cx.op('dve', lambda: nc.vector.tensor_reduce(m2[:, :nb], W2v, AX.X, ALU.max), reads=['rW2'], writes=['rm2'])
                for blk in range(nb):
                    cx.op('dve', lambda: nc.vector.tensor_scalar(W1[:, blk, :], L[:, blk, :], m2[:, blk:blk + 1], None, ALU.is_ge),
                          reads=['rL', 'rm2', 'rW1'], writes=['rW1'])
                    cx.op('dve', lambda: nc.vector.tensor_scalar(W2[:, blk, :], L[:, blk, :], m1[:, blk:blk + 1], None, ALU.subtract),
                          reads=['rL', 'rm1', 'rW2'], writes=['rW2'])
                cx.op('act', lambda: nc.scalar.activation(W2v, W2v, AF.Exp), reads=['rW2'], writes=['rW2'])
                cx.op('dve', lambda: nc.vector.tensor_tensor(W2v, W2v, W1v, ALU.mult), reads=['rW2', 'rW1'], writes=['rW2'])
                cx.op('dve', lambda: nc.vector.tensor_reduce(m1[:, :nb], W2v, AX.X, ALU.add), reads=['rW2', 'rm1'], writes=['rm1'])
                cx.op('dve', lambda: nc.vector.reciprocal(m1[:, :nb], m1[:, :nb]), reads=['rm1'], writes=['rm1'])
                for blk in range(nb):
                    cx.op('dve', lambda: nc.vector.tensor_scalar(W2[:, blk, :], W2[:, blk, :], m1[:, blk:blk + 1], None, ALU.mult),
                          reads=['rW2', 'rm1'], writes=['rW2'])
                bg = cx.bank()
                for blk in range(nb):
                    cx.mm([lambda: nc.tensor.matmul(P.ps[0:8, bg, blk * 128:(blk + 1) * 128], W2[:, blk, :], ident[:, :], start=True, stop=True)],
                          reads=['rW2', 'ident'], writes=[f"ps{bg}"], acc=(blk > 0))
                cx.op('dve', lambda: nc.vector.tensor_copy(gT[:, off:off + tn], P.ps[0:8, bg, :tn]), reads=[f"ps{bg}"], writes=['rgT'])
        if not moe:
            ffn_group(P, res, h2, tl, wi, wi_r0, wo, wo_r0, D_FF, None, XB, modg)
        else:
            for e in range(NE):
                Ge, Gk = Gp.get()
                for (off, xcol, tn, c) in tl:
                    be = cx.bank()
                    cx.mm([lambda: nc.tensor.matmul(P.ps[:, be, :tn], selm[:, e * 128:(e + 1) * 128], gT[:, off:off + tn], start=True, stop=True)],
                          reads=['selm', 'rgT'], writes=[f"ps{be}"])
                    cx.op('act', lambda: nc.scalar.activation(Ge[:, off:off + tn], P.ps[:, be, :tn], AF.Copy), reads=[f"ps{be}"], writes=[Gk])
                ffn_group(P, res, h2, tl, wi, wi_r0 + e * D, wo, wo_r0 + e * D_FFE, D_FFE, (Ge, Gk), XB, modg)


def phase_final(P, pes, XB, gfT_d, yo):
    cx, nc = P.cx, P.nc
    gf = pes.enter_context(nc.sbuf_tensor(cx.key("gf"), [128, 16], F32))
    cx.dma('sp', gf[:], gfT_d, writes=['gf'])
    xp = Pool(nc, pes, cx.key("fx"), [128, 16, 512], F32, 2)
    sqp = Pool(nc, pes, cx.key("fsq"), [128, 512], F32, 3)
    rp = Pool(nc, pes, cx.key("fr"), [128, 512], F32, 2)
    for i in range(4):
        xcol = 1024 + 512 * i
        xt, xk = xp.get()
        cx.dma('sp', xt[:], XB[:, xcol:xcol + 512].rearrange("(k p) t -> p k t", p=128), writes=[xk])
        b = cx.bank()
        for k in range(16):
            sq, sk = sqp.get()
            cx.op('act', lambda: nc.scalar.activation(sq[:], xt[:, k, :], AF.Square), reads=[xk], writes=[sk])
            cx.mm([lambda: nc.tensor.matmul(P.ps[:, b, :], P.ones_f[:], sq[:], start=(k == 0), stop=(k == 15))],
                  reads=[sk, 'ones_f'], writes=[f"ps{b}"], acc=(k > 0))
        rt, rk = rp.get()
        cx.op('dve', lambda: nc.vector.tensor_scalar(rt[:], P.ps[:, b, :], 1.0 / D, EPS, ALU.mult, ALU.add), reads=[f"ps{b}"], writes=[rk])
        cx.op('act', lambda: nc.scalar.activation(rt[:], rt[:], AF.Sqrt), reads=[rk], writes=[rk])
        cx.op('dve', lambda: nc.vector.reciprocal(rt[:], rt[:]), reads=[rk], writes=[rk])
        for k in range(16):
            e = 'dve'
            E = nc.vector
            cx.op(e, lambda: E.scalar_tensor_tensor(xt[:, k, :], xt[:, k, :], gf[:, k:k + 1], rt[:], ALU.mult, ALU.mult),
                  reads=[xk, rk, 'gf'], writes=[xk])
        cx.dma('sp', yo[:, 512 * i:512 * (i + 1)].rearrange("(k p) t -> p k t", p=128), xt[:], reads=[xk])


def build_all(depth=DEPTH, dbg=None):
    es = ExitStack()
    P = Prog(es)
    nc, cx = P.nc, P.cx
    I = lambda n, s, d: P.dram(n, s, d, "ExternalInput")
    n_dense, n_moe = (depth + 1) // 2, depth // 2
    L0 = DEPTH - depth
    xin = I("xT", [D, XC], F32)
    cT = I("cT", [128, 16, 2], F32)
    wmod = I("wmod", [depth * D, 6 * D], F32)
    bmodT = I("bmodT", [depth, 128, 96], F32)
    gT_d = I("gT", [128, depth * 32 + 16], F32)
    win = I("win", [depth * D, IN_DIM], F32)
    cosd, sind, tval = I("cosT", [128, XW], F32), I("sinT", [128, XW], F32), I("tval", [128, XW], F32)
    ttab = I("ttab", [depth * 8, 128, NIDX * 64], F32)
    maskL = [I(f"maskL{l}", [8, (NQR[L0 + l] // 8) * NKT * 128], BF16) for l in range(depth)]
    rsel = I("rsel", [8, 512], BF16)
    m3 = I("m3", [128, 384], F32)
    kvb = [I(f"kvb{l}", [128, NKVR[L0 + l] * GW // 128], F32) for l in range(depth)]
    sinkb = I("sinkb", [depth, 128, 8], F32)
    cwT = I("cwT", [depth, 128, 8, 31], F32)
    cvp = I("cvp", [depth, 3, 128, 8], F32)
    wbr = I("wbr", [depth * 3072, D], F32)
    wout = I("wout", [depth * D, D], F32)
    fwi = I("fwi", [n_dense * D, 2 * D_FF], F32)
    fwo = I("fwo", [n_dense * D_FF, D], F32)
    if n_moe:
        mwi = I("mwi", [n_moe * NE * D, 2 * D_FFE], F32)
        mwo = I("mwo", [n_moe * NE * D_FFE, D], F32)
        mwr = I("mwr", [n_moe * D, NE], F32)
        ident = I("ident", [128, 128], F32)
        selm = I("selm", [8, 8 * 128], F32)
    yo = P.dram("yo", [D, TL], F32, "ExternalOutput")
    T = lambda n, s, d: P.dram(n, s, d, "Internal")
    XB = T("XB", [D, XC], F32)
    HT = T("HT", [D, XC], BF16)
    NQM = NQR[0] * GW + CL
    KWM = NKVR[0] * GW + 2 * NAM + CL
    B = {'NAQ': T("NAQ", [1024, NQM], BF16), 'NAK': T("NAK", [1024, KWM], BF16), 'NAV': T("NAV", [KWM, 1024], BF16),
         'WAQ': T("WAQ", [1024, NQM], BF16), 'WAK': T("WAK", [256, XC], BF16), 'WAV': T("WAV", [XC, 256], BF16),
         'ZT': T("ZT", [1024, XC], F32), 'GST': T("GST", [6144, NQM], F32),
         'ONA': T("ONA", [1024, NQM], BF16), 'OCV': T("OCV", [1024, NQM], BF16), 'OWA': T("OWA", [1024, NQM], BF16)}
    dbg_out = {}
    if dbg:
        for k in dbg:
            a = B[k] if k in B else {'XB': XB, 'HT': HT}[k]
            dbg_out[k] = P.dram("dbg_" + k, list(a.shape), a.dtype, "ExternalOutput")
    mod = es.enter_context(nc.sbuf_tensor("mod", [128, 96, 2], F32))
    A = es.enter_context(nc.sbuf_tensor("modA", [128, 16, 2], F32))
    gT = es.enter_context(nc.sbuf_tensor("gT_sb", [128, depth * 32 + 16], F32))
    cx.dma('sp', gT[:], gT_d, writes=['gT'])
    for i in range(4):
        cx.dma('sp' if i % 2 == 0 else 'act', XB[i * 512:(i + 1) * 512, :], xin[i * 512:(i + 1) * 512, :], writes=['XB'])
    with ExitStack() as pes:
        zt = pes.enter_context(nc.sbuf_tensor("zt", [128, 8192], BF16))
        cx.op('pool', lambda: nc.gpsimd.memset(zt[:], 0.0), writes=['zt'])
        for r in range(8):
            cx.dma('sp', B['NAK'][r * 128:(r + 1) * 128, :], zt[:, 0:KWM], reads=['zt'], writes=['NAK'])
        nav_v = B['NAV'].rearrange("(t p) d -> p t d", p=128)
        ntile = KWM // 128
        for t0 in range(0, ntile, 8):
            tn = min(8, ntile - t0)
            cx.dma('sp', nav_v[:, t0:t0 + tn, :], zt[:, 0:tn * 1024].rearrange("p (t d) -> p t d", d=1024), reads=['zt'], writes=['NAV'])
        barrier(cx)
    for l in range(depth):
        g = L0 + l
        moe = (l % 2 == 1)
        li = l // 2
        last = (l == depth - 1)
        with ExitStack() as pes:
            phase_modvec(P, cT, wmod[l * D:(l + 1) * D, :], bmodT[l], mod, pes)
            make_A(P, mod, gT, A, 16, l * 32)
            barrier(cx)
        with ExitStack() as pes:
            phase_norm1(P, pes, g, XB, HT, A, mod[:, 0:16, :])
            barrier(cx)
        with ExitStack() as pes:
            phase_inproj(P, pes, g, HT, win, l * D, cosd, sind, tval, B)
            barrier(cx)
        with ExitStack() as pes:
            phase_conv(P, pes, g, B, cwT[l], cvp[l, 0], cvp[l, 1], cvp[l, 2])
            barrier(cx)
        with ExitStack() as pes:
            phase_na(P, pes, g, B, lambda h: ttab[l * 8 + h], maskL[l], rsel)
            barrier(cx)
        with ExitStack() as pes:
            phase_wa(P, pes, g, B, m3, kvb[l], sinkb[l])
            barrier(cx)
        with ExitStack() as pes:
            phase_merge(P, pes, g, B, XB, wbr, l * 3072, wout, l * D, mod)
            barrier(cx)
        make_A(P, mod, gT, A, 64, l * 32 + 16)
        with ExitStack() as pes:
            if moe:
                phase_ffn(P, pes, g, XB, mod, A, True, mwi, li * NE * D, mwo, li * NE * D_FFE, mwr[li * D:(li + 1) * D, :], ident, selm,
                          skip_ctx=last)
            else:
                phase_ffn(P, pes, g, XB, mod, A, False, fwi, li * D, fwo, li * D_FF, skip_ctx=last)
            barrier(cx)
    for k, o in dbg_out.items():
        a = B[k] if k in B else {'XB': XB, 'HT': HT}[k]
        cx.dma('sp', o, a)
    with ExitStack() as pes:
        phase_final(P, pes, XB, gT_d[:, depth * 32:depth * 32 + 16], yo)
        barrier(cx)
    cx.finish('sp')
    print("fused program: depth", depth, "inst", cx.n_inst, "waits", cx.n_wait)
    return P


def fm(v, n):
    return np.ascontiguousarray(np.asarray(v, dtype=np.float32).reshape(n, 128).T)


def rope_tables(tok0, ntok):
    t = np.arange(tok0, tok0 + ntok, dtype=np.int64)
    pos = np.stack([t // GW, t % GW], axis=-1).astype(np.float32)
    inv_freq = (1.0 / np.power(np.float32(10000.0), np.arange(32, dtype=np.float32) / np.float32(32))).astype(np.float32)
    ang = (pos[:, :, None] * inv_freq).astype(np.float32)
    cos, sin = np.cos(ang).astype(np.float32), np.sin(ang).astype(np.float32)
    cosT = np.zeros((128, ntok), np.float32)
    sinT = np.zeros((128, ntok), np.float32)
    for a in range(2):
        for j in range(2):
            sl = slice(a * 64 + j * 32, a * 64 + j * 32 + 32)
            cosT[sl] = cos[:, a, :].T
            sinT[sl] = (-1.0 if j == 0 else 1.0) * sin[:, a, :].T
    return cosT, sinT


def na_bias_table(rpb_l):
    p = np.arange(128)
    a, kc = p // 64, p % 64
    idx = np.arange(NIDX)
    c = np.arange(64)
    dr = a[:, None] + 13 - idx[None, :]
    drv = (np.abs(dr) <= 7)
    ws = np.clip(c - 8, 0, 48)
    colv = (kc[:, None] >= ws[None, :]) & (kc[:, None] < ws[None, :] + 16)
    cidx = np.clip(kc[:, None] - c[None, :] + 15, 0, 30)
    g = rpb_l[:, np.clip(dr + 7, 0, 14)[:, :, None], cidx[:, None, :]]
    g = np.where(drv[None, :, :, None], g, np.float32(0.0))
    g = np.where(colv[None, :, None, :], g, np.float32(NEG))
    return np.ascontiguousarray(g.reshape(8, 128, NIDX * 64).astype(np.float32))


def na_mask_lhs(s_row, nqb, rows):
    m = np.zeros((8, nqb, NKT, 2, 64), np.float32)
    for qb in range(nqb):
        for t in range(NKT):
            for a in range(2):
                kr = s_row + 8 * qb - 7 + 2 * t + a
                for rp in range(8):
                    r = s_row + 8 * qb + rp
                    r0 = min(max(r - 4, 0), rows - 8)
                    if not ((0 <= kr < rows) and (r0 <= kr < r0 + 8)):
                        m[rp, qb, t, a, :] = NEG
    return np.ascontiguousarray(m.reshape(8, nqb * NKT * 128)).astype(ml_dtypes.bfloat16)


def wa_band_mask():
    k = np.arange(128)[:, None]
    rel = np.arange(384)[None, :] // 128
    q = np.arange(384)[None, :] % 128
    ok = np.abs((rel - 1) * 128 + q - k) <= 128
    return np.where(ok, np.float32(0), np.float32(NEG)).astype(np.float32)


RSEL = np.zeros((8, 512), np.float32)
for _r in range(8):
    RSEL[_r, _r * 64:(_r + 1) * 64] = 1.0
RSEL = RSEL.astype(ml_dtypes.bfloat16)
IDENT = np.eye(128, dtype=np.float32)
SELM = np.zeros((8, 8 * 128), np.float32)
for _e in range(8):
    SELM[_e, _e * 128:(_e + 1) * 128] = 1.0

_PROGS = {}


def run_model(inp, depth, dbg=None, trace=False):
    x = np.asarray(inp['x'], np.float32)
    Bn, S, _ = x.shape
    rows = S // GW
    cpb = rows // 32
    ncore = Bn * cpb
    L0 = DEPTH - depth
    n_dense, n_moe = (depth + 1) // 2, depth // 2
    key = (depth, tuple(dbg) if dbg else None)
    if key not in _PROGS:
        _PROGS[key] = build_all(depth, dbg)
    P = _PROGS[key]
    f32 = lambda a: np.ascontiguousarray(np.asarray(a, np.float32))
    shared = dict(
        wmod=f32(inp['w_mod'][:depth]).reshape(depth * D, 6 * D),
        bmodT=np.stack([fm(inp['b_mod'][l], 96) for l in range(depth)]),
        gT=np.concatenate([np.concatenate([fm(inp['g_norm1'][l], 16), fm(inp['g_norm2'][l], 16)], axis=1) for l in range(depth)]
                          + [fm(inp['g_final'], 16)], axis=1),
        win=f32(inp['w_in'][:depth]).reshape(depth * D, IN_DIM),
        ttab=np.concatenate([na_bias_table(f32(inp['rpb'][l])) for l in range(depth)], axis=0),
        rsel=RSEL, m3=wa_band_mask(),
        sinkb=np.stack([np.ascontiguousarray(np.broadcast_to(f32(inp['sink'][l])[None, :], (128, 8))) for l in range(depth)]),
        cwT=np.stack([np.ascontiguousarray(f32(inp['conv_w'][l]).T.reshape(8, 128, 31).transpose(1, 0, 2)) for l in range(depth)]),
        cvp=np.stack([np.stack([fm(inp['conv_b'][l], 8), fm(inp['ln_g'][l], 8), fm(inp['ln_b'][l], 8)]) for l in range(depth)]),
        wbr=f32(inp['w_branch'][:depth]).reshape(depth * 3072, D),
        wout=f32(inp['w_out'][:depth]).reshape(depth * D, D),
        fwi=f32(inp['ffn_wi'][:n_dense]).reshape(n_dense * D, 2 * D_FF),
        fwo=f32(inp['ffn_wo'][:n_dense]).reshape(n_dense * D_FF, D),
    )
    if n_moe:
        shared.update(mwi=f32(inp['moe_wi'][:n_moe]).reshape(n_moe * NE * D, 2 * D_FFE),
                      mwo=f32(inp['moe_wo'][:n_moe]).reshape(n_moe * NE * D_FFE, D),
                      mwr=f32(inp['moe_router'][:n_moe]).reshape(n_moe * D, NE), ident=IDENT, selm=SELM)
    maps = []
    for core in range(ncore):
        b, ci = core // cpb, core % cpb
        R0 = 32 * ci
        w0 = R0 - 16
        xw = np.zeros((XW, D), np.float32)
        lo, hi = max(w0, 0), min(w0 + WROWS, rows)
        xw[(lo - w0) * GW:(hi - w0) * GW] = x[b, lo * GW:hi * GW]
        xT = np.ascontiguousarray(np.concatenate([xw.T, f32(inp['ctx'][b]).T], axis=1))
        cT = np.ascontiguousarray(np.stack([fm(inp['c'][b], 16), fm(inp['c_ctx'], 16)], axis=-1))
        cosT, sinT = rope_tables(w0 * GW, XW)
        tok = np.arange(XW) + w0 * GW
        valid = (tok >= 0) & (tok < S)
        tval = np.ascontiguousarray(np.broadcast_to(valid.astype(np.float32)[None, :], (128, XW)))
        d = dict(shared, xT=xT, cT=cT, cosT=cosT, sinT=sinT, tval=tval)
        for l in range(depth):
            g = L0 + l
            d[f"maskL{l}"] = na_mask_lhs(w0 + 4 * (g + 1), NQR[g] // 8, rows)
            nkt = NKVR[g] * GW // 128
            kv = valid[256 * g:256 * g + nkt * 128].reshape(nkt, 128).T
            d[f"kvb{l}"] = np.ascontiguousarray(np.where(kv, np.float32(0), np.float32(NEG)).astype(np.float32))
        maps.append(d)
    res = run_bass_kernel_spmd(P.nc, maps, core_ids=list(range(ncore)), **({'trace': True} if trace else {}))
    out = np.zeros((Bn, S, D), np.float32)
    for core in range(ncore):
        b, ci = core // cpb, core % cpb
        out[b, ci * TL:(ci + 1) * TL, :] = np.asarray(res.results[core]['yo']).T
    return out, res


def kernel(**inputs):
    out, _ = run_model(inputs, DEPTH)
    return out
```

```python
import numpy as np
from contextlib import ExitStack
import ml_dtypes
import concourse.bass as bass
import concourse.mybir as mybir
from concourse.bass_utils import run_bass_kernel_spmd

F32 = mybir.dt.float32
BF16 = mybir.dt.bfloat16
AF = mybir.ActivationFunctionType
ALU = mybir.AluOpType
AX = mybir.AxisListType

D = 2048
TL = 2048
CL = 256
TT = TL + CL
GW = 64
HD = 128
IN_DIM = 12800
D_FF = 5632
NE = 8
D_FFE = 4096
EPS = 1e-6
NEG = -1e30
SCALE = HD ** -0.5
TILES = [(0, 512), (512, 512), (1024, 512), (1536, 512), (2048, 256)]
NA_HALO = 7 * GW
WA_HALO = 128
CV_HALO = 15

SEM_ROT = 30000
N_DMA_SEMS = 32


class Ctx:
    def __init__(self, nc, es):
        self.nc = nc
        self.es = es
        self.eng = {'pe': nc.tensor, 'act': nc.scalar, 'dve': nc.vector, 'pool': nc.gpsimd, 'sp': nc.sync}
        self.sems = {}
        self.cur = {}
        self.gen = {e: 0 for e in self.eng}
        for e in self.eng:
            self._new_sem(e)
        self.dma_sems = []
        for j in range(N_DMA_SEMS):
            k = ('dma', j)
            self.sems[k] = es.enter_context(nc.semaphore(f"dma{j}"))
            self.dma_sems.append([k, 0])
        self.dma_rr = 0
        self.waited = {e: {} for e in self.eng}
        self.lastw = {}
        self.readers = {}
        self.n_inst = 0
        self.n_wait = 0
        self.bank_rr = 0
        self.uid = 0

    def _new_sem(self, e):
        k = (e, self.gen[e])
        self.gen[e] += 1
        self.sems[k] = self.es.enter_context(self.nc.semaphore(f"s_{e}_{k[1]}"))
        self.cur[e] = [k, 0]

    def _wait(self, e, tok):
        if tok is None:
            return
        k, v = tok
        w = self.waited[e]
        if w.get(k, 0) >= v:
            return
        self.eng[e].wait_ge(self.sems[k], v)
        w[k] = v
        self.n_wait += 1

    def _deps(self, e, reads, writes, acc=False):
        for r in reads:
            self._wait(e, self.lastw.get(r))
        for wk in writes:
            lw = self.lastw.get(wk)
            if not (acc and lw is not None and lw[0][0] == 'pe'):
                self._wait(e, lw)
            for t in self.readers.get(wk, ()):
                self._wait(e, t)

    def _commit(self, tok, reads, writes):
        for r in reads:
            lst = self.readers.setdefault(r, [])
            lst.append(tok)
            if len(lst) > 12:
                d = {}
                for k, v in lst:
                    d[k] = max(d.get(k, 0), v)
                self.readers[r] = list(d.items())
        for wk in writes:
            self.lastw[wk] = tok
            self.readers[wk] = []

    def _bump(self, e, ins):
        c = self.cur[e]
        c[1] += 1
        ins.then_inc(self.sems[c[0]], 1)
        tok = (c[0], c[1])
        if c[1] >= SEM_ROT:
            self._new_sem(e)
        return tok

    def op(self, e, fn, reads=(), writes=()):
        self._deps(e, reads, writes)
        ins = fn()
        tok = self._bump(e, ins)
        self._commit(tok, reads, writes)
        self.n_inst += 1
        return tok

    def mm(self, fns, reads=(), writes=(), acc=False):
        self._deps('pe', reads, writes, acc)
        ins = None
        for fn in fns:
            ins = fn()
            self.n_inst += 1
        tok = self._bump('pe', ins)
        self._commit(tok, reads, writes)
        return tok

    def dma(self, q, out, in_, reads=(), writes=(), **kw):
        self._deps(q, reads, writes)
        s = self.dma_sems[self.dma_rr]
        self.dma_rr = (self.dma_rr + 1) % len(self.dma_sems)
        k = s[0]
        if s[1] > 0:
            self._wait(q, (k, 16 * s[1]))
        ins = self.eng[q].dma_start(out=out, in_=in_, **kw)
        s[1] += 1
        ins.then_inc(self.sems[k], 16)
        tok = (k, 16 * s[1])
        self._commit(tok, reads, writes)
        self.n_inst += 1
        return tok

    def finish(self, e='sp'):
        for s in self.dma_sems:
            if s[1] > 0:
                self._wait(e, (s[0], 16 * s[1]))
        for en in self.eng:
            for g in range(self.gen[en]):
                k = (en, g)
                v = self.cur[en][1] if self.cur[en][0] == k else SEM_ROT
                if v > 0:
                    self._wait(e, (k, v))

    def bank(self, lo=0, hi=8):
        b = lo + (self.bank_rr % (hi - lo))
        self.bank_rr += 1
        return b

    def key(self, base):
        self.uid += 1
        return f"{base}_{self.uid}"


class Pool:
    def __init__(self, nc, es, name, shape, dtype, n):
        self.bufs = [es.enter_context(nc.sbuf_tensor(f"{name}{i}", shape, dtype)) for i in range(n)]
        self.keys = [f"{name}{i}" for i in range(n)]
        self.i = 0

    def get(self):
        j = self.i % len(self.bufs)
        self.i += 1
        return self.bufs[j], self.keys[j]


class Prog:
    def __init__(self, es):
        self.nc = bass.Bass("TRN2", target_bir_lowering=False)
        self.es = es
        self.cx = Ctx(self.nc, es)
        nc = self.nc
        self.ps = es.enter_context(nc.psum_tensor("ps", [128, 8, 512], F32))
        self.wpool = Pool(nc, es, "wb", [128, 8192], BF16, 3)
        self.ones_f = es.enter_context(nc.sbuf_tensor("ones_f", [128, 128], F32))
        self.ones_b = es.enter_context(nc.sbuf_tensor("ones_b", [128, 128], BF16))
        self.cx.op('dve', lambda: nc.vector.memset(self.ones_f[:], 1.0), writes=['ones_f'])
        self.cx.op('dve', lambda: nc.vector.memset(self.ones_b[:], 1.0), writes=['ones_b'])
        self.wq = 0

    def dram(self, name, shape, dt, kind):
        return self.nc.dram_tensor(name, list(shape), dt, kind=kind).ap()

    def load_w(self, W, r0, nk, c0, ncols, extra_reads=()):
        assert nk * ncols <= 8192
        buf, key = self.wpool.get()
        view = buf[:, 0:nk * ncols].rearrange("p (k c) -> p k c", k=nk)
        src = W[r0:r0 + nk * 128, c0:c0 + ncols].rearrange("(k p) c -> p k c", p=128)
        self.cx.dma('pool', view, src, reads=list(extra_reads), writes=[key])
        return view, key


def linear_fm(P, act, act_key, nk, W, r0, c0, ncols, tiles, evac, group=512):
    cx, nc = P.cx, P.nc
    g = min(group, 8192 // nk // 128 * 128)
    for gc in range(0, ncols, g):
        gn = min(g, ncols - gc)
        wt, wkey = P.load_w(W, r0, nk, c0 + gc, gn)
        for m in range(gn // 128):
            for (t0, tn) in tiles:
                b = cx.bank()
                pk = f"ps{b}"
                fns = [(lambda k=k: nc.tensor.matmul(P.ps[:, b, :tn], wt[:, k, m * 128:(m + 1) * 128],
                                                     act[:, k, t0:t0 + tn], start=(k == 0), stop=(k == nk - 1)))
                       for k in range(nk)]
                cx.mm(fns, reads=[act_key, wkey], writes=[pk])
                evac((gc // 128) + m, (t0, tn), P.ps[:, b, :tn], pk)


def linear_tm(P, act, act_key, nk, W, r0, c0, ncols, ntok, evac):
    cx, nc = P.cx, P.nc
    assert ncols <= 512
    wt, wkey = P.load_w(W, r0, nk, c0, ncols)
    for tt in range(ntok // 128):
        b = cx.bank()
        pk = f"ps{b}"
        fns = [(lambda k=k: nc.tensor.matmul(P.ps[:, b, :ncols], act[:, k, tt * 128:(tt + 1) * 128],
                                             wt[:, k, :], start=(k == 0), stop=(k == nk - 1)))
               for k in range(nk)]
        cx.mm(fns, reads=[act_key, wkey], writes=[pk])
        evac(tt, P.ps[:, b, :ncols], pk)


def alt_copy(P, i, out, in_, reads, writes, scale=None):
    cx, nc = P.cx, P.nc
    if i % 2 == 0:
        cx.op('act', lambda: nc.scalar.activation(out, in_, AF.Copy), reads=reads, writes=writes)
    else:
        cx.op('dve', lambda: nc.vector.tensor_copy(out, in_), reads=reads, writes=writes)


DEPTH = 4
NQR = [56, 48, 40, 32]
NKVR = [64, 56, 48, 40]
WROWS = 64
XW = WROWS * GW
XC = XW + CL
NAM = 3 * GW
NKT = 11
NIDX = 28


def barrier(cx):
    for e in cx.eng:
        cx.finish(e)


def kv_tiles(l):
    nq = NQR[l] * GW
    t = [(0, 256, 'm')] + [(256 + 512 * i, 512, 'o') for i in range(nq // 512)] + [(256 + nq, 256, 'm')]
    return t


def o_tiles(l):
    nq = NQR[l] * GW
    q0 = 256 * (l + 1)
    return [(q0 + 512 * i, 512 * i, 512, False) for i in range(nq // 512)] + [(XW, nq, CL, True)]


def rms_norm_tile(P, pools, xsrc, xcol, n, c, A, Sh, dst_fn, hf_cb=None):
    cx, nc = P.cx, P.nc
    xp, sqp, rp = pools
    b = cx.bank()
    pk = f"ps{b}"
    for k in range(16):
        xt, xk = xp.get()
        cx.dma('sp', xt[:, :n], xsrc[k * 128:(k + 1) * 128, xcol:xcol + n], writes=[xk])
        sq, sk = sqp.get()
        cx.op('act', lambda: nc.scalar.activation(sq[:, :n], xt[:, :n], AF.Square), reads=[xk], writes=[sk])
        cx.mm([lambda: nc.tensor.matmul(P.ps[:, b, :n], P.ones_f[:], sq[:, :n], start=(k == 0), stop=(k == 15))],
              reads=[sk, 'ones_f'], writes=[pk], acc=(k > 0))
    rt, rk = rp.get()
    cx.op('dve', lambda: nc.vector.tensor_scalar(rt[:, :n], P.ps[:, b, :n], 1.0 / D, EPS, ALU.mult, ALU.add), reads=[pk], writes=[rk])
    cx.op('act', lambda: nc.scalar.activation(rt[:, :n], rt[:, :n], AF.Sqrt), reads=[rk], writes=[rk])
    cx.op('dve', lambda: nc.vector.reciprocal(rt[:, :n], rt[:, :n]), reads=[rk], writes=[rk])
    for k in range(16):
        xt, xk = xp.get()
        cx.dma('sp', xt[:, :n], xsrc[k * 128:(k + 1) * 128, xcol:xcol + n], writes=[xk])
        sq, sk = sqp.get()
        cx.op('dve', lambda: nc.vector.scalar_tensor_tensor(sq[:, :n], xt[:, :n], A[:, k, c:c + 1], rt[:, :n], ALU.mult, ALU.mult),
              reads=[xk, rk, 'modA'], writes=[sk])
        dst, dkey = dst_fn(k)
        if hf_cb is None:
            cx.op('act', lambda: nc.scalar.activation(dst, sq[:, :n], AF.Identity, bias=Sh[:, k, c:c + 1]),
                  reads=[sk, 'modA'], writes=[dkey])
        else:
            cx.op('act', lambda: nc.scalar.activation(sq[:, :n], sq[:, :n], AF.Identity, bias=Sh[:, k, c:c + 1]),
                  reads=[sk, 'modA'], writes=[sk])
            cx.op('dve', lambda: nc.vector.tensor_copy(dst, sq[:, :n]), reads=[sk], writes=[dkey])
            hf_cb(k, n, sq, sk)


def norm_pools(P, pes):
    nc, cx = P.nc, P.cx
    return (Pool(nc, pes, cx.key("rx"), [128, 512], F32, 4), Pool(nc, pes, cx.key("rsq"), [128, 512], F32, 4),
            Pool(nc, pes, cx.key("rr"), [128, 512], F32, 2))


def phase_modvec(P, cT_d, wmod_d, bmodT_d, mod, pes):
    cx, nc = P.cx, P.nc
    cf = pes.enter_context(nc.sbuf_tensor(cx.key("cf"), [128, 16, 2], F32))
    cb = pes.enter_context(nc.sbuf_tensor(cx.key("cb"), [128, 16, 2], BF16))
    bm = pes.enter_context(nc.sbuf_tensor(cx.key("bm"), [128, 96], F32))
    cx.dma('sp', cf[:], cT_d, writes=['cf'])
    cx.dma('sp', bm[:], bmodT_d, writes=['bm'])
    cx.op('act', lambda: nc.scalar.activation(cb[:], cf[:], AF.Silu), reads=['cf'], writes=['cb'])
    b = cx.bank()
    pk = f"ps{b}"
    for g in range(24):
        wt, wkey = P.load_w(wmod_d, 0, 16, g * 512, 512)
        for m in range(4):
            f = g * 4 + m
            fns = [(lambda k=k: nc.tensor.matmul(P.ps[:, b, 2 * f:2 * f + 2], wt[:, k, m * 128:(m + 1) * 128], cb[:, k, :],
                                                 start=(k == 0), stop=(k == 15))) for k in range(16)]
            cx.mm(fns, reads=['cb', wkey], writes=[pk], acc=(f > 0))
    pv = P.ps[:, b, 0:192].rearrange("p (f c) -> p f c", c=2)
    for c in range(2):
        cx.op('dve', lambda: nc.vector.tensor_tensor(mod[:, :, c], pv[:, :, c], bm[:, :], ALU.add), reads=[pk, 'bm'], writes=['mod'])


def make_A(P, mod, gT, A, sc0, gcol):
    cx, nc = P.cx, P.nc
    for c in range(2):
        cx.op('dve', lambda: nc.vector.scalar_tensor_tensor(A[:, :, c], mod[:, sc0:sc0 + 16, c], 1.0, gT[:, gcol:gcol + 16],
                                                            ALU.add, ALU.mult), reads=['mod', 'gT'], writes=['modA'])


def phase_norm1(P, pes, l, XB, HT, A, Sh):
    cx, nc = P.cx, P.nc
    pools = norm_pools(P, pes)
    stg = Pool(nc, pes, cx.key("n1s"), [128, 16, 512], BF16, 2)
    nkv = NKVR[l] * GW
    tl = [(256 * l + c0, c0, n, 0) for (c0, n, _) in kv_tiles(l)] + [(XW, nkv, CL, 1)]
    for (xcol, hcol, n, c) in tl:
        st, sk = stg.get()
        rms_norm_tile(P, pools, XB, xcol, n, c, A, Sh, lambda k: (st[:, k, :n], sk))
        cx.dma('sp', HT[:, hcol:hcol + n].rearrange("(k p) t -> p k t", p=128), st[:, :, :n], reads=[sk])


def phase_inproj(P, pes, l, HT, win, lw, cos_d, sin_d, tval_d, B):
    cx, nc = P.cx, P.nc
    nq, nkv = NQR[l] * GW, NKVR[l] * GW
    nak_ctx = nkv + 2 * NAM
    hT = pes.enter_context(nc.sbuf_tensor(cx.key("hT"), [128, 16, 2048], BF16))
    sbp = Pool(nc, pes, cx.key("stb"), [128, 512], BF16, 4)
    sfp = Pool(nc, pes, cx.key("stf"), [128, 512], F32, 3)
    tmpf = Pool(nc, pes, cx.key("tmf"), [128, 512], F32, 4)
    tabp = Pool(nc, pes, cx.key("tab"), [128, 512], F32, 4)
    wperm = pes.enter_context(nc.sbuf_tensor(cx.key("wperm"), [128, 8192], BF16))
    tiles = [(c0, n, kind) for (c0, n, kind) in kv_tiles(l)] + [(nkv, CL, 'c')]
    chunks, cur, tot = [], [], 0
    for t in tiles:
        if tot + t[1] > 2048:
            chunks.append(cur)
            cur, tot = [], 0
        cur.append((tot,) + t)
        tot += t[1]
    chunks.append(cur)
    cnt = [0]
    for ch in chunks:
        ctot = sum(t[2] for t in ch)
        h0 = ch[0][1]
        cx.dma('sp', hT[:, :, :ctot], HT[:, h0:h0 + ctot].rearrange("(k p) t -> p k t", p=128), writes=['hT'])
        all_t = ch
        oc_t = [t for t in ch if t[3] in ('o', 'c')]

        def mm16(b, wt, m, hc, n):
            fns = [(lambda k=k: nc.tensor.matmul(P.ps[:, b, :n], wt[:, k, m * 128:(m + 1) * 128], hT[:, k, hc:hc + n],
                                                 start=(k == 0), stop=(k == 15))) for k in range(16)]
            return fns

        def plain(dst, c0, ncols, tl, colfn, sig=False, f32=False):
            if not tl:
                return
            for gc in range(0, ncols, 512):
                gn = min(512, ncols - gc)
                wt, wkey = P.load_w(win, lw, 16, c0 + gc, gn)
                for m in range(gn // 128):
                    mi = gc // 128 + m
                    for (hc, kvc, n, kind) in tl:
                        b = cx.bank()
                        cx.mm(mm16(b, wt, m, hc, n), reads=['hT', wkey], writes=[f"ps{b}"])
                        buf, key = (sfp if f32 else sbp).get()
                        cnt[0] += 1
                        if sig:
                            cx.op('act', lambda: nc.scalar.activation(buf[:, :n], P.ps[:, b, :n], AF.Sigmoid), reads=[f"ps{b}"], writes=[key])
                        else:
                            alt_copy(P, cnt[0], buf[:, :n], P.ps[:, b, :n], [f"ps{b}"], [key])
                        dc = colfn(kvc, kind)
                        cx.dma('sp', dst[mi * 128:(mi + 1) * 128, dc:dc + n], buf[:, :n], reads=[key])

        def vsec(dst, c0, ncols, rowfn):
            for gc in range(0, ncols, 512):
                gn = min(512, ncols - gc)
                wt, wkey = P.load_w(win, lw, 16, c0 + gc, gn)
                for (hc, kvc, n, kind) in all_t:
                    r0 = rowfn(kvc, kind)
                    for tt in range(n // 128):
                        b = cx.bank()
                        fns = [(lambda k=k: nc.tensor.matmul(P.ps[:, b, :gn], hT[:, k, hc + tt * 128:hc + (tt + 1) * 128], wt[:, k, :],
                                                             start=(k == 0), stop=(k == 15))) for k in range(16)]
                        cx.mm(fns, reads=['hT', wkey], writes=[f"ps{b}"])
                        buf, key = sbp.get()
                        cnt[0] += 1
                        alt_copy(P, cnt[0], buf[:, :gn], P.ps[:, b, :gn], [f"ps{b}"], [key])
                        cx.dma('sp', dst[r0 + tt * 128:r0 + (tt + 1) * 128, gc:gc + gn], buf[:, :gn], reads=[key])

        def rope(dst, c0, ncols, tl, colfn):
            if not tl:
                return
            for gc in range(0, ncols, 512):
                gn = min(512, ncols - gc)
                wt, wkey = P.load_w(win, lw, 16, c0 + gc, gn)
                wv = wt.rearrange("p k (h j f) -> p k h j f", j=2, f=32)
                pv = wperm[:, 0:16 * gn].rearrange("p (k c) -> p k c", k=16)
                pvv = pv.rearrange("p k (h j f) -> p k h j f", j=2, f=32)
                for j in range(2):
                    cx.op('pool', lambda: nc.gpsimd.tensor_copy(pvv[:, :, :, j, :], wv[:, :, :, 1 - j, :]), reads=[wkey], writes=['wperm'])
                for (hc, kvc, n, kind) in tl:
                    if kind != 'c':
                        ct, ck = tabp.get()
                        st_, stk = tabp.get()
                        wc = 256 * l + kvc
                        cx.dma('sp', ct[:, :n], cos_d[:, wc:wc + n], writes=[ck])
                        cx.dma('sp', st_[:, :n], sin_d[:, wc:wc + n], writes=[stk])
                    for m in range(gn // 128):
                        mi = gc // 128 + m
                        buf, key = sbp.get()
                        b1 = cx.bank()
                        cx.mm(mm16(b1, wt, m, hc, n), reads=['hT', wkey], writes=[f"ps{b1}"])
                        if kind != 'c':
                            b2 = cx.bank()
                            cx.mm(mm16(b2, pv, m, hc, n), reads=['hT', 'wperm'], writes=[f"ps{b2}"])
                            t1, k1 = tmpf.get()
                            t2, k2 = tmpf.get()
                            cx.op('dve', lambda: nc.vector.tensor_tensor(t1[:, :n], P.ps[:, b1, :n], ct[:, :n], ALU.mult),
                                  reads=[f"ps{b1}", ck], writes=[k1])
                            cx.op('dve', lambda: nc.vector.tensor_tensor(t2[:, :n], P.ps[:, b2, :n], st_[:, :n], ALU.mult),
                                  reads=[f"ps{b2}", stk], writes=[k2])
                            cx.op('dve', lambda: nc.vector.tensor_tensor(buf[:, :n], t1[:, :n], t2[:, :n], ALU.add),
                                  reads=[k1, k2], writes=[key])
                        else:
                            cx.op('act', lambda: nc.scalar.activation(buf[:, :n], P.ps[:, b1, :n], AF.Copy), reads=[f"ps{b1}"], writes=[key])
                        dc = colfn(kvc, kind)
                        cx.dma('sp', dst[mi * 128:(mi + 1) * 128, dc:dc + n], buf[:, :n], reads=[key])

        def glu():
            for gc in range(0, 1024, 512):
                wa, ka = P.load_w(win, lw, 16, 4608 + gc, 512)
                wg, kg = P.load_w(win, lw, 16, 5632 + gc, 512)
                for (hc, kvc, n, kind) in all_t:
                    if kind != 'c':
                        tv, tvk = tabp.get()
                        wc = 256 * l + kvc
                        cx.dma('sp', tv[:, :n], tval_d[:, wc:wc + n], writes=[tvk])
                    for m in range(4):
                        mi = gc // 128 + m
                        b1, b2 = cx.bank(), cx.bank()
                        cx.mm(mm16(b1, wa, m, hc, n), reads=['hT', ka], writes=[f"ps{b1}"])
                        cx.mm(mm16(b2, wg, m, hc, n), reads=['hT', kg], writes=[f"ps{b2}"])
                        t1, k1 = tmpf.get()
                        buf, key = sfp.get()
                        cx.op('act', lambda: nc.scalar.activation(t1[:, :n], P.ps[:, b2, :n], AF.Sigmoid), reads=[f"ps{b2}"], writes=[k1])
                        if kind != 'c':
                            cx.op('dve', lambda: nc.vector.tensor_tensor(t1[:, :n], P.ps[:, b1, :n], t1[:, :n], ALU.mult),
                                  reads=[f"ps{b1}", k1], writes=[k1])
                            cx.op('dve', lambda: nc.vector.tensor_tensor(buf[:, :n], t1[:, :n], tv[:, :n], ALU.mult),
                                  reads=[k1, tvk], writes=[key])
                        else:
                            cx.op('dve', lambda: nc.vector.tensor_tensor(buf[:, :n], P.ps[:, b1, :n], t1[:, :n], ALU.mult),
                                  reads=[f"ps{b1}", k1], writes=[key])
                        dc = kvc
                        cx.dma('sp', B['ZT'][mi * 128:(mi + 1) * 128, dc:dc + n], buf[:, :n], reads=[key])

        qcol = lambda kvc, kind: (nq if kind == 'c' else kvc - 256)
        plain(B['NAQ'], 0, 1024, oc_t, qcol)
        plain(B['NAK'], 1024, 1024, all_t, lambda kvc, kind: (nak_ctx if kind == 'c' else NAM + kvc))
        vsec(B['NAV'], 2048, 1024, lambda kvc, kind: (nak_ctx if kind == 'c' else NAM + kvc))
        rope(B['WAQ'], 3072, 1024, oc_t, qcol)
        rope(B['WAK'], 4096, 256, all_t, lambda kvc, kind: kvc)
        vsec(B['WAV'], 4352, 256, lambda kvc, kind: kvc)
        glu()
        plain(B['GST'], 6656, 6144, oc_t, qcol, sig=True, f32=True)


def phase_na(P, pes, l, B, ttab_h, maskL_d, rsel_d):
    cx, nc = P.cx, P.nc
    nq, nkv = NQR[l] * GW, NKVR[l] * GW
    kw = nkv + 2 * NAM
    nvt = kw // 128
    nqb = NQR[l] // 8
    kp = Pool(nc, pes, cx.key("nak"), [128, kw + CL], BF16, 2)
    qp = Pool(nc, pes, cx.key("naq"), [128, nq + CL], BF16, 2)
    vp = Pool(nc, pes, cx.key("nav"), [128, nvt + 2, 128], BF16, 2)
    tp = Pool(nc, pes, cx.key("ntt"), [128, NIDX * 64], F32, 2)
    op_ = Pool(nc, pes, cx.key("nao"), [128, nq + CL], BF16, 2)
    sp = Pool(nc, pes, cx.key("nas"), [128, 512], F32, 3)
    pp = Pool(nc, pes, cx.key("nap"), [128, 512], BF16, 4)
    rp = Pool(nc, pes, cx.key("nar"), [128, 512], F32, 2)
    mL = pes.enter_context(nc.sbuf_tensor(cx.key("namL"), [8, nqb * NKT * 128], BF16))
    rs = pes.enter_context(nc.sbuf_tensor(cx.key("nars"), [8, 512], BF16))
    cx.dma('sp', mL[:], maskL_d, writes=['namL'])
    cx.dma('sp', rs[:], rsel_d, writes=['nars'])
    PO, PD = 6, 7

    def attend(q_ap, n, tiles, kT, kk, V, vk, Tt, tk, qkey, obuf, okey, ocol):
        first = True
        for ti, tl in enumerate(tiles):
            last = ti == len(tiles) - 1
            b = cx.bank(0, 6)
            pk = f"ps{b}"
            pT, pkey = pp.get()
            if tl[0] == 'ctx':
                i = tl[1]
                kcol = kw + i * 128
                vt = nvt + i
                cx.mm([lambda: nc.tensor.matmul(P.ps[:, b, :n], kT[:, kcol:kcol + 128], q_ap, start=True, stop=True)],
                      reads=[kk, qkey], writes=[pk])
                cx.op('act', lambda: nc.scalar.activation(pT[:, :n], P.ps[:, b, :n], AF.Exp, scale=SCALE), reads=[pk], writes=[pkey])
            else:
                t, qb = tl[1], tl[2]
                vt = 4 * qb + t
                kcol = vt * 128
                mcol = (qb * NKT + t) * 128
                cx.mm([lambda: nc.tensor.matmul(P.ps[:, b, :n], kT[:, kcol:kcol + 128], q_ap, start=True, stop=False),
                       lambda: nc.tensor.matmul(P.ps[:, b, :n], mL[:, mcol:mcol + 128], rs[:, :n], start=False, stop=True)],
                      reads=[kk, qkey, 'namL', 'nars'], writes=[pk])
                sT, sk = sp.get()
                i0 = (20 - 2 * t) * 64
                cx.op('dve', lambda: nc.vector.scalar_tensor_tensor(sT[:, :n], P.ps[:, b, :n], SCALE, Tt[:, i0:i0 + 512],
                                                                    ALU.mult, ALU.add), reads=[pk, tk], writes=[sk])
                cx.op('act', lambda: nc.scalar.activation(pT[:, :n], sT[:, :n], AF.Exp), reads=[sk], writes=[pkey])
            cx.mm([lambda: nc.tensor.matmul(P.ps[:, PO, :n], V[:, vt, :], pT[:, :n], start=first, stop=last)],
                  reads=[vk, pkey], writes=[f"ps{PO}"], acc=not first)
            cx.mm([lambda: nc.tensor.matmul(P.ps[:, PD, :n], P.ones_b[:], pT[:, :n], start=first, stop=last)],
                  reads=['ones_b', pkey], writes=[f"ps{PD}"], acc=not first)
            first = False
        rd, rk = rp.get()
        cx.op('dve', lambda: nc.vector.reciprocal(rd[:, :n], P.ps[:, PD, :n]), reads=[f"ps{PD}"], writes=[rk])
        cx.op('dve', lambda: nc.vector.tensor_tensor(obuf[:, ocol:ocol + n], P.ps[:, PO, :n], rd[:, :n], ALU.mult),
              reads=[f"ps{PO}", rk], writes=[okey])

    for h in range(8):
        kT, kk = kp.get()
        qT, qk = qp.get()
        V, vk = vp.get()
        Tt, tk = tp.get()
        ob, ok = op_.get()
        cx.dma('sp', kT[:], B['NAK'][h * 128:(h + 1) * 128, 0:kw + CL], writes=[kk])
        cx.dma('sp', qT[:], B['NAQ'][h * 128:(h + 1) * 128, 0:nq + CL], writes=[qk])
        cx.dma('sp', V[:], B['NAV'][0:kw + CL, h * 128:(h + 1) * 128].rearrange("(t p) d -> p t d", p=128), writes=[vk])
        cx.dma('sp', Tt[:], ttab_h(h), writes=[tk])
        for qb in range(nqb):
            tiles = [('ctx', 0), ('ctx', 1)] + [('lat', t, qb) for t in range(NKT)]
            attend(qT[:, qb * 512:(qb + 1) * 512], 512, tiles, kT, kk, V, vk, Tt, tk, qk, ob, ok, qb * 512)
        attend(qT[:, nq:nq + CL], CL, [('ctx', 0), ('ctx', 1)], kT, kk, V, vk, Tt, tk, qk, ob, ok, nq)
        cx.dma('sp', B['ONA'][h * 128:(h + 1) * 128, 0:nq + CL], ob[:], reads=[ok])


def phase_wa(P, pes, l, B, m3_d, kvb_d, sinkb_d):
    cx, nc = P.cx, P.nc
    nq, nkv = NQR[l] * GW, NKVR[l] * GW
    nkt = nkv // 128
    kp = Pool(nc, pes, cx.key("wak"), [128, nkv + CL], BF16, 2)
    qp = Pool(nc, pes, cx.key("waq"), [128, nq + CL], BF16, 2)
    vp = Pool(nc, pes, cx.key("wav"), [128, nkt + 2, 128], BF16, 2)
    op_ = Pool(nc, pes, cx.key("wao"), [128, nq + CL], BF16, 2)
    sp = Pool(nc, pes, cx.key("was"), [128, 384], F32, 3)
    pp = Pool(nc, pes, cx.key("wap"), [128, 512], BF16, 4)
    rp = Pool(nc, pes, cx.key("war"), [128, 512], F32, 2)
    m3 = pes.enter_context(nc.sbuf_tensor(cx.key("wam3"), [128, 384], F32))
    kvb = pes.enter_context(nc.sbuf_tensor(cx.key("wakvb"), [128, nkt], F32))
    esink = pes.enter_context(nc.sbuf_tensor(cx.key("waes"), [128, 8], F32))
    cx.dma('sp', m3[:], m3_d, writes=['wam3'])
    cx.dma('sp', kvb[:], kvb_d, writes=['waed'])
    cx.dma('sp', esink[:], sinkb_d, writes=['waes'])
    cx.op('act', lambda: nc.scalar.activation(esink[:], esink[:], AF.Exp), reads=['waes'], writes=['waes'])
    PO, PD = 6, 7

    def finalize(n, h, obuf, okey, ocol):
        rd, rk = rp.get()
        cx.op('dve', lambda: nc.vector.tensor_scalar(rd[:, :n], P.ps[:, PD, :n], esink[:, h:h + 1], None, ALU.add),
              reads=[f"ps{PD}", 'waes'], writes=[rk])
        cx.op('dve', lambda: nc.vector.reciprocal(rd[:, :n], rd[:, :n]), reads=[rk], writes=[rk])
        cx.op('dve', lambda: nc.vector.tensor_tensor(obuf[:, ocol:ocol + n], P.ps[:, PO, :n], rd[:, :n], ALU.mult),
              reads=[f"ps{PO}", rk], writes=[okey])

    def ctx_tiles(q_ap, n, kT, kk, V, vk, qk, last_i):
        for i in range(2):
            b = cx.bank(0, 6)
            pk = f"ps{b}"
            pT, pkey = pp.get()
            kcol = nkv + i * 128
            cx.mm([lambda: nc.tensor.matmul(P.ps[:, b, :n], kT[:, kcol:kcol + 128], q_ap, start=True, stop=True)],
                  reads=[kk, qk], writes=[pk])
            cx.op('act', lambda: nc.scalar.activation(pT[:, :n], P.ps[:, b, :n], AF.Exp, scale=SCALE), reads=[pk], writes=[pkey])
            lst = (i == 1) and last_i
            cx.mm([lambda: nc.tensor.matmul(P.ps[:, PO, :n], V[:, nkt + i, :], pT[:, :n], start=(i == 0), stop=lst)],
                  reads=[vk, pkey], writes=[f"ps{PO}"], acc=(i > 0))
            cx.mm([lambda: nc.tensor.matmul(P.ps[:, PD, :n], P.ones_b[:], pT[:, :n], start=(i == 0), stop=lst)],
                  reads=['ones_b', pkey], writes=[f"ps{PD}"], acc=(i > 0))

    for g in range(2):
        kT, kk = kp.get()
        V, vk = vp.get()
        cx.dma('sp', kT[:], B['WAK'][g * 128:(g + 1) * 128, 0:nkv + CL], writes=[kk])
        cx.dma('sp', V[:], B['WAV'][0:nkv + CL, g * 128:(g + 1) * 128].rearrange("(t p) d -> p t d", p=128), writes=[vk])
        for hh in range(4):
            h = 4 * g + hh
            qT, qk = qp.get()
            ob, ok = op_.get()
            cx.dma('sp', qT[:], B['WAQ'][h * 128:(h + 1) * 128, 0:nq + CL], writes=[qk])
            for Q in range(nq // 512):
                ctx_tiles(qT[:, Q * 512:(Q + 1) * 512], 512, kT, kk, V, vk, qk, False)
                for jj in range(4 * Q + 1, 4 * Q + 7):
                    j = jj - 2
                    qlo, qhi = max(j - 1, 4 * Q), min(j + 1, 4 * Q + 3)
                    n = (qhi - qlo + 1) * 128
                    c0 = (qlo - 4 * Q) * 128
                    mc0 = (qlo - j + 1) * 128
                    b = cx.bank(0, 6)
                    pk = f"ps{b}"
                    q_ap = qT[:, Q * 512 + c0:Q * 512 + c0 + n]
                    cx.mm([lambda: nc.tensor.matmul(P.ps[:, b, :n], kT[:, jj * 128:(jj + 1) * 128], q_ap, start=True, stop=True)],
                          reads=[kk, qk], writes=[pk])
                    sT, sk = sp.get()
                    cx.op('dve', lambda: nc.vector.tensor_tensor(sT[:, :n], P.ps[:, b, :n], m3[:, mc0:mc0 + n], ALU.add),
                          reads=[pk, 'wam3'], writes=[sk])
                    pT, pkey = pp.get()
                    cx.op('act', lambda: nc.scalar.activation(pT[:, :n], sT[:, :n], AF.Exp, bias=kvb[:, jj:jj + 1], scale=SCALE),
                          reads=[sk, 'waed'], writes=[pkey])
                    lst = jj == 4 * Q + 6
                    cx.mm([lambda: nc.tensor.matmul(P.ps[:, PO, c0:c0 + n], V[:, jj, :], pT[:, :n], start=False, stop=lst)],
                          reads=[vk, pkey], writes=[f"ps{PO}"], acc=True)
                    cx.mm([lambda: nc.tensor.matmul(P.ps[:, PD, c0:c0 + n], P.ones_b[:], pT[:, :n], start=False, stop=lst)],
                          reads=['ones_b', pkey], writes=[f"ps{PD}"], acc=True)
                finalize(512, h, ob, ok, Q * 512)
            ctx_tiles(qT[:, nq:nq + CL], CL, kT, kk, V, vk, qk, True)
            finalize(CL, h, ob, ok, nq)
            cx.dma('sp', B['OWA'][h * 128:(h + 1) * 128, 0:nq + CL], ob[:], reads=[ok])


def phase_conv(P, pes, l, B, cwT_d, cvb_d, lng_d, lnb_d):
    cx, nc = P.cx, P.nc
    nq, nkv = NQR[l] * GW, NKVR[l] * GW
    SEG = 2048
    co = pes.enter_context(nc.sbuf_tensor(cx.key("cvo"), [128, 8, SEG], F32))
    zp = Pool(nc, pes, cx.key("cvz"), [128, SEG + 2 * CV_HALO], F32, 2)
    cw = pes.enter_context(nc.sbuf_tensor(cx.key("cvw"), [128, 8, 31], F32))
    cb = pes.enter_context(nc.sbuf_tensor(cx.key("cvb"), [128, 8], F32))
    lg = pes.enter_context(nc.sbuf_tensor(cx.key("cvg"), [128, 8], F32))
    lb = pes.enter_context(nc.sbuf_tensor(cx.key("cvlb"), [128, 8], F32))
    sqp = Pool(nc, pes, cx.key("cvs"), [128, 512], F32, 3)
    stp = Pool(nc, pes, cx.key("cvt"), [128, 512], F32, 6)
    obp = Pool(nc, pes, cx.key("cvob"), [128, 512], BF16, 4)
    cx.dma('sp', cw[:], cwT_d, writes=['cvw'])
    cx.dma('sp', cb[:], cvb_d, writes=['cvw'])
    cx.dma('sp', lg[:], lng_d, writes=['cvw'])
    cx.dma('sp', lb[:], lnb_d, writes=['cvw'])
    segs = [(s0, min(SEG, nq - s0), False) for s0 in range(0, nq, SEG)] + [(nq, CL, True)]
    for si, (s0, sn, isc) in enumerate(segs):
        for j in range(8):
            e = 'dve'
            E = nc.vector
            z, zk = zp.get()
            if isc:
                cx.op(e, lambda: E.memset(z[:, 0:CV_HALO], 0.0), writes=[zk])
                cx.op(e, lambda: E.memset(z[:, CV_HALO + CL:2 * CV_HALO + CL], 0.0), writes=[zk])
                cx.dma('sp', z[:, CV_HALO:CV_HALO + CL], B['ZT'][j * 128:(j + 1) * 128, nkv:nkv + CL], writes=[zk])
            else:
                zc = 256 + s0 - CV_HALO
                cx.dma('sp', z[:, 0:sn + 2 * CV_HALO], B['ZT'][j * 128:(j + 1) * 128, zc:zc + sn + 2 * CV_HALO], writes=[zk])
            acc = co[:, j, 0:sn]
            ck = f"cvo{j}"
            cx.op(e, lambda: E.tensor_scalar(acc, z[:, 0:sn], cw[:, j, 0:1], cb[:, j:j + 1], ALU.mult, ALU.add),
                  reads=[zk, 'cvw'], writes=[ck])
            for tap in range(1, 31):
                cx.op(e, lambda: E.scalar_tensor_tensor(acc, z[:, tap:tap + sn], cw[:, j, tap:tap + 1], acc,
                                                        ALU.mult, ALU.add), reads=[zk, 'cvw', ck], writes=[ck])
        for t0 in range(0, sn, 512):
            tn = min(512, sn - t0)
            bmu, bsq = cx.bank(), cx.bank()
            for j in range(8):
                ck = f"cvo{j}"
                cx.mm([lambda: nc.tensor.matmul(P.ps[:, bmu, :tn], P.ones_f[:], co[:, j, t0:t0 + tn], start=(j == 0), stop=(j == 7))],
                      reads=[ck, 'ones_f'], writes=[f"ps{bmu}"], acc=(j > 0))
                sq, sk = sqp.get()
                cx.op('act', lambda: nc.scalar.activation(sq[:, :tn], co[:, j, t0:t0 + tn], AF.Square), reads=[ck], writes=[sk])
                cx.mm([lambda: nc.tensor.matmul(P.ps[:, bsq, :tn], P.ones_f[:], sq[:, :tn], start=(j == 0), stop=(j == 7))],
                      reads=[sk, 'ones_f'], writes=[f"ps{bsq}"], acc=(j > 0))
            mu, mk = stp.get()
            ms, msk = stp.get()
            rs, rk = stp.get()
            cx.op('dve', lambda: nc.vector.tensor_scalar(mu[:, :tn], P.ps[:, bmu, :tn], 1.0 / 1024, None, ALU.mult), reads=[f"ps{bmu}"], writes=[mk])
            cx.op('dve', lambda: nc.vector.tensor_tensor(ms[:, :tn], mu[:, :tn], mu[:, :tn], ALU.mult), reads=[mk], writes=[msk])
            cx.op('dve', lambda: nc.vector.scalar_tensor_tensor(rs[:, :tn], P.ps[:, bsq, :tn], 1.0 / 1024, ms[:, :tn], ALU.mult, ALU.subtract),
                  reads=[f"ps{bsq}", msk], writes=[rk])
            cx.op('dve', lambda: nc.vector.tensor_scalar(rs[:, :tn], rs[:, :tn], EPS, None, ALU.add), reads=[rk], writes=[rk])
            cx.op('act', lambda: nc.scalar.activation(rs[:, :tn], rs[:, :tn], AF.Sqrt), reads=[rk], writes=[rk])
            cx.op('dve', lambda: nc.vector.reciprocal(rs[:, :tn], rs[:, :tn]), reads=[rk], writes=[rk])
            for j in range(8):
                ck = f"cvo{j}"
                t1, k1 = sqp.get()
                cx.op('dve', lambda: nc.vector.tensor_tensor(t1[:, :tn], co[:, j, t0:t0 + tn], mu[:, :tn], ALU.subtract), reads=[ck, mk], writes=[k1])
                cx.op('pool', lambda: nc.gpsimd.tensor_tensor(t1[:, :tn], t1[:, :tn], rs[:, :tn], ALU.mult), reads=[k1, rk], writes=[k1])
                ob, obk = obp.get()
                cx.op('act', lambda: nc.scalar.activation(ob[:, :tn], t1[:, :tn], AF.Silu, bias=lb[:, j:j + 1], scale=lg[:, j:j + 1]),
                      reads=[k1, 'cvw'], writes=[obk])
                cx.dma('sp', B['OCV'][j * 128:(j + 1) * 128, s0 + t0:s0 + t0 + tn], ob[:, :tn], reads=[obk])


def phase_merge(P, pes, l, B, XB, wbr, wbr_r0, wout, wout_r0, mod):
    cx, nc = P.cx, P.nc
    otp = Pool(nc, pes, cx.key("mo"), [128, 24, 512], BF16, 2)
    ytp = Pool(nc, pes, cx.key("my"), [128, 16, 512], BF16, 2)
    gtp = Pool(nc, pes, cx.key("mg"), [128, 3, 512], F32, 3)
    tmp = Pool(nc, pes, cx.key("mt"), [128, 512], F32, 6)
    xp = Pool(nc, pes, cx.key("mx"), [128, 512], F32, 4)
    gs_v = B['GST'].rearrange("(i m p) t -> p i m t", i=3, p=128)
    srcs = (B['ONA'], B['OCV'], B['OWA'])
    for (xcol, t0, tn, isc) in o_tiles(l):
        c = 1 if isc else 0
        ot, otk = otp.get()
        for i in range(3):
            cx.dma('sp', ot[:, i * 8:(i + 1) * 8, :tn], srcs[i][:, t0:t0 + tn].rearrange("(k p) t -> p k t", p=128), writes=[otk])
        yT, yk = ytp.get()
        for mg in range(4):
            wts = [P.load_w(wbr, wbr_r0 + i * 1024, 8, mg * 512, 512) for i in range(3)]
            for m in range(4):
                mi = mg * 4 + m
                gt, gk = gtp.get()
                cx.dma('sp', gt[:, :, :tn], gs_v[:, :, mi, t0:t0 + tn], writes=[gk])
                bs = []
                for i in range(3):
                    b = cx.bank()
                    wt, wk = wts[i]
                    fns = [(lambda k=k: nc.tensor.matmul(P.ps[:, b, :tn], wt[:, k, m * 128:(m + 1) * 128], ot[:, i * 8 + k, :tn],
                                                         start=(k == 0), stop=(k == 7))) for k in range(8)]
                    cx.mm(fns, reads=[otk, wk], writes=[f"ps{b}"])
                    bs.append(b)
                ys = []
                for i in range(3):
                    t1, k1 = tmp.get()
                    cx.op('dve', lambda: nc.vector.tensor_tensor(t1[:, :tn], P.ps[:, bs[i], :tn], gt[:, i, :tn], ALU.mult),
                          reads=[f"ps{bs[i]}", gk], writes=[k1])
                    ys.append((t1, k1))
                cx.op('dve', lambda: nc.vector.tensor_tensor(ys[0][0][:, :tn], ys[0][0][:, :tn], ys[1][0][:, :tn], ALU.add),
                      reads=[ys[0][1], ys[1][1]], writes=[ys[0][1]])
                cx.op('dve', lambda: nc.vector.tensor_tensor(yT[:, mi, :tn], ys[0][0][:, :tn], ys[2][0][:, :tn], ALU.add),
                      reads=[ys[0][1], ys[2][1]], writes=[yk])
        for mg in range(4):
            wt, wk = P.load_w(wout, wout_r0, 16, mg * 512, 512)
            for m in range(4):
                mi = mg * 4 + m
                b = cx.bank()
                fns = [(lambda k=k: nc.tensor.matmul(P.ps[:, b, :tn], wt[:, k, m * 128:(m + 1) * 128], yT[:, k, :tn],
                                                     start=(k == 0), stop=(k == 15))) for k in range(16)]
                cx.mm(fns, reads=[yk, wk], writes=[f"ps{b}"])
                xt, xk = xp.get()
                cx.dma('sp', xt[:, :tn], XB[mi * 128:(mi + 1) * 128, xcol:xcol + tn], writes=[xk])
                cx.op('dve', lambda: nc.vector.scalar_tensor_tensor(xt[:, :tn], P.ps[:, b, :tn], mod[:, 32 + mi, c:c + 1], xt[:, :tn],
                                                                    ALU.mult, ALU.add), reads=[f"ps{b}", xk, 'mod'], writes=[xk])
                cx.dma('sp', XB[mi * 128:(mi + 1) * 128, xcol:xcol + tn], xt[:, :tn], reads=[xk])


def ffn_group(P, res, h2, tl, wi, wi_r0, wo, wo_r0, dff, Ge, XB, modg):
    cx, nc = P.cx, P.nc
    nj = dff // 128
    g, gk = res['g'], 'ffg'
    tmp, xp = res['tmp'], res['xp']
    for jg in range(0, nj, 4):
        wa, ka = P.load_w(wi, wi_r0, 16, jg * 128, 512)
        wb, kb = P.load_w(wi, wi_r0, 16, dff + jg * 128, 512)
        for m in range(4):
            j = jg + m
            for (off, xcol, n, c) in tl:
                b1, b2 = cx.bank(), cx.bank()
                for (bb, ww, kk) in ((b1, wa, ka), (b2, wb, kb)):
                    fns = [(lambda k=k: nc.tensor.matmul(P.ps[:, bb, :n], ww[:, k, m * 128:(m + 1) * 128], h2[:, k, off:off + n],
                                                         start=(k == 0), stop=(k == 15))) for k in range(16)]
                    cx.mm(fns, reads=['h2', kk], writes=[f"ps{bb}"])
                t1, k1 = tmp.get()
                cx.op('act', lambda: nc.scalar.activation(t1[:, :n], P.ps[:, b1, :n], AF.Silu), reads=[f"ps{b1}"], writes=[k1])
                cx.op('dve', lambda: nc.vector.tensor_tensor(g[:, j, off:off + n], t1[:, :n], P.ps[:, b2, :n], ALU.mult),
                      reads=[k1, f"ps{b2}"], writes=[gk])
    wc = 256 if nj * 256 <= 8192 else 128
    for m0 in range(0, 16, wc // 128):
        wt, wk = P.load_w(wo, wo_r0, nj, m0 * 128, wc)
        for mm in range(wc // 128):
            m = m0 + mm
            for (off, xcol, n, c) in tl:
                b = cx.bank()
                fns = [(lambda k=k: nc.tensor.matmul(P.ps[:, b, :n], wt[:, k, mm * 128:(mm + 1) * 128], g[:, k, off:off + n],
                                                     start=(k == 0), stop=(k == nj - 1))) for k in range(nj)]
                cx.mm(fns, reads=[gk, wk], writes=[f"ps{b}"])
                xt, xk = xp.get()
                dk = f"XB_{m}_{xcol}"
                cx.dma('sp', xt[:, :n], XB[m * 128:(m + 1) * 128, xcol:xcol + n], reads=[dk], writes=[xk])
                if Ge is None:
                    cx.op('dve', lambda: nc.vector.scalar_tensor_tensor(xt[:, :n], P.ps[:, b, :n], modg(m, c), xt[:, :n],
                                                                        ALU.mult, ALU.add), reads=[f"ps{b}", xk, 'mod'], writes=[xk])
                else:
                    t1, k1 = tmp.get()
                    cx.op('dve', lambda: nc.vector.scalar_tensor_tensor(t1[:, :n], P.ps[:, b, :n], modg(m, c), Ge[0][:, off:off + n],
                                                                        ALU.mult, ALU.mult), reads=[f"ps{b}", Ge[1], 'mod'], writes=[k1])
                    cx.op('dve', lambda: nc.vector.tensor_tensor(xt[:, :n], xt[:, :n], t1[:, :n], ALU.add), reads=[xk, k1], writes=[xk])
                cx.dma('sp', XB[m * 128:(m + 1) * 128, xcol:xcol + n], xt[:, :n], reads=[xk], writes=[dk])


def phase_ffn(P, pes, l, XB, mod, A2, moe, wi, wi_r0, wo, wo_r0, wr_d=None, ident_d=None, selm_d=None, skip_ctx=False):
    cx, nc = P.cx, P.nc
    TG = 1024
    h2 = pes.enter_context(nc.sbuf_tensor(cx.key("h2"), [128, 16, TG], BF16))
    res = {'g': pes.enter_context(nc.sbuf_tensor(cx.key("ffg"), [128, 32 if moe else 44, TG], BF16)),
           'tmp': Pool(nc, pes, cx.key("fft"), [128, 512], F32, 4),
           'xp': Pool(nc, pes, cx.key("ffx"), [128, 512], F32, 3)}
    pools = (Pool(nc, pes, cx.key("rx"), [128, 512], F32, 2), Pool(nc, pes, cx.key("rsq"), [128, 512], F32, 2),
             Pool(nc, pes, cx.key("rr"), [128, 512], F32, 2))
    if moe:
        wr = pes.enter_context(nc.sbuf_tensor(cx.key("wr"), [128, 16, 8], F32))
        ident = pes.enter_context(nc.sbuf_tensor(cx.key("ident"), [128, 128], F32))
        selm = pes.enter_context(nc.sbuf_tensor(cx.key("selm"), [8, 8 * 128], F32))
        lgT = pes.enter_context(nc.sbuf_tensor(cx.key("lgT"), [8, 512], F32))
        L = pes.enter_context(nc.sbuf_tensor(cx.key("rL"), [128, 4, 8], F32))
        W1 = pes.enter_context(nc.sbuf_tensor(cx.key("rW1"), [128, 4, 8], F32))
        W2 = pes.enter_context(nc.sbuf_tensor(cx.key("rW2"), [128, 4, 8], F32))
        m1 = pes.enter_context(nc.sbuf_tensor(cx.key("rm1"), [128, 4], F32))
        m2 = pes.enter_context(nc.sbuf_tensor(cx.key("rm2"), [128, 4], F32))
        gT = pes.enter_context(nc.sbuf_tensor(cx.key("rgT"), [8, TG], F32))
        Gp = Pool(nc, pes, cx.key("rG"), [128, TG], F32, 2)
        cx.dma('sp', wr[:], wr_d.rearrange("(k p) e -> p k e", p=128), writes=['wr'])
        cx.dma('sp', ident[:], ident_d, writes=['ident'])
        cx.dma('sp', selm[:], selm_d, writes=['selm'])
    tiles = [(xcol, n, 1 if isc else 0) for (xcol, _, n, isc) in o_tiles(l) if not (isc and skip_ctx)]
    groups, cur, tot = [], [], 0
    for (xcol, n, c) in tiles:
        if tot + n > TG:
            groups.append(cur)
            cur, tot = [], 0
        cur.append((tot, xcol, n, c))
        tot += n
    groups.append(cur)
    modg = lambda m, c: mod[:, 80 + m, c:c + 1]
    for tl in groups:
        for (off, xcol, tn, c) in tl:
            nb = tn // 128
            hf_cb = None
            if moe:
                br = cx.bank()

                def hf_cb(k, n, hf, hk):
                    cx.mm([lambda: nc.tensor.matmul(P.ps[0:8, br, :n], wr[:, k, :], hf[:, :n], start=(k == 0), stop=(k == 15))],
                          reads=[hk, 'wr'], writes=[f"ps{br}"], acc=(k > 0))
            rms_norm_tile(P, pools, XB, xcol, tn, c, A2, mod[:, 48:64, :], lambda k: (h2[:, k, off:off + tn], 'h2'), hf_cb=hf_cb)
            if moe:
                cx.op('dve', lambda: nc.vector.tensor_copy(lgT[:, :tn], P.ps[0:8, br, :tn]), reads=[f"ps{br}"], writes=['lgT'])
                bt = cx.bank()
                for blk in range(nb):
                    cx.mm([lambda: nc.tensor.matmul(P.ps[:, bt, blk * 8:(blk + 1) * 8], lgT[:, blk * 128:(blk + 1) * 128], ident[0:8, 0:8],
                                                    start=True, stop=True)], reads=['lgT', 'ident'], writes=[f"ps{bt}"], acc=(blk > 0))
                Lv, W1v, W2v = L[:, :nb, :], W1[:, :nb, :], W2[:, :nb, :]
                cx.op('dve', lambda: nc.vector.tensor_copy(Lv, P.ps[:, bt, 0:nb * 8].rearrange("p (b e) -> p b e", e=8)),
                      reads=[f"ps{bt}"], writes=['rL'])
                cx.op('dve', lambda: nc.vector.tensor_reduce(m1[:, :nb], Lv, AX.X, ALU.max), reads=['rL'], writes=['rm1'])
                for blk in range(nb):
                    cx.op('dve', lambda: nc.vector.tensor_scalar(W1[:, blk, :], L[:, blk, :], m1[:, blk:blk + 1], None, ALU.is_equal),
                          reads=['rL', 'rm1'], writes=['rW1'])
                cx.op('dve', lambda: nc.vector.scalar_tensor_tensor(W2v, W1v, NEG, Lv, ALU.mult, ALU.add), reads=['rW1', 'rL'], writes=['rW2'])
                cx.op('dve', lambda: nc.vector.tensor_reduce(m2[:, :nb], W2v, AX.X, ALU.max), reads=['rW2'], writes=['rm2'])
                for blk in range(nb):
                    cx.op('dve', lambda: nc.vector.tensor_scalar(W1[:, blk, :], L[:, blk, :], m2[:, blk:blk + 1], None, ALU.is_ge),
                          reads=['rL', 'rm2', 'rW1'], writes=['rW1'])
                    cx.op('dve', lambda: nc.vector.tensor_scalar(W2[:, blk, :], L[:, blk, :], m1[:, blk:blk + 1], None, ALU.subtract),
                          reads=['rL', 'rm1', 'rW2'], writes=['rW2'])
                cx.op('act', lambda: nc.scalar.activation(W2v, W2v, AF.Exp), reads=['rW2'], writes=['rW2'])
                cx.op('dve', lambda: nc.vector.tensor_tensor(W2v, W2v, W1v, ALU.mult), reads=['rW2', 'rW1'], writes=['rW2'])
                cx.op('dve', lambda: nc.vector.tensor_reduce(m1[:, :nb], W2v, AX.X, ALU.add), reads=['rW2', 'rm1'], writes=['rm1'])
                cx.op('dve', lambda: nc.vector.reciprocal(m1[:, :nb], m1[:, :nb]), reads=['rm1'], writes=['rm1'])
                for blk in range(nb):
                    cx.op('dve', lambda: nc.vector.tensor_scalar(W2[:, blk, :], W2[:, blk, :], m1[:, blk:blk + 1], None, ALU.mult),
                          reads=['rW2', 'rm1'], writes=['rW2'])
                bg = cx.bank()
                for blk in range(nb):
                    cx.mm([lambda: nc.tensor.matmul(P.ps[0:8, bg, blk * 128:(blk + 1) * 128], W2[:, blk, :], ident[:, :], start=True, stop=True)],
                          reads=['rW2', 'ident'], writes=[f"ps{bg}"], acc=(blk > 0))
                cx.op('dve', lambda: nc.vector.tensor_copy(gT[:, off:off + tn], P.ps[0:8, bg, :tn]), reads=[f"ps{bg}"], writes=['rgT'])
        if not moe:
            ffn_group(P, res, h2, tl, wi, wi_r0, wo, wo_r0, D_FF, None, XB, modg)
        else:
            for e in range(NE):
                Ge, Gk = Gp.get()
                for (off, xcol, tn, c) in tl:
                    be = cx.bank()
                    cx.mm([lambda: nc.tensor.matmul(P.ps[:, be, :tn], selm[:, e * 128:(e + 1) * 128], gT[:, off:off + tn], start=True, stop=True)],
                          reads=['selm', 'rgT'], writes=[f"ps{be}"])
                    cx.op('act', lambda: nc.scalar.activation(Ge[:, off:off + tn], P.ps[:, be, :tn], AF.Copy), reads=[f"ps{be}"], writes=[Gk])
                ffn_group(P, res, h2, tl, wi, wi_r0 + e * D, wo, wo_r0 + e * D_FFE, D_FFE, (Ge, Gk), XB, modg)


def phase_final(P, pes, XB, gfT_d, yo):
    cx, nc = P.cx, P.nc
    gf = pes.enter_context(nc.sbuf_tensor(cx.key("gf"), [128, 16], F32))
    cx.dma('sp', gf[:], gfT_d, writes=['gf'])
    xp = Pool(nc, pes, cx.key("fx"), [128, 16, 512], F32, 2)
    sqp = Pool(nc, pes, cx.key("fsq"), [128, 512], F32, 3)
    rp = Pool(nc, pes, cx.key("fr"), [128, 512], F32, 2)
    for i in range(4):
        xcol = 1024 + 512 * i
        xt, xk = xp.get()
        cx.dma('sp', xt[:], XB[:, xcol:xcol + 512].rearrange("(k p) t -> p k t", p=128), writes=[xk])
        b = cx.bank()
        for k in range(16):
            sq, sk = sqp.get()
            cx.op('act', lambda: nc.scalar.activation(sq[:], xt[:, k, :], AF.Square), reads=[xk], writes=[sk])
            cx.mm([lambda: nc.tensor.matmul(P.ps[:, b, :], P.ones_f[:], sq[:], start=(k == 0), stop=(k == 15))],
                  reads=[sk, 'ones_f'], writes=[f"ps{b}"], acc=(k > 0))
        rt, rk = rp.get()
        cx.op('dve', lambda: nc.vector.tensor_scalar(rt[:], P.ps[:, b, :], 1.0 / D, EPS, ALU.mult, ALU.add), reads=[f"ps{b}"], writes=[rk])
        cx.op('act', lambda: nc.scalar.activation(rt[:], rt[:], AF.Sqrt), reads=[rk], writes=[rk])
        cx.op('dve', lambda: nc.vector.reciprocal(rt[:], rt[:]), reads=[rk], writes=[rk])
        for k in range(16):
            e = 'dve'
            E = nc.vector
            cx.op(e, lambda: E.scalar_tensor_tensor(xt[:, k, :], xt[:, k, :], gf[:, k:k + 1], rt[:], ALU.mult, ALU.mult),
                  reads=[xk, rk, 'gf'], writes=[xk])
        cx.dma('sp', yo[:, 512 * i:512 * (i + 1)].rearrange("(k p) t -> p k t", p=128), xt[:], reads=[xk])


def build_all(depth=DEPTH, dbg=None):
    es = ExitStack()
    P = Prog(es)
    nc, cx = P.nc, P.cx
    I = lambda n, s, d: P.dram(n, s, d, "ExternalInput")
    n_dense, n_moe = (depth + 1) // 2, depth // 2
    L0 = DEPTH - depth
    xin = I("xT", [D, XC], F32)
    cT = I("cT", [128, 16, 2], F32)
    wmod = I("wmod", [depth * D, 6 * D], F32)
    bmodT = I("bmodT", [depth, 128, 96], F32)
    gT_d = I("gT", [128, depth * 32 + 16], F32)
    win = I("win", [depth * D, IN_DIM], F32)
    cosd, sind, tval = I("cosT", [128, XW], F32), I("sinT", [128, XW], F32), I("tval", [128, XW], F32)
    ttab = I("ttab", [depth * 8, 128, NIDX * 64], F32)
    maskL = [I(f"maskL{l}", [8, (NQR[L0 + l] // 8) * NKT * 128], BF16) for l in range(depth)]
    rsel = I("rsel", [8, 512], BF16)
    m3 = I("m3", [128, 384], F32)
    kvb = [I(f"kvb{l}", [128, NKVR[L0 + l] * GW // 128], F32) for l in range(depth)]
    sinkb = I("sinkb", [depth, 128, 8], F32)
    cwT = I("cwT", [depth, 128, 8, 31], F32)
    cvp = I("cvp", [depth, 3, 128, 8], F32)
    wbr = I("wbr", [depth * 3072, D], F32)
    wout = I("wout", [depth * D, D], F32)
    fwi = I("fwi", [n_dense * D, 2 * D_FF], F32)
    fwo = I("fwo", [n_dense * D_FF, D], F32)
    if n_moe:
        mwi = I("mwi", [n_moe * NE * D, 2 * D_FFE], F32)
        mwo = I("mwo", [n_moe * NE * D_FFE, D], F32)
        mwr = I("mwr", [n_moe * D, NE], F32)
        ident = I("ident", [128, 128], F32)
        selm = I("selm", [8, 8 * 128], F32)
    yo = P.dram("yo", [D, TL], F32, "ExternalOutput")
    T = lambda n, s, d: P.dram(n, s, d, "Internal")
    XB = T("XB", [D, XC], F32)
    HT = T("HT", [D, XC], BF16)
    NQM = NQR[0] * GW + CL
    KWM = NKVR[0] * GW + 2 * NAM + CL
    B = {'NAQ': T("NAQ", [1024, NQM], BF16), 'NAK': T("NAK", [1024, KWM], BF16), 'NAV': T("NAV", [KWM, 1024], BF16),
         'WAQ': T("WAQ", [1024, NQM], BF16), 'WAK': T("WAK", [256, XC], BF16), 'WAV': T("WAV", [XC, 256], BF16),
         'ZT': T("ZT", [1024, XC], F32), 'GST': T("GST", [6144, NQM], F32),
         'ONA': T("ONA", [1024, NQM], BF16), 'OCV': T("OCV", [1024, NQM], BF16), 'OWA': T("OWA", [1024, NQM], BF16)}
    dbg_out = {}
    if dbg:
        for k in dbg:
            a = B[k] if k in B else {'XB': XB, 'HT': HT}[k]
            dbg_out[k] = P.dram("dbg_" + k, list(a.shape), a.dtype, "ExternalOutput")
    mod = es.enter_context(nc.sbuf_tensor("mod", [128, 96, 2], F32))
    A = es.enter_context(nc.sbuf_tensor("modA", [128, 16, 2], F32))
    gT = es.enter_context(nc.sbuf_tensor("gT_sb", [128, depth * 32 + 16], F32))
    cx.dma('sp', gT[:], gT_d, writes=['gT'])
    for i in range(4):
        cx.dma('sp' if i % 2 == 0 else 'act', XB[i * 512:(i + 1) * 512, :], xin[i * 512:(i + 1) * 512, :], writes=['XB'])
    with ExitStack() as pes:
        zt = pes.enter_context(nc.sbuf_tensor("zt", [128, 8192], BF16))
        cx.op('pool', lambda: nc.gpsimd.memset(zt[:], 0.0), writes=['zt'])
        for r in range(8):
            cx.dma('sp', B['NAK'][r * 128:(r + 1) * 128, :], zt[:, 0:KWM], reads=['zt'], writes=['NAK'])
        nav_v = B['NAV'].rearrange("(t p) d -> p t d", p=128)
        ntile = KWM // 128
        for t0 in range(0, ntile, 8):
            tn = min(8, ntile - t0)
            cx.dma('sp', nav_v[:, t0:t0 + tn, :], zt[:, 0:tn * 1024].rearrange("p (t d) -> p t d", d=1024), reads=['zt'], writes=['NAV'])
        barrier(cx)
    for l in range(depth):
        g = L0 + l
        moe = (l % 2 == 1)
        li = l // 2
        last = (l == depth - 1)
        with ExitStack() as pes:
            phase_modvec(P, cT, wmod[l * D:(l + 1) * D, :], bmodT[l], mod, pes)
            make_A(P, mod, gT, A, 16, l * 32)
            barrier(cx)
        with ExitStack() as pes:
            phase_norm1(P, pes, g, XB, HT, A, mod[:, 0:16, :])
            barrier(cx)
        with ExitStack() as pes:
            phase_inproj(P, pes, g, HT, win, l * D, cosd, sind, tval, B)
            barrier(cx)
        with ExitStack() as pes:
            phase_conv(P, pes, g, B, cwT[l], cvp[l, 0], cvp[l, 1], cvp[l, 2])
            barrier(cx)
        with ExitStack() as pes:
            phase_na(P, pes, g, B, lambda h: ttab[l * 8 + h], maskL[l], rsel)
            barrier(cx)
        with ExitStack() as pes:
            phase_wa(P, pes, g, B, m3, kvb[l], sinkb[l])
            barrier(cx)
        with ExitStack() as pes:
            phase_merge(P, pes, g, B, XB, wbr, l * 3072, wout, l * D, mod)
            barrier(cx)
        make_A(P, mod, gT, A, 64, l * 32 + 16)
        with ExitStack() as pes:
            if moe:
                phase_ffn(P, pes, g, XB, mod, A, True, mwi, li * NE * D, mwo, li * NE * D_FFE, mwr[li * D:(li + 1) * D, :], ident, selm,
                          skip_ctx=last)
            else:
                phase_ffn(P, pes, g, XB, mod, A, False, fwi, li * D, fwo, li * D_FF, skip_ctx=last)
            barrier(cx)
    for k, o in dbg_out.items():
        a = B[k] if k in B else {'XB': XB, 'HT': HT}[k]
        cx.dma('sp', o, a)
    with ExitStack() as pes:
        phase_final(P, pes, XB, gT_d[:, depth * 32:depth * 32 + 16], yo)
        barrier(cx)
    cx.finish('sp')
    print("fused program: depth", depth, "inst", cx.n_inst, "waits", cx.n_wait)
    return P


def fm(v, n):
    return np.ascontiguousarray(np.asarray(v, dtype=np.float32).reshape(n, 128).T)


def rope_tables(tok0, ntok):
    t = np.arange(tok0, tok0 + ntok, dtype=np.int64)
    pos = np.stack([t // GW, t % GW], axis=-1).astype(np.float32)
    inv_freq = (1.0 / np.power(np.float32(10000.0), np.arange(32, dtype=np.float32) / np.float32(32))).astype(np.float32)
    ang = (pos[:, :, None] * inv_freq).astype(np.float32)
    cos, sin = np.cos(ang).astype(np.float32), np.sin(ang).astype(np.float32)
    cosT = np.zeros((128, ntok), np.float32)
    sinT = np.zeros((128, ntok), np.float32)
    for a in range(2):
        for j in range(2):
            sl = slice(a * 64 + j * 32, a * 64 + j * 32 + 32)
            cosT[sl] = cos[:, a, :].T
            sinT[sl] = (-1.0 if j == 0 else 1.0) * sin[:, a, :].T
    return cosT, sinT


def na_bias_table(rpb_l):
    p = np.arange(128)
    a, kc = p // 64, p % 64
    idx = np.arange(NIDX)
    c = np.arange(64)
    dr = a[:, None] + 13 - idx[None, :]
    drv = (np.abs(dr) <= 7)
    ws = np.clip(c - 8, 0, 48)
    colv = (kc[:, None] >= ws[None, :]) & (kc[:, None] < ws[None, :] + 16)
    cidx = np.clip(kc[:, None] - c[None, :] + 15, 0, 30)
    g = rpb_l[:, np.clip(dr + 7, 0, 14)[:, :, None], cidx[:, None, :]]
    g = np.where(drv[None, :, :, None], g, np.float32(0.0))
    g = np.where(colv[None, :, None, :], g, np.float32(NEG))
    return np.ascontiguousarray(g.reshape(8, 128, NIDX * 64).astype(np.float32))


def na_mask_lhs(s_row, nqb, rows):
    m = np.zeros((8, nqb, NKT, 2, 64), np.float32)
    for qb in range(nqb):
        for t in range(NKT):
            for a in range(2):
                kr = s_row + 8 * qb - 7 + 2 * t + a
                for rp in range(8):
                    r = s_row + 8 * qb + rp
                    r0 = min(max(r - 4, 0), rows - 8)
                    if not ((0 <= kr < rows) and (r0 <= kr < r0 + 8)):
                        m[rp, qb, t, a, :] = NEG
    return np.ascontiguousarray(m.reshape(8, nqb * NKT * 128)).astype(ml_dtypes.bfloat16)


def wa_band_mask():
    k = np.arange(128)[:, None]
    rel = np.arange(384)[None, :] // 128
    q = np.arange(384)[None, :] % 128
    ok = np.abs((rel - 1) * 128 + q - k) <= 128
    return np.where(ok, np.float32(0), np.float32(NEG)).astype(np.float32)


RSEL = np.zeros((8, 512), np.float32)
for _r in range(8):
    RSEL[_r, _r * 64:(_r + 1) * 64] = 1.0
RSEL = RSEL.astype(ml_dtypes.bfloat16)
IDENT = np.eye(128, dtype=np.float32)
SELM = np.zeros((8, 8 * 128), np.float32)
for _e in range(8):
    SELM[_e, _e * 128:(_e + 1) * 128] = 1.0

_PROGS = {}


def run_model(inp, depth, dbg=None, trace=False):
    x = np.asarray(inp['x'], np.float32)
    Bn, S, _ = x.shape
    rows = S // GW
    cpb = rows // 32
    ncore = Bn * cpb
    L0 = DEPTH - depth
    n_dense, n_moe = (depth + 1) // 2, depth // 2
    key = (depth, tuple(dbg) if dbg else None)
    if key not in _PROGS:
        _PROGS[key] = build_all(depth, dbg)
    P = _PROGS[key]
    f32 = lambda a: np.ascontiguousarray(np.asarray(a, np.float32))
    shared = dict(
        wmod=f32(inp['w_mod'][:depth]).reshape(depth * D, 6 * D),
        bmodT=np.stack([fm(inp['b_mod'][l], 96) for l in range(depth)]),
        gT=np.concatenate([np.concatenate([fm(inp['g_norm1'][l], 16), fm(inp['g_norm2'][l], 16)], axis=1) for l in range(depth)]
                          + [fm(inp['g_final'], 16)], axis=1),
        win=f32(inp['w_in'][:depth]).reshape(depth * D, IN_DIM),
        ttab=np.concatenate([na_bias_table(f32(inp['rpb'][l])) for l in range(depth)], axis=0),
        rsel=RSEL, m3=wa_band_mask(),
        sinkb=np.stack([np.ascontiguousarray(np.broadcast_to(f32(inp['sink'][l])[None, :], (128, 8))) for l in range(depth)]),
        cwT=np.stack([np.ascontiguousarray(f32(inp['conv_w'][l]).T.reshape(8, 128, 31).transpose(1, 0, 2)) for l in range(depth)]),
        cvp=np.stack([np.stack([fm(inp['conv_b'][l], 8), fm(inp['ln_g'][l], 8), fm(inp['ln_b'][l], 8)]) for l in range(depth)]),
        wbr=f32(inp['w_branch'][:depth]).reshape(depth * 3072, D),
        wout=f32(inp['w_out'][:depth]).reshape(depth * D, D),
        fwi=f32(inp['ffn_wi'][:n_dense]).reshape(n_dense * D, 2 * D_FF),
        fwo=f32(inp['ffn_wo'][:n_dense]).reshape(n_dense * D_FF, D),
    )
    if n_moe:
        shared.update(mwi=f32(inp['moe_wi'][:n_moe]).reshape(n_moe * NE * D, 2 * D_FFE),
                      mwo=f32(inp['moe_wo'][:n_moe]).reshape(n_moe * NE * D_FFE, D),
                      mwr=f32(inp['moe_router'][:n_moe]).reshape(n_moe * D, NE), ident=IDENT, selm=SELM)
    maps = []
    for core in range(ncore):
        b, ci = core // cpb, core % cpb
        R0 = 32 * ci
        w0 = R0 - 16
        xw = np.zeros((XW, D), np.float32)
        lo, hi = max(w0, 0), min(w0 + WROWS, rows)
        xw[(lo - w0) * GW:(hi - w0) * GW] = x[b, lo * GW:hi * GW]
        xT = np.ascontiguousarray(np.concatenate([xw.T, f32(inp['ctx'][b]).T], axis=1))
        cT = np.ascontiguousarray(np.stack([fm(inp['c'][b], 16), fm(inp['c_ctx'], 16)], axis=-1))
        cosT, sinT = rope_tables(w0 * GW, XW)
        tok = np.arange(XW) + w0 * GW
        valid = (tok >= 0) & (tok < S)
        tval = np.ascontiguousarray(np.broadcast_to(valid.astype(np.float32)[None, :], (128, XW)))
        d = dict(shared, xT=xT, cT=cT, cosT=cosT, sinT=sinT, tval=tval)
        for l in range(depth):
            g = L0 + l
            d[f"maskL{l}"] = na_mask_lhs(w0 + 4 * (g + 1), NQR[g] // 8, rows)
            nkt = NKVR[g] * GW // 128
            kv = valid[256 * g:256 * g + nkt * 128].reshape(nkt, 128).T
            d[f"kvb{l}"] = np.ascontiguousarray(np.where(kv, np.float32(0), np.float32(NEG)).astype(np.float32))
        maps.append(d)
    res = run_bass_kernel_spmd(P.nc, maps, core_ids=list(range(ncore)), **({'trace': True} if trace else {}))
    out = np.zeros((Bn, S, D), np.float32)
    for core in range(ncore):
        b, ci = core // cpb, core % cpb
        out[b, ci * TL:(ci + 1) * TL, :] = np.asarray(res.results[core]['yo']).T
    return out, res


def kernel(**inputs):
    out, _ = run_model(inputs, DEPTH)
    return out
```
